# Optimizing a Trainium2 kernel written in Bass

```python
import jax
import jax.numpy as jnp
from jax import lax
import numpy as np

D_MODEL = 1024
BATCH = 2
SEQ = 16384
DEPTH = 2

CTX_LEN = 256
GRID_W = 64
N_MIXERS = 4
MIX_WIDTH = D_MODEL
GROUP_WIDTH = MIX_WIDTH // N_MIXERS
GROUP_HEADS = 4
HEAD_DIM = GROUP_WIDTH // GROUP_HEADS
NA_ROWS = 8
NA_COLS = 16
ML_CHUNK = 64
ML_CONV = 5
MLA_Q_RANK = 256
MLA_KV_RANK = 128
MLA_NOPE = 64
MLA_ROPE = 32
MLA_V = 64
SWA_KV_HEADS = 2
SWA_WINDOW = 128
ATTN_BLOCK = 128
PEER_HEADS = 8
PEER_NKEYS = 128
PEER_EXPERTS = PEER_NKEYS * PEER_NKEYS
PEER_DKEY = 128
PEER_TOPK = 16
PEER_BLOCK = 128
ROPE_BASE = 10000.0
EPS = 1e-6
IN_SIZES = (GROUP_WIDTH, GROUP_WIDTH, GROUP_WIDTH,
            2 * GROUP_WIDTH, GROUP_WIDTH, GROUP_WIDTH, 4 * GROUP_HEADS,
            MLA_Q_RANK, MLA_KV_RANK, MLA_ROPE,
            GROUP_WIDTH, SWA_KV_HEADS * HEAD_DIM, SWA_KV_HEADS * HEAD_DIM)
IN_WIDTH = sum(IN_SIZES)
F32 = jnp.float32

kernel_name = 'hybrid_na_mlstm_mla_swa_peer_dit'


def rmsnorm(x, g):
    xf = x.astype(F32)
    y = xf * lax.rsqrt(jnp.mean(xf * xf, axis=-1, keepdims=True) + EPS) * g.astype(F32)
    return y.astype(x.dtype)


def heads(a, h):
    return a.reshape(a.shape[:-1] + (h, a.shape[-1] // h))


def split_cols(p):
    return jnp.split(p, np.cumsum(IN_SIZES)[:-1].tolist(), axis=-1)


def axial_angles(T, rot_dim):
    t = jnp.arange(T)
    row = (t // GRID_W).astype(F32)
    col = (t % GRID_W).astype(F32)
    half = rot_dim // 2
    inv = 1.0 / (ROPE_BASE ** (jnp.arange(0, half, 2, dtype=F32) / half))
    return row[:, None] * inv, col[:, None] * inv


def rope_1d(x, ang):
    cos = jnp.cos(ang)[None, :, None, :]
    sin = jnp.sin(ang)[None, :, None, :]
    x1, x2 = jnp.split(x.astype(F32), 2, axis=-1)
    return jnp.concatenate([x1 * cos - x2 * sin, x1 * sin + x2 * cos], axis=-1)


def rope_2d(x, angs):
    xr, xc = jnp.split(x, 2, axis=-1)
    return jnp.concatenate([rope_1d(xr, angs[0]), rope_1d(xc, angs[1])], axis=-1).astype(x.dtype)


def ctx_attn(q, k, v, scale, sink=None):
    rep = q.shape[2] // k.shape[2]
    k = jnp.repeat(k, rep, axis=2)
    v = jnp.repeat(v, rep, axis=2)
    s = jnp.einsum('bqhd,bkhd->bhqk', q, k).astype(F32) * scale
    nk = s.shape[-1]
    if sink is not None:
        s = jnp.concatenate([s, jnp.broadcast_to(sink.astype(F32)[None, :, None, None], s.shape[:-1] + (1,))], axis=-1)
    p = jax.nn.softmax(s, axis=-1)[..., :nk].astype(v.dtype)
    out = jnp.einsum('bhqk,bkhd->bqhd', p, v)
    return out.reshape(out.shape[:2] + (-1,))


def neighbourhood_attention(q, k, v, kc, vc, rpb):
    B, T, H, d = q.shape
    rows = T // GRID_W
    nr = min(NA_ROWS, rows)
    scale = d ** -0.5
    qg = jnp.swapaxes(q.reshape(B, rows, GRID_W, H, d), 0, 1)
    kg = k.reshape(B, rows, GRID_W, H, d)
    vg = v.reshape(B, rows, GRID_W, H, d)
    col_start = np.clip(np.arange(GRID_W) - NA_COLS // 2, 0, GRID_W - NA_COLS)
    col_idx = col_start[:, None] + np.arange(NA_COLS)[None, :]
    dc = col_idx - np.arange(GRID_W)[:, None] + (NA_COLS - 1)
    rpb_c = rpb.astype(F32)[:, :, dc]
    n_loc = nr * NA_COLS

    def row_block(args):
        qr, r = args
        rs = jnp.clip(r - nr // 2, 0, rows - nr)
        kr = lax.dynamic_slice_in_dim(kg, rs, nr, axis=1)[:, :, col_idx]
        vr = lax.dynamic_slice_in_dim(vg, rs, nr, axis=1)[:, :, col_idx]
        dr = rs + jnp.arange(nr) - r + (NA_ROWS - 1)
        bias = jnp.take(rpb_c, dr, axis=1)
        s_loc = jnp.einsum('bqhd,brqchd->bhqrc', qr, kr).astype(F32) * scale + jnp.transpose(bias, (0, 2, 1, 3))[None]
        s_ctx = jnp.einsum('bqhd,bkhd->bhqk', qr, kc).astype(F32) * scale
        p = jax.nn.softmax(jnp.concatenate([s_loc.reshape(B, H, GRID_W, n_loc), s_ctx], axis=-1), axis=-1).astype(v.dtype)
        p_loc = p[..., :n_loc].reshape(B, H, GRID_W, nr, NA_COLS)
        return (jnp.einsum('bhqrc,brqchd->bqhd', p_loc, vr)
                + jnp.einsum('bhqk,bkhd->bqhd', p[..., n_loc:], vc))

    out = lax.map(row_block, (qg, jnp.arange(rows)))
    return jnp.swapaxes(out, 0, 1).reshape(B, T, H * d)


def short_conv(a, w):
    T = a.shape[1]
    pad = w.shape[0] // 2
    ap = jnp.pad(a, ((0, 0), (pad, pad), (0, 0)))
    out = ap[:, :T] * w[0]
    for j in range(1, w.shape[0]):
        out = out + ap[:, j:j + T] * w[j]
    return out


def mlstm_scan(q, k, v, ig, lf, state):
    B, T, H, d = q.shape
    nc = T // ML_CHUNK

    def chunks(a):
        a = a.astype(F32).reshape((B, nc, ML_CHUNK) + a.shape[2:])
        return jnp.swapaxes(jnp.swapaxes(a, 0, 1), 2, 3)

    seen = jnp.tril(jnp.ones((ML_CHUNK, ML_CHUNK), dtype=bool))

    def step(carry, inp):
        C, n, m = carry
        qt, kt, vt, it, ft = inp
        b = jnp.cumsum(ft, axis=-1)
        d_log = jnp.where(seen, b[..., :, None] - b[..., None, :] + it[..., None, :], -jnp.inf)
        inter = b + m[..., None]
        m_t = jnp.maximum(inter, jnp.max(d_log, axis=-1))
        w = jnp.exp(d_log - m_t[..., None])
        a = jnp.exp(inter - m_t)
        s = jnp.einsum('bhtk,bhsk->bhts', qt, kt) * w
        num = jnp.einsum('bhts,bhsv->bhtv', s, vt) + a[..., None] * jnp.einsum('bhtk,bhkv->bhtv', qt, C)
        den = jnp.sum(s, axis=-1) + a * jnp.einsum('bhtk,bhk->bht', qt, n)
        h = num / jnp.maximum(jnp.abs(den), jnp.exp(-m_t))[..., None]
        g = b[..., -1:] - b + it
        m_new = jnp.maximum(b[..., -1] + m, jnp.max(g, axis=-1))
        wk = jnp.exp(g - m_new[..., None])
        decay = jnp.exp(b[..., -1] + m - m_new)
        C = decay[..., None, None] * C + jnp.einsum('bhs,bhsk,bhsv->bhkv', wk, kt, vt)
        n = decay[..., None] * n + jnp.einsum('bhs,bhsk->bhk', wk, kt)
        return (C, n, m_new), h

    state, h = lax.scan(step, state, (chunks(q), chunks(k), chunks(v), chunks(ig), chunks(lf)))
    h = jnp.swapaxes(jnp.swapaxes(h, 2, 3), 0, 1).reshape(B, T, H, d)
    return h.astype(v.dtype), state


def mlstm_prep(qk, v, gates, conv_w, gate_b):
    qk = jax.nn.silu(short_conv(qk, conv_w))
    q, k = jnp.split(qk, 2, axis=-1)
    g = (gates + gate_b).astype(F32)
    i_f, f_f, i_b, f_b = jnp.split(g, 4, axis=-1)
    return (heads(q, GROUP_HEADS) * HEAD_DIM ** -0.5, heads(k, GROUP_HEADS), heads(v, GROUP_HEADS),
            (i_f, jax.nn.log_sigmoid(f_f), i_b, jax.nn.log_sigmoid(f_b)))


def mlstm_mixer(lat, ctx, conv_w, gate_b):
    ql, kl, vl, gl = mlstm_prep(lat[0], lat[1], lat[2], conv_w, gate_b)
    qc, kc, vc, gc = mlstm_prep(ctx[0], ctx[1], ctx[2], conv_w, gate_b)
    B, _, H, d = ql.shape
    st0 = (jnp.zeros((B, H, d, d), F32), jnp.zeros((B, H, d), F32), jnp.zeros((B, H), F32))

    def rev(a):
        return a[:, ::-1]

    hc_f, st_f = mlstm_scan(qc, kc, vc, gc[0], gc[1], st0)
    hl_f, _ = mlstm_scan(ql, kl, vl, gl[0], gl[1], st_f)
    hc_b, st_b = mlstm_scan(rev(qc), rev(kc), rev(vc), rev(gc[2]), rev(gc[3]), st0)
    hl_b, _ = mlstm_scan(rev(ql), rev(kl), rev(vl), rev(gl[2]), rev(gl[3]), st_b)
    return hl_f + rev(hl_b), hc_f + rev(hc_b)


def mla_project(cq, ckv, kr, q_norm, w_uq, kv_norm, w_ukv, angs):
    q = heads(rmsnorm(cq, q_norm) @ w_uq, GROUP_HEADS)
    kv = heads(rmsnorm(ckv, kv_norm) @ w_ukv, GROUP_HEADS)
    q_nope, q_rope = q[..., :MLA_NOPE], q[..., MLA_NOPE:]
    k_nope, v = kv[..., :MLA_NOPE], kv[..., MLA_NOPE:]
    k_rope = kr[:, :, None, :]
    if angs is not None:
        q_rope = rope_2d(q_rope, angs)
        k_rope = rope_2d(k_rope, angs)
    k_rope = jnp.broadcast_to(k_rope, k_nope.shape[:-1] + (MLA_ROPE,))
    return (jnp.concatenate([q_nope, q_rope], axis=-1), jnp.concatenate([k_nope, k_rope], axis=-1), v)


def block_dense_attention(q, k_all, v_all, scale):
    B, T, H, dq = q.shape
    nb = T // ATTN_BLOCK
    qb = jnp.swapaxes(q.reshape(B, nb, ATTN_BLOCK, H, dq), 0, 1)

    def one(qblk):
        s = jnp.einsum('bqhd,bkhd->bhqk', qblk, k_all).astype(F32) * scale
        p = jax.nn.softmax(s, axis=-1).astype(v_all.dtype)
        return jnp.einsum('bhqk,bkhd->bqhd', p, v_all)

    out = lax.map(one, qb)
    return jnp.swapaxes(out, 0, 1).reshape(B, T, -1)


def window_attention(q, k, v, kc, vc, sink):
    B, T, H, d = q.shape
    KVH = k.shape[2]
    G = H // KVH
    nb = T // ATTN_BLOCK
    span = ATTN_BLOCK + 2 * SWA_WINDOW
    n_ctx = kc.shape[1]
    scale = d ** -0.5
    padw = ((0, 0), (SWA_WINDOW, SWA_WINDOW), (0, 0), (0, 0))
    kp = jnp.pad(k, padw)
    vp = jnp.pad(v, padw)
    start = jnp.arange(nb) * ATTN_BLOCK
    idx = start[:, None] + jnp.arange(span)[None, :]
    kb = kp[:, idx]
    vb = vp[:, idx]
    key_pos = idx - SWA_WINDOW
    q_pos = start[:, None] + jnp.arange(ATTN_BLOCK)[None, :]
    mask = ((jnp.abs(q_pos[:, :, None] - key_pos[:, None, :]) <= SWA_WINDOW)
            & (key_pos >= 0)[:, None, :] & (key_pos < T)[:, None, :])
    qb = q.reshape(B, nb, ATTN_BLOCK, KVH, G, d)
    s_loc = jnp.einsum('bnqhgd,bnkhd->bnhgqk', qb, kb).astype(F32) * scale
    s_loc = jnp.where(mask[None, :, None, None], s_loc, -jnp.inf)
    s_ctx = jnp.einsum('bnqhgd,bkhd->bnhgqk', qb, kc).astype(F32) * scale
    s_sink = jnp.broadcast_to(sink.astype(F32).reshape(1, 1, KVH, G, 1, 1), s_loc.shape[:-1] + (1,))
    p = jax.nn.softmax(jnp.concatenate([s_loc, s_ctx, s_sink], axis=-1), axis=-1).astype(v.dtype)
    out = (jnp.einsum('bnhgqk,bnkhd->bnqhgd', p[..., :span], vb)
           + jnp.einsum('bnhgqk,bkhd->bnqhgd', p[..., span:span + n_ctx], vc))
    return out.reshape(B, T, H * d)


def peer_ffn(h, wq, sub_keys, u, v):
    N, D = h.shape
    hb = h.reshape(N // PEER_BLOCK, PEER_BLOCK, D)

    def block(xb):
        qry = (xb @ wq).reshape(PEER_BLOCK, PEER_HEADS, 2, PEER_DKEY)
        s = jnp.einsum('thpk,hpnk->thpn', qry, sub_keys).astype(F32)
        sv, si = lax.top_k(s, PEER_TOPK)
        cand_s = (sv[:, :, 0, :, None] + sv[:, :, 1, None, :]).reshape(PEER_BLOCK, PEER_HEADS, PEER_TOPK * PEER_TOPK)
        cand_i = (si[:, :, 0, :, None] * PEER_NKEYS + si[:, :, 1, None, :]).reshape(PEER_BLOCK, PEER_HEADS, PEER_TOPK * PEER_TOPK)
        fs, fpos = lax.top_k(cand_s, PEER_TOPK)
        eidx = jnp.take_along_axis(cand_i, fpos, axis=-1)
        gate = jax.nn.softmax(fs, axis=-1)
        act = jax.nn.gelu(jnp.einsum('td,thkd->thk', xb, u[eidx]).astype(F32), approximate=False)
        w = (gate * act).astype(xb.dtype)
        return jnp.einsum('thk,thkd->td', w, v[eidx])

    return lax.map(block, hb).reshape(N, D)


def hybrid_layer(x, xc, c, c_ctx, need_ctx, angs_mla, angs_swa,
                 norm1_g, norm2_g, w_ada, b_ada, w_in, na_rpb, ml_conv, ml_gate_b,
                 mla_q_norm, mla_w_uq, mla_kv_norm, mla_w_ukv, swa_sink, w_out,
                 peer_wq, peer_keys, peer_u, peer_v):
    B, T, D = x.shape
    H = GROUP_HEADS
    sh1, sc1, g1, sh2, sc2, g2 = jnp.split((jax.nn.silu(c) @ w_ada + b_ada)[:, None, :], 6, axis=-1)
    sh1c, sc1c, g1c, sh2c, sc2c, g2c = jnp.split(jax.nn.silu(c_ctx) @ w_ada + b_ada, 6, axis=-1)
    h = rmsnorm(x, norm1_g) * (1.0 + sc1) + sh1
    hc = rmsnorm(xc, norm1_g) * (1.0 + sc1c) + sh1c
    (na_q, na_k, na_v, ml_qk, ml_v, ml_o, ml_g,
     mla_cq, mla_ckv, mla_kr, sw_q, sw_k, sw_v) = split_cols(h @ w_in)
    (na_qc, na_kc, na_vc, ml_qkc, ml_vc, ml_oc, ml_gc,
     mla_cqc, mla_ckvc, mla_krc, sw_qc, sw_kc, sw_vc) = split_cols(hc @ w_in)
    attn_scale = HEAD_DIM ** -0.5
    mla_scale = (MLA_NOPE + MLA_ROPE) ** -0.5
    kc_a, vc_a = heads(na_kc, H), heads(na_vc, H)
    y_a = neighbourhood_attention(heads(na_q, H), heads(na_k, H), heads(na_v, H), kc_a, vc_a, na_rpb)
    h_lat, h_ctx = mlstm_mixer((ml_qk, ml_v, ml_g), (ml_qkc, ml_vc, ml_gc), ml_conv, ml_gate_b)
    y_b = h_lat.reshape(B, T, GROUP_WIDTH) * jax.nn.sigmoid(ml_o)
    q_m, k_m, v_m = mla_project(mla_cq, mla_ckv, mla_kr, mla_q_norm, mla_w_uq, mla_kv_norm, mla_w_ukv, angs_mla)
    qc_m, kc_m, vc_m = mla_project(mla_cqc, mla_ckvc, mla_krc, mla_q_norm, mla_w_uq, mla_kv_norm, mla_w_ukv, None)
    y_c = block_dense_attention(q_m, jnp.concatenate([kc_m, k_m], axis=1), jnp.concatenate([vc_m, v_m], axis=1), mla_scale)
    kc_d, vc_d = heads(sw_kc, SWA_KV_HEADS), heads(sw_vc, SWA_KV_HEADS)
    y_d = window_attention(rope_2d(heads(sw_q, H), angs_swa), rope_2d(heads(sw_k, SWA_KV_HEADS), angs_swa),
                           heads(sw_v, SWA_KV_HEADS), kc_d, vc_d, swa_sink)
    x = x + g1 * (jnp.concatenate([y_a, y_b, y_c, y_d], axis=-1) @ w_out)
    h2 = rmsnorm(x, norm2_g) * (1.0 + sc2) + sh2
    if need_ctx:
        Tc = xc.shape[1]
        y_ctx = jnp.concatenate([
            ctx_attn(heads(na_qc, H), kc_a, vc_a, attn_scale),
            h_ctx.reshape(B, Tc, GROUP_WIDTH) * jax.nn.sigmoid(ml_oc),
            ctx_attn(qc_m, kc_m, vc_m, mla_scale),
            ctx_attn(heads(sw_qc, H), kc_d, vc_d, attn_scale, swa_sink)], axis=-1)
        xc = xc + g1c * (y_ctx @ w_out)
        h2c = rmsnorm(xc, norm2_g) * (1.0 + sc2c) + sh2c
        f = peer_ffn(jnp.concatenate([h2.reshape(B * T, D), h2c.reshape(B * Tc, D)], axis=0),
                     peer_wq, peer_keys, peer_u, peer_v)
        x = x + g2 * f[:B * T].reshape(B, T, D)
        xc = xc + g2c * f[B * T:].reshape(B, Tc, D)
        return x, xc
    x = x + g2 * peer_ffn(h2.reshape(B * T, D), peer_wq, peer_keys, peer_u, peer_v).reshape(B, T, D)
    return x, None


def setup_inputs(seed: int = 0) -> dict:
    key = jax.random.key(seed)
    ks = iter(jax.random.split(key, 32))

    def nrm(shape, std):
        return jax.random.normal(next(ks), shape, F32) * std

    L, D, H = DEPTH, D_MODEL, GROUP_HEADS
    f_base = jnp.linspace(3.0, 6.0, H, dtype=F32)
    zero_h = jnp.zeros((H,), F32)
    gate_base = jnp.stack([zero_h, f_base, zero_h, f_base])
    return {
        'x': nrm((BATCH, SEQ, D), 1.0),
        'c': nrm((BATCH, D), 1.0),
        'ctx': nrm((BATCH, CTX_LEN, D), 1.0),
        'c_ctx': nrm((D,), 1.0),
        'norm1_g': 1.0 + nrm((L, D), 0.02),
        'norm2_g': 1.0 + nrm((L, D), 0.02),
        'w_ada': nrm((L, D, 6 * D), 0.5 * D ** -0.5),
        'b_ada': nrm((L, 6 * D), 0.02),
        'w_in': nrm((L, D, IN_WIDTH), D ** -0.5),
        'na_rpb': nrm((L, H, 2 * NA_ROWS - 1, 2 * NA_COLS - 1), 0.5),
        'ml_conv': nrm((L, ML_CONV, 2 * GROUP_WIDTH), ML_CONV ** -0.5),
        'ml_gate_b': (gate_base[None] + nrm((L, 4, H), 0.1)).reshape(L, 4 * H),
        'mla_q_norm': 1.0 + nrm((L, MLA_Q_RANK), 0.02),
        'mla_w_uq': nrm((L, MLA_Q_RANK, H * (MLA_NOPE + MLA_ROPE)), MLA_Q_RANK ** -0.5),
        'mla_kv_norm': 1.0 + nrm((L, MLA_KV_RANK), 0.02),
        'mla_w_ukv': nrm((L, MLA_KV_RANK, H * (MLA_NOPE + MLA_V)), MLA_KV_RANK ** -0.5),
        'swa_sink': nrm((L, H), 0.5),
        'w_out': nrm((L, MIX_WIDTH, D), MIX_WIDTH ** -0.5),
        'peer_wq': nrm((L, D, PEER_HEADS * 2 * PEER_DKEY), D ** -0.5),
        'peer_keys': nrm((L, PEER_HEADS, 2, PEER_NKEYS, PEER_DKEY), PEER_DKEY ** -0.5),
        'peer_u': nrm((L, PEER_EXPERTS, D), D ** -0.5),
        'peer_v': nrm((L, PEER_EXPERTS, D), 0.5),
        'final_norm_g': 1.0 + nrm((D,), 0.02),
    }


def reference(x, c, ctx, c_ctx, norm1_g, norm2_g, w_ada, b_ada, w_in, na_rpb, ml_conv, ml_gate_b,
              mla_q_norm, mla_w_uq, mla_kv_norm, mla_w_ukv, swa_sink, w_out,
              peer_wq, peer_keys, peer_u, peer_v, final_norm_g):
    T = x.shape[1]
    angs_mla = axial_angles(T, MLA_ROPE)
    angs_swa = axial_angles(T, HEAD_DIM)
    xc = ctx
    for l in range(DEPTH):
        x, xc = hybrid_layer(x, xc, c, c_ctx, l < DEPTH - 1, angs_mla, angs_swa,
                             norm1_g[l], norm2_g[l], w_ada[l], b_ada[l], w_in[l], na_rpb[l],
                             ml_conv[l], ml_gate_b[l], mla_q_norm[l], mla_w_uq[l], mla_kv_norm[l],
                             mla_w_ukv[l], swa_sink[l], w_out[l], peer_wq[l], peer_keys[l],
                             peer_u[l], peer_v[l])
    return rmsnorm(x, final_norm_g)
```

```python
import numpy as np
import concourse.bass as bass
import concourse.mybir as mybir
from concourse.bass_utils import run_bass_kernel_spmd

F32 = mybir.dt.float32
BF16 = mybir.dt.bfloat16
AF = mybir.ActivationFunctionType
ALU = mybir.AluOpType
AX = mybir.AxisListType

D = 1024
CTX = 256
GW = 64
NCORES = 8
EPS = 1e-6
IN_W = 2736
EPOCH = 30000
NDMA = 6


class View:
    def __init__(self, t, ap):
        self.t = t
        self.ap = ap

    def __getitem__(self, idx):
        return View(self.t, self.ap[idx])

    def bc(self, shape):
        return View(self.t, self.ap.broadcast_to(list(shape)))

    def rr(self, s, **kw):
        return View(self.t, self.ap.rearrange(s, **kw))


class Tl:
    def __init__(self, h, is_dram=False):
        self.h = h
        self.last_w = None
        self.readers = {}
        self.is_dram = is_dram

    def __getitem__(self, idx):
        ap = self.h.ap() if self.is_dram else self.h
        return View(self, ap[idx])


class PB:
    def __init__(self):
        self.nc = bass.Bass("TRN2", target_bir_lowering=False)
        nc = self.nc
        self.eng = {"pe": nc.tensor, "dve": nc.vector, "act": nc.scalar, "pool": nc.gpsimd, "sp": nc.sync}
        self.sems = {}
        self.cnt = {}
        self.seen = {e: {} for e in self.eng}
        self.epoch = {e: 0 for e in ("pe", "dve", "act", "pool")}
        self.dma_rr = {}
        self.dma_ep = {}
        self.n_inst = 0
        self.uid = 0

    def _sem(self, key):
        if key not in self.sems:
            self.sems[key] = self.nc.alloc_semaphore(name="s%d" % len(self.sems))
            self.cnt[key] = 0
        return self.sems[key]

    def name(self, n):
        self.uid += 1
        return "%s_%d" % (n, self.uid)

    def din(self, name, shape, dt=F32):
        return Tl(self.nc.dram_tensor(name, list(shape), dt, kind="ExternalInput"), True)

    def dout(self, name, shape, dt=F32):
        return Tl(self.nc.dram_tensor(name, list(shape), dt, kind="ExternalOutput"), True)

    def sb(self, name, shape, dt=F32):
        return Tl(self.nc.alloc_sbuf_tensor(self.name(name), list(shape), dt))

    def ps(self, name, shape, dt=F32):
        return Tl(self.nc.alloc_psum_tensor(self.name(name), list(shape), dt))

    def _wait(self, e, deps):
        for key, v in deps:
            if e == "pe" and key[0] == "pe":
                continue
            if self.seen[e].get(key, 0) >= v:
                continue
            self.eng[e].wait_ge(self.sems[key], v)
            self.seen[e][key] = v

    @staticmethod
    def _deps(reads, writes):
        deps = []
        for t in reads:
            if t.last_w is not None:
                deps.append(t.last_w)
        for t in writes:
            if t.last_w is not None:
                deps.append(t.last_w)
            deps.extend(t.readers.items())
        return deps

    @staticmethod
    def _mark(dep, reads, writes):
        key, v = dep
        for t in reads:
            if t.readers.get(key, 0) < v:
                t.readers[key] = v
        for t in writes:
            t.last_w = dep
            t.readers = {}

    def op(self, e, reads, writes, fn):
        reads = [v.t for v in reads if isinstance(v, View)]
        writes = [v.t for v in writes]
        self._wait(e, self._deps(reads, writes))
        key = (e, self.epoch[e])
        sem = self._sem(key)
        inst = fn(self.eng[e])
        inst.then_inc(sem, 1)
        self.cnt[key] += 1
        self.n_inst += 1
        self._mark((key, self.cnt[key]), reads, writes)
        if self.cnt[key] >= EPOCH:
            self.epoch[e] += 1

    def dma(self, out, in_, q="sp"):
        i = self.dma_rr.get(q, 0)
        self.dma_rr[q] = (i + 1) % NDMA
        ep = self.dma_ep.get((q, i), 0)
        key = ("d" + q, i, ep)
        sem = self._sem(key)
        if self.cnt[key] >= 60000:
            self.dma_ep[(q, i)] = ep + 1
            old = (key, self.cnt[key])
            key = ("d" + q, i, ep + 1)
            sem = self._sem(key)
            self._wait(q, [old])
        deps = self._deps([in_.t], [out.t])
        if self.cnt[key] > 0:
            deps.append((key, self.cnt[key]))
        self._wait(q, deps)
        self.eng[q].dma_start(out=out.ap, in_=in_.ap).then_inc(sem, 16)
        self.cnt[key] += 16
        self.n_inst += 1
        self._mark((key, self.cnt[key]), [in_.t], [out.t])

    def finish(self):
        deps = [(k, v) for k, v in self.cnt.items() if v > 0]
        self._wait("sp", deps)

    def mm(self, out, lhsT, rhs, start=True, stop=True, skip=False):
        self.op("pe", [lhsT, rhs], [out],
                lambda e: e.matmul(out.ap, lhsT=lhsT.ap, rhs=rhs.ap, start=start, stop=stop, skip_group_check=skip))

    def tr(self, out, in_, ident):
        self.op("pe", [in_, ident], [out], lambda e: e.transpose(out.ap, in_.ap, ident.ap))

    def act(self, out, in_, func, bias=None, scale=1.0, accum=None, e="act"):
        kw = {}
        if bias is not None:
            kw["bias"] = bias.ap if isinstance(bias, View) else bias
        kw["scale"] = scale.ap if isinstance(scale, View) else scale
        w = [out]
        if accum is not None:
            kw["accum_out"] = accum.ap
            w.append(accum)
        self.op(e, [in_, bias, scale], w,
                lambda g: g.activation(out=out.ap, in_=in_.ap, func=func, **kw))

    def tt(self, out, a, b, op, e="dve"):
        self.op(e, [a, b], [out], lambda g: g.tensor_tensor(out=out.ap, in0=a.ap, in1=b.ap, op=op))

    def ts(self, out, a, s1, op0, s2=None, op1=None, e="dve", accum=None):
        x1 = s1.ap if isinstance(s1, View) else s1
        x2 = s2.ap if isinstance(s2, View) else s2
        kw = {}
        if op1 is not None:
            kw["op1"] = op1
        w = [out]
        if accum is not None:
            kw["accum_out"] = accum.ap
            w.append(accum)
        self.op(e, [a, s1, s2], w,
                lambda g: g.tensor_scalar(out=out.ap, in0=a.ap, scalar1=x1, scalar2=x2, op0=op0, **kw))

    def stt(self, out, a, s, b, op0, op1, e="dve"):
        x = s.ap if isinstance(s, View) else s
        self.op(e, [a, s, b], [out],
                lambda g: g.scalar_tensor_tensor(out=out.ap, in0=a.ap, scalar=x, in1=b.ap, op0=op0, op1=op1))

    def cp(self, out, in_, e="dve"):
        if e == "act":
            self.act(out, in_, AF.Copy)
        else:
            self.op(e, [in_], [out], lambda g: g.tensor_copy(out=out.ap, in_=in_.ap))

    def memset(self, out, val, e="dve"):
        self.op(e, [], [out], lambda g: g.memset(out.ap, val))

    def vmax8(self, out, in_):
        self.op("dve", [in_], [out], lambda g: g.max(out=out.ap, in_=in_.ap))

    def match_replace(self, out, vals, in_, imm):
        self.op("dve", [vals, in_], [out],
                lambda g: g.match_replace(out=out.ap, in_to_replace=vals.ap, in_values=in_.ap, imm_value=imm))

    def reduce(self, out, in_, op, e="dve"):
        self.op(e, [in_], [out], lambda g: g.tensor_reduce(out=out.ap, in_=in_.ap, axis=AX.X, op=op))


P_NAQ, P_NAK, P_NAV, P_MLQK, P_MLV, P_MLO, P_MLG = 0, 256, 512, 768, 1280, 1536, 1792
P_CQ, P_CKV, P_KR, P_SWQ, P_SWK, P_SWV = 1808, 2064, 2192, 2224, 2480, 2608
P_RSW, P_RKR, P_END = 2736, 3120, 3152
O_MLAQ, O_MLAK, O_MLAV, O_SWQ, O_SWK, O_SWV, O_END = 1808, 2192, 2576, 2832, 3088, 3216, 3344


def rstd(pb, out, ms):
    pb.act(out, ms, AF.Ln, bias=pb.eps_t[:, 0:1])
    pb.act(out, out, AF.Exp, scale=-0.5)


def rot_half(pb, dst, src, nblk, bs, e="dve"):
    h = bs // 2
    d3 = dst.rr("p (b t h) -> p b t h", b=nblk, t=2, h=h)
    s3 = src.rr("p (b t h) -> p b t h", b=nblk, t=2, h=h)
    pb.ts(d3[:, :, 0, :], s3[:, :, 1, :], -1.0, ALU.mult, e=e)
    pb.cp(d3[:, :, 1, :], s3[:, :, 0, :], e=e)


def stage(pb):
    if not hasattr(pb, "stg"):
        pb.stg = [pb.sb("stg%d" % i, [128, 8, 256], F32) for i in range(2)]
        pb.stg_i = 0
    pb.stg_i += 1
    return pb.stg[pb.stg_i % 2]


def load_mod_bc(pb, cT, w_ada, b_ada, col0, ncols, g, ps_pool, tag, sl):
    out = pb.sb("mod" + tag, [128, ncols], F32)
    pb.act(sl[:, :, :], cT[:, :, g:g + 1].bc([128, 8, 128]), AF.Silu)
    pb.dma(out[:, :], View(b_ada, b_ada.h.ap()[col0:col0 + ncols].partition_broadcast(128)))
    for n0 in range(0, ncols, 256):
        wst = stage(pb)
        pb.dma(wst[:, :, :], View(w_ada, w_ada.h.ap()[:, col0 + n0:col0 + n0 + 256].rearrange("(kc k) n -> k kc n", k=128)))
        pt = ps_pool[(n0 // 256) % len(ps_pool)]
        for kc in range(8):
            pb.mm(pt[:, 0:256], sl[:, kc, :], wst[:, kc, :], start=(kc == 0), stop=(kc == 7))
        pb.tt(out[:, n0:n0 + 256], pt[:, 0:256], out[:, n0:n0 + 256], ALU.add)
    return out


def build_l1(NB):
    pb = PB()
    x = pb.din("x", [NB, 128, D])
    cT = pb.din("cT", [128, 8, 2])
    ident_d = pb.din("ident", [128, 128])
    g1 = pb.din("norm_g", [D])
    w_ada = pb.din("w_ada", [D, 6 * D])
    b_ada = pb.din("b_ada", [6 * D])
    w_in = pb.din("w_in", [D, IN_W])
    qn = pb.din("q_norm", [256])
    kvn = pb.din("kv_norm", [128])
    w_uq = pb.din("w_uq", [256, 384])
    w_ukv = pb.din("w_ukv", [128, 512])
    cs_sw = pb.din("cs_sw", [NB, 128, 2, 384])
    cs_r = pb.din("cs_r", [NB, 128, 2, 32])
    out = pb.dout("out", [NB, 128, O_END])

    psA = [pb.ps("psA%d" % i, [128, 512]) for i in range(4)]
    psT = [pb.ps("psT%d" % i, [128, 512]) for i in range(2)]
    ident = pb.sb("ident", [128, 128])
    pb.dma(ident[:, :], ident_d[:, :])
    pb.eps_t = pb.sb("eps", [128, 1])
    pb.memset(pb.eps_t[:, :], EPS)
    cTs = pb.sb("cTs", [128, 8, 2])
    pb.dma(cTs[:, :, :], cT[:, :, :])
    mods = []
    sl = pb.sb("sl", [128, 8, 128])
    gbc = pb.sb("gbc", [128, D])
    pb.dma(gbc[:, :], View(g1, g1.h.ap().partition_broadcast(128)))
    for g in range(2):
        m = load_mod_bc(pb, cTs, w_ada, b_ada, 0, 2048, g, psA, "g%d" % g, sl)
        gm = pb.sb("gm%d" % g, [128, D])
        pb.stt(gm[:, :], m[:, 1024:2048], 1.0, gbc[:, :], ALU.add, ALU.mult)
        mods.append((gm, m))
    W = pb.sb("W", [128, 8, P_END], BF16)
    for n0 in range(0, IN_W, 256):
        n1 = min(IN_W, n0 + 256)
        st = stage(pb)
        pb.dma(st[:, :, 0:n1 - n0], View(w_in, w_in.h.ap()[:, n0:n1].rearrange("(kc k) n -> k kc n", k=128)))
        pb.cp(W[:, :, n0:n1], st[:, :, 0:n1 - n0], e="pool")
    for kc in range(8):
        rot_half(pb, W[:, kc, P_RSW:P_RSW + 384], W[:, kc, P_SWQ:P_SWQ + 384], 12, 32)
        rot_half(pb, W[:, kc, P_RKR:P_RKR + 32], W[:, kc, P_KR:P_KR + 32], 2, 16)
    uq32 = pb.sb("uq32", [128, 2, 384])
    pb.dma(uq32[:, :, :], View(w_uq, w_uq.h.ap().rearrange("(kc k) n -> k kc n", k=128)))
    Wuq = pb.sb("Wuq", [128, 2, 512], BF16)
    pb.cp(Wuq[:, :, 0:384], uq32[:, :, :])
    for kc in range(2):
        for h in range(4):
            rot_half(pb, Wuq[:, kc, 384 + 32 * h:384 + 32 * h + 32], Wuq[:, kc, 96 * h + 64:96 * h + 96], 2, 16)
    ukv32 = pb.sb("ukv32", [128, 512])
    pb.dma(ukv32[:, :], w_ukv[:, :])
    Wukv = pb.sb("Wukv", [128, 512], BF16)
    pb.cp(Wukv[:, :], ukv32[:, :])
    qnb = pb.sb("qnb", [128, 256])
    pb.dma(qnb[:, :], View(qn, qn.h.ap().partition_broadcast(128)))
    kvnb = pb.sb("kvnb", [128, 128])
    pb.dma(kvnb[:, :], View(kvn, kvn.h.ap().partition_broadcast(128)))

    NBUF = 2
    xt = [pb.sb("xt%d" % i, [128, D]) for i in range(NBUF)]
    hT = [pb.sb("hT%d" % i, [128, 8, 128], BF16) for i in range(NBUF)]
    Pb = [pb.sb("Pb%d" % i, [128, P_END]) for i in range(NBUF)]
    Qb = [pb.sb("Qb%d" % i, [128, O_SWV - O_MLAQ]) for i in range(NBUF)]
    cst = [pb.sb("cst%d" % i, [128, 2, 384]) for i in range(NBUF)]
    crt = [pb.sb("crt%d" % i, [128, 2, 32]) for i in range(NBUF)]
    sm = [pb.sb("sm%d" % i, [128, 8]) for i in range(NBUF)]
    tmp = [pb.sb("tmp%d" % i, [128, D]) for i in range(NBUF)]
    cqT = [pb.sb("cqT%d" % i, [128, 3, 128], BF16) for i in range(NBUF)]
    for b in range(NB):
        i = b % NBUF
        g = 1 if b == NB - 1 else 0
        gm, m = mods[g]
        X, H, Pt, Q, S, T = xt[i], hT[i], Pb[i], Qb[i], sm[i], tmp[i]
        pb.dma(X[:, :], x[b, :, :])
        pb.dma(cst[i][:, :, :], cs_sw[b, :, :, :])
        pb.dma(crt[i][:, :, :], cs_r[b, :, :, :])
        pb.act(T[:, :], X[:, :], AF.Square, accum=S[:, 0:1], scale=float(D) ** -0.5)
        rstd(pb, S[:, 1:2], S[:, 0:1])
        pb.stt(T[:, :], X[:, :], S[:, 1:2], gm[:, :], ALU.mult, ALU.mult)
        pb.tt(T[:, :], T[:, :], m[:, 0:1024], ALU.add, e="pool")
        for half in range(2):
            pt = psT[half]
            for j in range(4):
                kc = half * 4 + j
                pb.tr(pt[:, 128 * j:128 * j + 128], T[:, 128 * kc:128 * kc + 128], ident[:, :])
            pb.cp(H[:, 4 * half:4 * half + 4, :], pt[:, :].rr("p (a b) -> p a b", a=4), e=("act" if half else "dve"))
        nt = 0
        for n0 in range(0, P_END, 512):
            n1 = min(P_END, n0 + 512)
            pt = psA[nt % 4]
            for kc in range(8):
                pb.mm(pt[:, 0:n1 - n0], H[:, kc, :], W[:, kc, n0:n1], start=(kc == 0), stop=(kc == 7))
            pb.cp(Pt[:, n0:n1], pt[:, 0:n1 - n0], e=("act" if nt % 2 else "dve"))
            nt += 1
        pb.dma(out[b, :, 0:O_MLAQ], Pt[:, 0:P_CQ])
        pb.dma(out[b, :, O_SWV:O_END], Pt[:, P_SWV:P_SWV + 128])
        qo = lambda a, n: Q[:, a - O_MLAQ:a - O_MLAQ + n]
        pb.tt(qo(O_SWQ, 384), Pt[:, P_SWQ:P_SWQ + 384], cst[i][:, 0, :], ALU.mult)
        pb.tt(T[:, 0:384], Pt[:, P_RSW:P_RSW + 384], cst[i][:, 1, :], ALU.mult, e="pool")
        pb.tt(qo(O_SWQ, 384), qo(O_SWQ, 384), T[:, 0:384], ALU.add)
        pb.tt(T[:, 400:432], Pt[:, P_KR:P_KR + 32], crt[i][:, 0, :], ALU.mult)
        pb.tt(T[:, 432:464], Pt[:, P_RKR:P_RKR + 32], crt[i][:, 1, :], ALU.mult)
        pb.tt(T[:, 400:432], T[:, 400:432], T[:, 432:464], ALU.add)
        pb.act(T[:, 512:768], Pt[:, P_CQ:P_CQ + 256], AF.Square, accum=S[:, 2:3], scale=1.0 / 16.0)
        rstd(pb, S[:, 3:4], S[:, 2:3])
        pb.stt(T[:, 512:768], Pt[:, P_CQ:P_CQ + 256], S[:, 3:4], qnb[:, :], ALU.mult, ALU.mult)
        pb.act(T[:, 768:896], Pt[:, P_CKV:P_CKV + 128], AF.Square, accum=S[:, 4:5], scale=128.0 ** -0.5)
        rstd(pb, S[:, 5:6], S[:, 4:5])
        pb.stt(T[:, 768:896], Pt[:, P_CKV:P_CKV + 128], S[:, 5:6], kvnb[:, :], ALU.mult, ALU.mult)
        pt = psT[0]
        for j in range(3):
            pb.tr(pt[:, 128 * j:128 * j + 128], T[:, 512 + 128 * j:512 + 128 * j + 128], ident[:, :])
        pb.cp(cqT[i][:, :, :], pt[:, 0:384].rr("p (a b) -> p a b", a=3))
        pq = psA[nt % 4]
        pb.mm(pq[:, :], cqT[i][:, 0, :], Wuq[:, 0, :], start=True, stop=False)
        pb.mm(pq[:, :], cqT[i][:, 1, :], Wuq[:, 1, :], start=False, stop=True)
        pk = psA[(nt + 1) % 4]
        pb.mm(pk[:, :], cqT[i][:, 2, :], Wukv[:, :], start=True, stop=True)
        q3 = qo(O_MLAQ, 384).rr("p (h d) -> p h d", h=4)
        pq3 = pq[:, 0:384].rr("p (h d) -> p h d", h=4)
        pqr = pq[:, 384:512].rr("p (h d) -> p h d", h=4)
        pb.cp(q3[:, :, 0:64], pq3[:, :, 0:64], e="act")
        cosb = crt[i][:, 0:1, :].bc([128, 4, 32])
        sinb = crt[i][:, 1:2, :].bc([128, 4, 32])
        t3 = T[:, 0:128].rr("p (h d) -> p h d", h=4)
        pb.tt(q3[:, :, 64:96], pq3[:, :, 64:96], cosb, ALU.mult)
        pb.tt(t3, pqr, sinb, ALU.mult)
        pb.tt(q3[:, :, 64:96], q3[:, :, 64:96], t3, ALU.add)
        k3 = qo(O_MLAK, 384).rr("p (h d) -> p h d", h=4)
        v3 = qo(O_MLAV, 256).rr("p (h d) -> p h d", h=4)
        pk3 = pk[:, :].rr("p (h d) -> p h d", h=4)
        pb.cp(k3[:, :, 0:64], pk3[:, :, 0:64], e="act")
        pb.cp(v3[:, :, :], pk3[:, :, 64:128], e="act")
        pb.cp(k3[:, :, 64:96], T[:, 400:432].rr("p (o d) -> p o d", o=1).bc([128, 4, 32]), e="pool")
        pb.dma(out[b, :, O_MLAQ:O_SWV], Q[:, :])
    pb.finish()
    return pb


_CACHE = {}


def _prog(key, fn):
    if key not in _CACHE:
        _CACHE[key] = fn()
    return _CACHE[key]


def rope_tables(T):
    t = np.arange(T)
    row = (t // GW).astype(np.float32)
    col = (t % GW).astype(np.float32)

    def tab(rot):
        half = rot // 2
        inv = (1.0 / (np.float32(10000.0) ** (np.arange(0, half, 2, dtype=np.float32) / np.float32(half)))).astype(np.float32)
        ar = (row[:, None] * inv).astype(np.float32)
        ac = (col[:, None] * inv).astype(np.float32)
        cos = np.concatenate([np.cos(ar), np.cos(ar), np.cos(ac), np.cos(ac)], -1)
        sin = np.concatenate([np.sin(ar), np.sin(ar), np.sin(ac), np.sin(ac)], -1)
        return np.stack([cos, sin], 1).astype(np.float32)

    return tab(64), tab(32)


def ident_tables(n):
    one = np.ones((n, 2, 1), np.float32)
    one[:, 1] = 0
    return one


def cT_layout(cvec2):
    return np.ascontiguousarray(cvec2.reshape(2, 8, 128).transpose(2, 1, 0))


def run_l1(x, xc, c, c_ctx, lp):
    B, T, _ = x.shape
    NBL = T // 4 // 128
    NB = NBL + 1
    pb = _prog(("l1", NB), lambda: build_l1(NB))
    cs64, cs32 = rope_tables(T)
    cs_sw_lat = np.tile(cs64, (1, 1, 6))
    in_maps = []
    for k in range(NCORES):
        b, qd = k // 4, k % 4
        t0 = qd * (T // 4)
        xb = np.concatenate([x[b, t0:t0 + T // 4].reshape(NBL, 128, D),
                             xc[b, (k % 2) * 128:(k % 2) * 128 + 128][None]], 0)
        csw = np.concatenate([cs_sw_lat[t0:t0 + T // 4].reshape(NBL, 128, 2, 384),
                              np.broadcast_to(ident_tables(128), (128, 2, 384))[None]], 0)
        csr = np.concatenate([cs32[t0:t0 + T // 4].reshape(NBL, 128, 2, 32),
                              np.broadcast_to(ident_tables(128), (128, 2, 32))[None]], 0)
        in_maps.append({
            "x": np.ascontiguousarray(xb, np.float32),
            "cT": cT_layout(np.stack([c[b], c_ctx])),
            "ident": np.eye(128, dtype=np.float32),
            "norm_g": lp["norm1_g"], "w_ada": lp["w_ada"], "b_ada": lp["b_ada"], "w_in": lp["w_in"],
            "q_norm": lp["mla_q_norm"], "kv_norm": lp["mla_kv_norm"], "w_uq": lp["mla_w_uq"], "w_ukv": lp["mla_w_ukv"],
            "cs_sw": np.ascontiguousarray(csw, np.float32), "cs_r": np.ascontiguousarray(csr, np.float32),
        })
    res = run_bass_kernel_spmd(pb.nc, in_maps, core_ids=list(range(NCORES))).results
    P_lat = np.empty((B, T, O_END), np.float32)
    P_ctx = np.empty((B, CTX, O_END), np.float32)
    for k in range(NCORES):
        b, qd = k // 4, k % 4
        o = res[k]["out"]
        P_lat[b, qd * (T // 4):(qd + 1) * (T // 4)] = o[:NBL].reshape(T // 4, O_END)
        if qd < 2:
            P_ctx[b, qd * 128:(qd + 1) * 128] = o[NBL]
    return P_lat, P_ctx


def build_attn(NQB, H, Hkv, dq, dv, NKB, sched, n_bias, n_mask, use_sink, scale):
    pb = PB()
    NQ, NK = NQB * 128, NKB * 128
    QT = pb.din("QT", [H, dq, NQ])
    KT = pb.din("KT", [Hkv, dq, NK])
    V = pb.din("V", [NKB, 128, Hkv, dv])
    bias = pb.din("bias", [H, max(n_bias, 1), 128, 128])
    mask = pb.din("mask", [max(n_mask, 1), 128, 128])
    sink = pb.din("sink", [128, H])
    Yd = pb.dout("Y", [NQB, 128, H * dv])

    psS = [pb.ps("psS%d" % i, [128, 512]) for i in range(3)]
    psO = [pb.ps("psO%d" % i, [128, 4, 128]) for i in range(2)]
    QTb = pb.sb("QTb", [dq, H, NQ], BF16)
    KTb = pb.sb("KTb", [dq, NK], BF16)
    Vb = pb.sb("Vb", [128, NKB, dv + 1], BF16)
    Y = pb.sb("Y", [128, NQB, H * dv])
    CH = 2048
    kst = [pb.sb("kst%d" % i, [dq, CH]) for i in range(2)]
    VC = 16
    vst = [pb.sb("vst%d" % i, [128, VC, dv]) for i in range(2)]
    Et = [pb.sb("Et%d" % i, [128, 512], BF16) for i in range(3)]
    tmpS = [pb.sb("tmpS%d" % i, [128, 128]) for i in range(2)]
    bt = [pb.sb("bt%d" % i, [128, 128]) for i in range(3)]
    mt = [pb.sb("mt%d" % i, [128, 128]) for i in range(3)]
    den = [pb.sb("den%d" % i, [128, 4, 2]) for i in range(2)]
    sk = pb.sb("sk", [128, H])
    pb.dma(sk[:, :], sink[:, :])
    pb.act(sk[:, :], sk[:, :], AF.Exp)
    pb.memset(Vb[:, :, dv:dv + 1], 1.0)
    n = 0
    for h in range(H):
        for c0 in range(0, NQ, CH):
            c1 = min(NQ, c0 + CH)
            st = kst[n % 2]
            n += 1
            pb.dma(st[:, 0:c1 - c0], QT[h, :, c0:c1])
            pb.cp(QTb[:, h, c0:c1], st[:, 0:c1 - c0], e="pool")
    cnt = 0
    for hkv in range(Hkv):
        for c0 in range(0, NK, CH):
            c1 = min(NK, c0 + CH)
            st = kst[n % 2]
            n += 1
            pb.dma(st[:, 0:c1 - c0], KT[hkv, :, c0:c1])
            pb.cp(KTb[:, c0:c1], st[:, 0:c1 - c0], e="pool")
        for b0 in range(0, NKB, VC):
            b1 = min(NKB, b0 + VC)
            st = vst[n % 2]
            n += 1
            pb.dma(st[:, 0:b1 - b0, :], V[b0:b1, :, hkv, :].rr("b p d -> p b d"))
            pb.cp(Vb[:, b0:b1, 0:dv], st[:, 0:b1 - b0, :], e="pool")
        for h in range(hkv * (H // Hkv), (hkv + 1) * (H // Hkv)):
            for (i0, g, klist) in sched:
                po = psO[cnt % 2]
                dn = den[cnt % 2]
                cnt += 1
                nk = len(klist)
                for ki, (j, bid, mid) in enumerate(klist):
                    ps = psS[ki % 3]
                    E = Et[ki % 3]
                    pb.mm(ps[:, 0:128 * g], KTb[:, 128 * j:128 * j + 128], QTb[:, h, 128 * i0:128 * (i0 + g)])
                    if bid is not None:
                        assert g == 1
                        B_ = bt[ki % 3]
                        pb.dma(B_[:, :], bias[h, bid, :, :])
                        tS = tmpS[ki % 2]
                        pb.stt(tS[:, :], ps[:, 0:128], scale, B_[:, :], ALU.mult, ALU.add)
                        pb.act(E[:, 0:128], tS[:, :], AF.Exp)
                    else:
                        pb.act(E[:, 0:128 * g], ps[:, 0:128 * g], AF.Exp, scale=scale)
                    if mid is not None:
                        assert g == 1
                        M_ = mt[ki % 3]
                        pb.dma(M_[:, :], mask[mid, :, :])
                        pb.tt(E[:, 0:128], E[:, 0:128], M_[:, :], ALU.mult, e="pool")
                    for s in range(g):
                        pb.mm(po[:, s, 0:dv + 1], E[:, 128 * s:128 * s + 128], Vb[:, j, :],
                              start=(ki == 0 and s == 0), stop=(ki == nk - 1), skip=(g > 1))
                if use_sink:
                    pb.ts(dn[:, 0:g, 0:1], po[:, 0:g, dv:dv + 1], sk[:, h:h + 1], ALU.add)
                    pb.op("dve", [dn[:, :, :]], [dn[:, :, :]],
                          lambda e, dn=dn, g=g: e.reciprocal(out=dn.h[:, 0:g, 1:2], in_=dn.h[:, 0:g, 0:1]))
                else:
                    pb.op("dve", [po[:, :, :]], [dn[:, :, :]],
                          lambda e, dn=dn, po=po, g=g: e.reciprocal(out=dn.h[:, 0:g, 1:2], in_=po.h[:, 0:g, dv:dv + 1]))
                pb.tt(Y[:, i0:i0 + g, h * dv:(h + 1) * dv], po[:, 0:g, 0:dv],
                      dn[:, 0:g, 1:2].bc([128, g, dv]), ALU.mult)
    for i in range(NQB):
        pb.dma(Yd[i, :, :], Y[:, i, :])
    pb.finish()
    return pb


def _cls(i, NBL, edge):
    ncls = min(NBL, 2 * edge + 1)
    if i < edge:
        return i, ncls
    if i >= NBL - edge:
        return ncls - (NBL - i), ncls
    return edge, ncls


def _na_tiles(m, delta, nblk, rpb):
    rows = nblk * 2
    mk = m + delta
    if mk < 0 or mk >= nblk:
        return np.zeros((4, 128, 128), np.float32), np.zeros((128, 128), np.float32)
    idx = np.arange(128)
    qr, qc = 2 * m + idx // 64, idx % 64
    kr, kc = 2 * mk + idx // 64, idx % 64
    rs = np.clip(qr - 4, 0, rows - 8)
    cs = np.clip(qc - 8, 0, 64 - 16)
    valid = ((kr[:, None] >= rs[None, :]) & (kr[:, None] < rs[None, :] + 8) &
             (kc[:, None] >= cs[None, :]) & (kc[:, None] < cs[None, :] + 16))
    dr = np.clip(kr[:, None] - qr[None, :] + 7, 0, 14)
    dc = np.clip(kc[:, None] - qc[None, :] + 15, 0, 30)
    return np.ascontiguousarray(rpb[:, dr, dc], np.float32), valid.astype(np.float32)


def _swa_mask(m, delta, nblk):
    mk = m + delta
    if mk < 0 or mk >= nblk:
        return np.zeros((128, 128), np.float32)
    k = np.arange(128)[:, None]
    q = np.arange(128)[None, :]
    if delta == -1:
        return (q <= k).astype(np.float32)
    if delta == 1:
        return (k <= q).astype(np.float32)
    return np.ones((128, 128), np.float32)


def _halo(a, lo, hi):
    n = a.shape[0]
    out = np.zeros((hi - lo,) + a.shape[1:], a.dtype)
    s, e = max(lo, 0), min(hi, n)
    if e > s:
        out[s - lo:e - lo] = a[s:e]
    return out


def run_attn(kind, P_lat, P_ctx, lp):
    B, T, _ = P_lat.shape
    NBL = T // 4 // 128
    NBLT = T // 128
    NQB = NBL + 1
    if kind == "na":
        H, Hkv, dq, dv, halo, scale = 4, 4, 64, 64, 3, 64 ** -0.5
        oq, ok, ov = P_NAQ, P_NAK, P_NAV
    elif kind == "swa":
        H, Hkv, dq, dv, halo, scale = 4, 2, 64, 64, 1, 64 ** -0.5
        oq, ok, ov = O_SWQ, O_SWK, O_SWV
    else:
        H, Hkv, dq, dv, halo, scale = 4, 4, 96, 64, None, 96 ** -0.5
        oq, ok, ov = O_MLAQ, O_MLAK, O_MLAV
    NKB = 2 + (NBLT if halo is None else NBL + 2 * halo)
    sched = []
    ctxk = [(0, None, None), (1, None, None)]
    if kind == "mla":
        allk = [(j, None, None) for j in range(NKB)]
        for i0 in range(0, NBL, 4):
            sched.append((i0, min(4, NBL - i0), allk))
        n_bias = n_mask = 0
    else:
        nd = 2 * halo + 1
        edge = 2 if kind == "na" else 1
        for i in range(NBL):
            c, ncls = _cls(i, NBL, edge)
            kl = list(ctxk)
            for d in range(-halo, halo + 1):
                tid = c * nd + d + halo
                kl.append((2 + i + d + halo, tid if kind == "na" else None, tid))
            sched.append((i, 1, kl))
        n_mask = ncls * nd
        n_bias = n_mask if kind == "na" else 0
    sched.append((NBL, 1, ctxk))
    key = ("attn", kind, NQB)
    pb = _prog(key, lambda: build_attn(NQB, H, Hkv, dq, dv, NKB, sched, n_bias, n_mask, kind == "swa", scale))
    in_maps = []
    for k in range(NCORES):
        b, qd = k // 4, k % 4
        t0, t1 = qd * NBL * 128, (qd + 1) * NBL * 128
        ch = (k % 2) * 128
        q = np.concatenate([P_lat[b, t0:t1, oq:oq + H * dq], P_ctx[b, ch:ch + 128, oq:oq + H * dq]], 0)
        QT = np.ascontiguousarray(q.reshape(NQB * 128, H, dq).transpose(1, 2, 0))
        kl = P_lat[b, :, ok:ok + Hkv * dq].reshape(NBLT, 128, Hkv, dq)
        vl = P_lat[b, :, ov:ov + Hkv * dv].reshape(NBLT, 128, Hkv, dv)
        if halo is not None:
            kl = _halo(kl, qd * NBL - halo, (qd + 1) * NBL + halo)
            vl = _halo(vl, qd * NBL - halo, (qd + 1) * NBL + halo)
        kk = np.concatenate([P_ctx[b, :, ok:ok + Hkv * dq].reshape(2, 128, Hkv, dq), kl], 0)
        vv = np.concatenate([P_ctx[b, :, ov:ov + Hkv * dv].reshape(2, 128, Hkv, dv), vl], 0)
        KT = np.ascontiguousarray(kk.reshape(NKB * 128, Hkv, dq).transpose(1, 2, 0))
        bias = np.zeros((H, max(n_bias, 1), 128, 128), np.float32)
        mask = np.zeros((max(n_mask, 1), 128, 128), np.float32)
        if kind != "mla":
            for i in range(NBL):
                c, _ = _cls(i, NBL, edge)
                for d in range(-halo, halo + 1):
                    tid = c * nd + d + halo
                    if kind == "na":
                        g_, m_ = _na_tiles(qd * NBL + i, d, NBLT, lp["na_rpb"])
                        bias[:, tid] = g_
                        mask[tid] = m_
                    else:
                        mask[tid] = _swa_mask(qd * NBL + i, d, NBLT)
        in_maps.append({"QT": QT, "KT": KT, "V": np.ascontiguousarray(vv), "bias": bias, "mask": mask,
                        "sink": np.ascontiguousarray(np.broadcast_to(lp["swa_sink"][None, :], (128, H)), np.float32)})
    res = run_bass_kernel_spmd(pb.nc, in_maps, core_ids=list(range(NCORES))).results
    y_lat = np.empty((B, T, H * dv), np.float32)
    y_ctx = np.empty((B, CTX, H * dv), np.float32)
    for k in range(NCORES):
        b, qd = k // 4, k % 4
        o = res[k]["Y"]
        y_lat[b, qd * NBL * 128:(qd + 1) * NBL * 128] = o[:NBL].reshape(NBL * 128, H * dv)
        if qd < 2:
            y_ctx[b, qd * 128:(qd + 1) * 128] = o[NBL]
    return y_lat, y_ctx


SC = 16


def build_mlstm(T):
    pb = PB()
    NCH = (CTX + T) // 64
    LP = CTX + T + 8
    groups = [(0, 4)] + [(4 + SC * i, SC) for i in range((T // 64) // SC)]
    raw = pb.din("raw", [2, 64, LP])
    convw = pb.din("convw", [2, 64, 5])
    vd = pb.din("v", [64, NCH, 64])
    od = pb.din("o", [64, NCH, 64])
    gd = pb.din("g", [64, NCH, 4])
    gbd = pb.din("gb", [64, 4])
    cd = pb.din("consts", [64, 6, 64])
    out = pb.dout("out", [64, NCH, 64])
    hf = Tl(pb.nc.dram_tensor("hf_scratch", [64, NCH, 64], F32), True)

    cs = pb.sb("cs", [64, 6, 64])
    pb.dma(cs[:, :, :], cd[:, :, :])
    cw = pb.sb("cw", [64, 2, 5])
    pb.dma(cw[:, :, :], convw[:, :, :].rr("a p j -> p a j"))
    gb = pb.sb("gb", [64, 4])
    pb.dma(gb[:, :], gbd[:, :])
    one = pb.sb("one", [64, 1])
    pb.memset(one[:, :], 1.0)
    psS = [pb.ps("psS%d" % i, [64, 64]) for i in range(2)]
    psO = [pb.ps("psO%d" % i, [64, 65]) for i in range(2)]
    psU = [pb.ps("psU%d" % i, [64, 65]) for i in range(2)]
    psT = pb.ps("psT", [64, 512])
    psG = pb.ps("psG", [64, 3, SC])
    W = 64 * SC
    rw = [pb.sb("rw%d" % i, [64, W + 4]) for i in range(2)]
    qk = [pb.sb("qk%d" % i, [64, W]) for i in range(2)]
    ktok = pb.sb("ktok", [64, SC, 64])
    vaug = pb.sb("vaug", [64, SC, 65])
    pb.memset(vaug[:, :, 64:65], 1.0)
    gt = pb.sb("gt", [64, SC, 4])
    gi = pb.sb("gi", [64, SC])
    lf = pb.sb("lf", [64, SC])
    ex = pb.sb("ex", [64, 4, SC])
    tg = pb.sb("tg", [64, 2, SC])
    PT = [pb.sb("PT%d" % i, [64, 64]) for i in range(2)]
    kw = [pb.sb("kw%d" % i, [64, 64]) for i in range(2)]
    Cst = [pb.sb("C%d" % i, [64, 65]) for i in range(2)]
    Ob = pb.sb("Ob", [64, SC, 65])
    Hb = pb.sb("Hb", [64, SC, 64])
    H2 = pb.sb("H2", [64, SC, 64])
    ot = pb.sb("ot", [64, SC, 64])
    cf = pb.sb("cf", [64, 2, SC])
    for d in range(2):
        t_in, t_ex = (0, 1) if d == 0 else (2, 3)
        order = groups if d == 0 else [groups[0]] + groups[:0:-1]
        ci = 0
        pb.memset(Cst[0][:, :], 0.0)
        for (c0, n) in order:
            w = 64 * n
            start = (2 + 64 * c0) if c0 < 4 else (CTX + 6 + 64 * (c0 - 4))
            for a in range(2):
                pb.dma(rw[a][:, 0:w + 4], raw[a, :, start - 2:start + w + 2])
                acc = qk[a]
                pb.ts(acc[:, 0:w], rw[a][:, 0:w], cw[:, a, 0:1], ALU.mult)
                for j in range(1, 5):
                    pb.stt(acc[:, 0:w], rw[a][:, j:j + w], cw[:, a, j:j + 1], acc[:, 0:w], ALU.mult, ALU.add)
                pb.act(acc[:, 0:w], acc[:, 0:w], AF.Silu)
            pb.ts(qk[0][:, 0:w], qk[0][:, 0:w], 0.125, ALU.mult)
            pb.dma(vaug[:, 0:n, 0:64], vd[:, c0:c0 + n, :])
            pb.dma(gt[:, 0:n, :], gd[:, c0:c0 + n, :])
            for c in range(n):
                pb.tr(psT[:, 64 * (c % 8):64 * (c % 8) + 64], qk[1][:, 64 * c:64 * c + 64], cs[:, 5, :])
                if c % 8 == 7 or c == n - 1:
                    b0 = c - (c % 8)
                    pb.cp(ktok[:, b0:c + 1, :], psT[:, 0:64 * (c - b0 + 1)].rr("p (a b) -> p a b", b=64), e="act")
            pb.ts(gi[:, 0:n], gt[:, 0:n, 2 * d], gb[:, 2 * d:2 * d + 1], ALU.add)
            pb.ts(lf[:, 0:n], gt[:, 0:n, 2 * d + 1], gb[:, 2 * d + 1:2 * d + 2], ALU.add)
            pb.act(lf[:, 0:n], lf[:, 0:n], AF.Exp, scale=-1.0)
            pb.act(lf[:, 0:n], lf[:, 0:n], AF.Ln, bias=one[:, 0:1])
            pb.ts(lf[:, 0:n], lf[:, 0:n], -1.0, ALU.mult)
            for r, sel in enumerate((t_in, t_ex, 4)):
                pb.mm(psG[:, r, 0:n], cs[:, sel, :], lf[:, 0:n])
            pb.act(ex[:, 0, 0:n], psG[:, 0, 0:n], AF.Exp)
            pb.tt(tg[:, 0, 0:n], gi[:, 0:n], psG[:, 0, 0:n], ALU.subtract)
            pb.act(ex[:, 1, 0:n], tg[:, 0, 0:n], AF.Exp)
            pb.tt(tg[:, 1, 0:n], gi[:, 0:n], psG[:, 1, 0:n], ALU.add)
            pb.act(ex[:, 2, 0:n], tg[:, 1, 0:n], AF.Exp)
            pb.act(ex[:, 3, 0:n], psG[:, 2, 0:n], AF.Exp)
            chunks = list(range(n)) if d == 0 else list(range(n - 1, -1, -1))
            for c in chunks:
                Cc, Cn = Cst[ci % 2], Cst[(ci + 1) % 2]
                pS, pO, pU = psS[ci % 2], psO[ci % 2], psU[ci % 2]
                P_, K_ = PT[ci % 2], kw[ci % 2]
                ci += 1
                qT = qk[0][:, 64 * c:64 * c + 64]
                kT = qk[1][:, 64 * c:64 * c + 64]
                pb.mm(pS[:, :], kT, qT)
                pb.stt(P_[:, :], pS[:, :], ex[:, 1, c:c + 1], cs[:, t_in, :], ALU.mult, ALU.mult)
                pb.ts(K_[:, :], ktok[:, c, :], ex[:, 2, c:c + 1], ALU.mult, e="pool")
                pb.mm(pO[:, :], P_[:, :], vaug[:, c, :], start=True, stop=False)
                pb.mm(pO[:, :], qT, Cc[:, :], start=False, stop=True)
                pb.cp(Ob[:, c, :], pO[:, :], e="act")
                pb.mm(pU[:, :], K_[:, :], vaug[:, c, :])
                pb.stt(Cn[:, :], Cc[:, :], ex[:, 3, c:c + 1], pU[:, :], ALU.mult, ALU.add)
            pb.tt(cf[:, 0, 0:n], Ob[:, 0:n, 64], ex[:, 0, 0:n], ALU.mult)
            pb.act(cf[:, 0, 0:n], cf[:, 0, 0:n], AF.Abs)
            pb.ts(cf[:, 0, 0:n], cf[:, 0, 0:n], 1.0, ALU.max)
            pb.op("dve", [cf[:, :, :]], [cf[:, :, :]],
                  lambda e, n=n: e.reciprocal(out=cf.h[:, 1, 0:n], in_=cf.h[:, 0, 0:n]))
            pb.tt(cf[:, 1, 0:n], cf[:, 1, 0:n], ex[:, 0, 0:n], ALU.mult)
            pb.tt(Hb[:, 0:n, :], Ob[:, 0:n, 0:64], cf[:, 1, 0:n].rr("p (n o) -> p n o", o=1).bc([64, n, 64]), ALU.mult)
            if d == 0:
                pb.dma(hf[:, c0:c0 + n, :], Hb[:, 0:n, :])
            else:
                pb.dma(H2[:, 0:n, :], hf[:, c0:c0 + n, :])
                pb.dma(ot[:, 0:n, :], od[:, c0:c0 + n, :])
                pb.act(ot[:, 0:n, :], ot[:, 0:n, :], AF.Sigmoid)
                pb.tt(H2[:, 0:n, :], H2[:, 0:n, :], Hb[:, 0:n, :], ALU.add)
                pb.tt(H2[:, 0:n, :], H2[:, 0:n, :], ot[:, 0:n, :], ALU.mult, e="pool")
                pb.dma(out[:, c0:c0 + n, :], H2[:, 0:n, :])
    pb.finish()
    return pb


def run_mlstm(P_lat, P_ctx, lp):
    B, T, _ = P_lat.shape
    NCH = (CTX + T) // 64
    pb = _prog(("mlstm", T), lambda: build_mlstm(T))
    u = np.arange(64)[:, None]
    t = np.arange(64)[None, :]
    consts = np.stack([(u <= t), (u > t), (u >= t), (u < t), np.ones((64, 64), bool), (u == t)], 1).astype(np.float32)
    in_maps = []
    for k in range(NCORES):
        b, h = k // 4, k % 4
        seq = np.concatenate([P_ctx[b], P_lat[b]], 0)
        raw = np.zeros((2, 64, CTX + T + 8), np.float32)
        for a in range(2):
            col = P_MLQK + 256 * a + 64 * h
            raw[a, :, 2:2 + CTX] = seq[:CTX, col:col + 64].T
            raw[a, :, CTX + 6:CTX + 6 + T] = seq[CTX:, col:col + 64].T
        cw = np.stack([lp["ml_conv"][:, 256 * a + 64 * h:256 * a + 64 * h + 64].T for a in range(2)], 0)
        chunk = lambda a: np.ascontiguousarray(a.reshape(NCH, 64, -1).transpose(1, 0, 2))
        gcols = [P_MLG + 4 * j + h for j in range(4)]
        in_maps.append({
            "raw": raw, "convw": np.ascontiguousarray(cw, np.float32),
            "v": chunk(seq[:, P_MLV + 64 * h:P_MLV + 64 * h + 64]),
            "o": chunk(seq[:, P_MLO + 64 * h:P_MLO + 64 * h + 64]),
            "g": chunk(seq[:, gcols]),
            "gb": np.ascontiguousarray(np.broadcast_to(lp["ml_gate_b"][[h, 4 + h, 8 + h, 12 + h]][None, :], (64, 4)), np.float32),
            "consts": consts,
        })
    res = run_bass_kernel_spmd(pb.nc, in_maps, core_ids=list(range(NCORES))).results
    y_lat = np.empty((B, T, 256), np.float32)
    y_ctx = np.empty((B, CTX, 256), np.float32)
    for k in range(NCORES):
        b, h = k // 4, k % 4
        o = res[k]["out"].transpose(1, 0, 2).reshape(CTX + T, 64)
        y_ctx[b, :, 64 * h:64 * h + 64] = o[:CTX]
        y_lat[b, :, 64 * h:64 * h + 64] = o[CTX:]
    return y_lat, y_ctx


def build_out(NB, ctx_last):
    pb = PB()
    x = pb.din("x", [NB, 128, D])
    yc = pb.din("ycat", [NB, 128, D])
    cT = pb.din("cT", [128, 8, 2])
    ident_d = pb.din("ident", [128, 128])
    g2n = pb.din("norm_g", [D])
    w_ada = pb.din("w_ada", [D, 6 * D])
    b_ada = pb.din("b_ada", [6 * D])
    w_out = pb.din("w_out", [D, D])
    wq_d = pb.din("wq", [D, 2048])
    keysT = pb.din("keysT", [128, 16, 128])
    x1o = pb.dout("x1", [NB, 128, D])
    h2o = pb.dout("h2", [NB, 128, D])
    shpo = pb.dout("shp", [NB, 128, 16, 128])
    paro = pb.dout("par", [NB, 128, 8, 2])

    psA = [pb.ps("psA%d" % i, [128, 512]) for i in range(4)]
    psT = [pb.ps("psT%d" % i, [128, 512]) for i in range(2)]
    psK = [pb.ps("psK%d" % i, [128, 512]) for i in range(2)]
    ident = pb.sb("ident", [128, 128])
    pb.dma(ident[:, :], ident_d[:, :])
    pb.eps_t = pb.sb("eps", [128, 1])
    pb.memset(pb.eps_t[:, :], EPS)
    cTs = pb.sb("cTs", [128, 8, 2])
    pb.dma(cTs[:, :, :], cT[:, :, :])
    QR = pb.sb("QR", [128, 2048])
    QT = pb.sb("QT", [128, 16, 128])
    svs = [QR[:, :].rr("p (a b) -> p a b", a=8), QT[:, :, :].rr("p (a c) b -> p a (c b)", a=8)]
    sl = pb.sb("sl", [128, 8, 128])
    gbc = pb.sb("gbc", [128, D])
    pb.dma(gbc[:, :], View(g2n, g2n.h.ap().partition_broadcast(128)))
    mods = []
    for g in range(2 if ctx_last else 1):
        m = pb.sb("modo%d" % g, [128, 4096])
        pb.act(sl[:, :, :], cTs[:, :, g:g + 1].bc([128, 8, 128]), AF.Silu)
        pb.dma(m[:, :], View(b_ada, b_ada.h.ap()[2048:6144].partition_broadcast(128)))
        for n0 in range(0, 4096, 256):
            sv = svs[(n0 // 256) % 2]
            pb.dma(sv, View(w_ada, w_ada.h.ap()[:, 2048 + n0:2048 + n0 + 256].rearrange("(kc k) n -> k kc n", k=128)))
            pt = psA[(n0 // 256) % 4]
            for kc in range(8):
                pb.mm(pt[:, 0:256], sl[:, kc, :], sv[:, kc, :], start=(kc == 0), stop=(kc == 7))
            pb.tt(m[:, n0:n0 + 256], pt[:, 0:256], m[:, n0:n0 + 256], ALU.add)
        pb.stt(m[:, 2048:3072], m[:, 2048:3072], 1.0, gbc[:, :], ALU.add, ALU.mult)
        mods.append(m)
    Wo = pb.sb("Wo", [128, 8, D], BF16)
    for kc in range(8):
        pb.dma(QR[:, 0:1024], w_out[128 * kc:128 * kc + 128, :])
        pb.cp(Wo[:, kc, :], QR[:, 0:1024])
    wq = pb.sb("wq", [128, 8, 2048])
    for kc in range(8):
        pb.dma(wq[:, kc, :], wq_d[128 * kc:128 * kc + 128, :])
    kT = pb.sb("kT", [128, 16, 128])
    pb.dma(kT[:, :, :], keysT[:, :, :])

    X = pb.sb("X", [128, D])
    Yc = pb.sb("Yc", [128, D])
    YT = pb.sb("YT", [128, 8, 128], BF16)
    X1 = pb.sb("X1", [128, D])
    T = pb.sb("T", [128, D])
    H2 = pb.sb("H2", [128, D])
    H2T = pb.sb("H2T", [128, 8, 128])
    SHP = pb.sb("SHP", [128, 16, 128])
    T16 = pb.sb("T16", [128, 16, 16])
    CAND = pb.sb("CAND", [128, 8, 256])
    F16 = pb.sb("F16", [128, 8, 16])
    tS = pb.sb("tS", [128, 256])
    S = pb.sb("S", [128, 8])
    Z = pb.sb("Z", [128, 8, 2])
    PAR = pb.sb("PAR", [128, 8, 2])
    tF = pb.sb("tF", [128, 8, 16])
    for b in range(NB):
        m = mods[1 if (ctx_last and b == NB - 1) else 0]
        pb.dma(X[:, :], x[b, :, :])
        pb.dma(Yc[:, :], yc[b, :, :])
        for half in range(2):
            for j in range(4):
                kc = half * 4 + j
                pb.tr(psT[half][:, 128 * j:128 * j + 128], Yc[:, 128 * kc:128 * kc + 128], ident[:, :])
            pb.cp(YT[:, 4 * half:4 * half + 4, :], psT[half][:, :].rr("p (a b) -> p a b", a=4), e=("act" if half else "dve"))
        for nt in range(2):
            for kc in range(8):
                pb.mm(psA[nt][:, :], YT[:, kc, :], Wo[:, kc, 512 * nt:512 * nt + 512], start=(kc == 0), stop=(kc == 7))
            pb.tt(T[:, 512 * nt:512 * nt + 512], psA[nt][:, :], m[:, 512 * nt:512 * nt + 512], ALU.mult)
        pb.tt(X1[:, :], X[:, :], T[:, :], ALU.add, e="pool")
        pb.dma(x1o[b, :, :], X1[:, :])
        pb.act(T[:, :], X1[:, :], AF.Square, accum=S[:, 0:1], scale=float(D) ** -0.5)
        rstd(pb, S[:, 1:2], S[:, 0:1])
        pb.stt(H2[:, :], X1[:, :], S[:, 1:2], m[:, 2048:3072], ALU.mult, ALU.mult)
        pb.tt(H2[:, :], H2[:, :], m[:, 1024:2048], ALU.add, e="pool")
        pb.dma(h2o[b, :, :], H2[:, :])
        for half in range(2):
            for j in range(4):
                kc = half * 4 + j
                pb.tr(psT[half][:, 128 * j:128 * j + 128], H2[:, 128 * kc:128 * kc + 128], ident[:, :])
            pb.cp(H2T[:, 4 * half:4 * half + 4, :], psT[half][:, :].rr("p (a b) -> p a b", a=4), e=("act" if half else "dve"))
        for nt in range(4):
            pt = psA[2 + nt % 2]
            for kc in range(8):
                pb.mm(pt[:, :], H2T[:, kc, :], wq[:, kc, 512 * nt:512 * nt + 512], start=(kc == 0), stop=(kc == 7))
            pb.cp(QR[:, 512 * nt:512 * nt + 512], pt[:, :], e=("act" if nt % 2 else "dve"))
        for q4 in range(4):
            pt = psT[q4 % 2]
            for j in range(4):
                pb.tr(pt[:, 128 * j:128 * j + 128], QR[:, 128 * (4 * q4 + j):128 * (4 * q4 + j) + 128], ident[:, :])
            pb.cp(QT[:, 4 * q4:4 * q4 + 4, :], pt[:, :].rr("p (a b) -> p a b", a=4), e=("act" if q4 % 2 else "dve"))
        for q4 in range(4):
            pt = psK[q4 % 2]
            for j in range(4):
                pb.mm(pt[:, 128 * j:128 * j + 128], QT[:, 4 * q4 + j, :], kT[:, 4 * q4 + j, :], start=True, stop=True)
            pb.cp(SHP[:, 4 * q4:4 * q4 + 4, :], pt[:, :].rr("p (a b) -> p a b", a=4), e=("act" if q4 % 2 else "dve"))
        pb.dma(shpo[b, :, :, :], SHP[:, :, :])
        for j in range(16):
            pb.vmax8(T16[:, j, 0:8], SHP[:, j, :])
            pb.match_replace(tS[:, 0:128], T16[:, j, 0:8], SHP[:, j, :], -1e30)
            pb.vmax8(T16[:, j, 8:16], tS[:, 0:128])
        A = T16[:, :, :].rr("p (h two) r -> p h two r", two=2)
        pb.tt(CAND[:, :, :].rr("p h (a b) -> p h a b", a=16),
              A[:, :, 0, :].rr("p h (a o) -> p h a o", o=1).bc([128, 8, 16, 16]),
              A[:, :, 1, :].rr("p h (o b) -> p h o b", o=1).bc([128, 8, 16, 16]), ALU.add)
        for h in range(8):
            pb.vmax8(F16[:, h, 0:8], CAND[:, h, :])
            pb.match_replace(tS[:, :], F16[:, h, 0:8], CAND[:, h, :], -1e30)
            pb.vmax8(F16[:, h, 8:16], tS[:, :])
        pb.tt(tF[:, :, :], F16[:, :, :], F16[:, :, 0:1].bc([128, 8, 16]), ALU.subtract)
        pb.act(tF[:, :, :], tF[:, :, :], AF.Exp)
        pb.reduce(Z[:, :, 0], tF[:, :, :], ALU.add)
        pb.act(Z[:, :, 1], Z[:, :, 0], AF.Ln)
        pb.cp(PAR[:, :, 0], F16[:, :, 15], e="pool")
        pb.tt(PAR[:, :, 1], F16[:, :, 0], Z[:, :, 1], ALU.add)
        pb.ts(PAR[:, :, 1], PAR[:, :, 1], -1.0, ALU.mult)
        pb.dma(paro[b, :, :, :], PAR[:, :, :])
    pb.finish()
    return pb


IC = 16


def build_peer(NB, ctx_last, final):
    pb = PB()
    h2T = pb.din("h2T", [NB, 128, 8, 128])
    shp = pb.din("shp", [NB, 128, 16, 128])
    par = pb.din("par", [NB, 128, 8, 2])
    x1 = pb.din("x1", [NB, 128, D])
    cT = pb.din("cT", [128, 8, 2])
    ident_d = pb.din("ident", [128, 128])
    w_ada = pb.din("w_ada", [D, 6 * D])
    b_ada = pb.din("b_ada", [6 * D])
    uT = pb.din("uT", [128, 8, 16384])
    vd = pb.din("v", [16384, D])
    fg = pb.din("final_g", [D])
    out = pb.dout("out", [NB, 128, D])

    psO = [pb.ps("psO%d" % i, [128, 512]) for i in range(4)]
    psA = [pb.ps("psA%d" % i, [128, 256]) for i in range(2)]
    psW = [pb.ps("psW%d" % i, [128, 256]) for i in range(2)]
    psM = psO[0]
    ident = pb.sb("ident", [128, 128])
    pb.dma(ident[:, :], ident_d[:, :])
    pb.eps_t = pb.sb("eps", [128, 1])
    pb.memset(pb.eps_t[:, :], EPS)
    cTs = pb.sb("cTs", [128, 8, 2])
    pb.dma(cTs[:, :, :], cT[:, :, :])
    sl = pb.sb("sl", [128, 8, 128])
    fgb = pb.sb("fgb", [128, D])
    pb.dma(fgb[:, :], View(fg, fg.h.ap().partition_broadcast(128)))
    stg = [pb.sb("stg%d" % i, [128, 8, 256]) for i in range(2)]
    mods = []
    for g in range(2 if ctx_last else 1):
        m = pb.sb("modp%d" % g, [128, D])
        pb.act(sl[:, :, :], cTs[:, :, g:g + 1].bc([128, 8, 128]), AF.Silu)
        pb.dma(m[:, :], View(b_ada, b_ada.h.ap()[5120:6144].partition_broadcast(128)))
        for n0 in range(0, D, 256):
            sv = stg[(n0 // 256) % 2]
            pb.dma(sv[:, :, :], View(w_ada, w_ada.h.ap()[:, 5120 + n0:5120 + n0 + 256].rearrange("(kc k) n -> k kc n", k=128)))
            for kc in range(8):
                pb.mm(psM[:, 0:256], sl[:, kc, :], sv[:, kc, :], start=(kc == 0), stop=(kc == 7))
            pb.tt(m[:, n0:n0 + 256], psM[:, 0:256], m[:, n0:n0 + 256], ALU.add)
        mods.append(m)

    ub = Tl(pb.nc.dram_tensor("ub_scratch", [128, 128, 1024], BF16), True)
    vb = Tl(pb.nc.dram_tensor("vb_scratch", [128, 128, 1024], BF16), True)
    c32 = [pb.sb("c32_%d" % i, [128, 1024]) for i in range(4)]
    c16 = [pb.sb("c16_%d" % i, [128, 1024], BF16) for i in range(4)]
    for i in range(128):
        a, b_ = c32[(2 * i) % 4], c32[(2 * i + 1) % 4]
        a16, b16 = c16[(2 * i) % 4], c16[(2 * i + 1) % 4]
        pb.dma(a[:, :].rr("p (k e) -> p k e", k=8), uT[:, :, 128 * i:128 * i + 128])
        pb.dma(b_[:, :], vd[128 * i:128 * i + 128, :])
        pb.cp(a16[:, :], a[:, :], e="act")
        pb.cp(b16[:, :], b_[:, :], e="dve")
        pb.dma(ub[i, :, :], a16[:, :])
        pb.dma(vb[i, :, :], b16[:, :])
    HT32 = pb.sb("HT32", [128, 8, 128])
    HT = pb.sb("HT", [128, 8, 256], BF16)
    SH = pb.sb("SH", [128, 2, 16, 128])
    PR = pb.sb("PR", [128, 2, 8, 2])
    X1 = pb.sb("X1", [128, 2, D])
    Sb = [pb.sb("Sb%d" % i, [128, IC, 128]) for i in range(2)]
    Eb = [pb.sb("Eb%d" % i, [128, IC, 128]) for i in range(2)]
    Wh = [pb.sb("Wh%d" % i, [128, IC, 128]) for i in range(2)]
    Ws = [[pb.sb("Ws%d_%d" % (k, i), [128, IC, 128]) for i in range(2)] for k in range(2)]
    ut = [pb.sb("ut%d" % i, [128, 8, 128], BF16) for i in range(3)]
    vt = [pb.sb("vt%d" % i, [128, D], BF16) for i in range(3)]
    Gs = [pb.sb("Gs%d" % i, [128, 256]) for i in range(2)]
    GT = [pb.sb("GT%d" % i, [128, 256], BF16) for i in range(2)]
    O = pb.sb("O", [128, D])
    T = pb.sb("T", [128, D])
    S = pb.sb("S", [128, 4])
    tiles = [(b0, min(2, NB - b0)) for b0 in range(0, NB, 2)]
    cnt = 0
    wcnt = 0
    for (b0, ts) in tiles:
        tw = 128 * ts
        for k in range(ts):
            pb.dma(HT32[:, :, :], h2T[b0 + k, :, :, :])
            pb.cp(HT[:, :, 128 * k:128 * k + 128], HT32[:, :, :], e="act")
            pb.dma(SH[:, k, :, :], shp[b0 + k, :, :, :])
            pb.dma(PR[:, k, :, :], par[b0 + k, :, :, :])
            pb.dma(X1[:, k, :], x1[b0 + k, :, :])
        for ic in range(128 // IC):
            Wsum = Ws[ic % 2]
            for k in range(ts):
                for h in range(8):
                    S_, E_, W_ = Sb[wcnt % 2], Eb[wcnt % 2], Wh[wcnt % 2]
                    wcnt += 1
                    pb.tt(S_[:, :, :],
                          SH[:, k, 2 * h, IC * ic:IC * ic + IC].rr("p (a o) -> p a o", o=1).bc([128, IC, 128]),
                          SH[:, k, 2 * h + 1, :].rr("p (o b) -> p o b", o=1).bc([128, IC, 128]), ALU.add)
                    pb.act(E_[:, :, :], S_[:, :, :], AF.Exp, bias=PR[:, k, h, 1:2])
                    dst = Wsum[k] if h == 0 else W_
                    pb.stt(dst[:, :, :], S_[:, :, :], PR[:, k, h, 0:1], E_[:, :, :], ALU.is_ge, ALU.mult)
                    if h > 0:
                        pb.tt(Wsum[k][:, :, :], Wsum[k][:, :, :], W_[:, :, :], ALU.add, e="pool")
            for ii in range(IC):
                i = ic * IC + ii
                U_, V_ = ut[cnt % 3], vt[cnt % 3]
                pA, pW = psA[cnt % 2], psW[cnt % 2]
                G1, G2 = Gs[cnt % 2], GT[cnt % 2]
                cnt += 1
                pb.dma(U_[:, :, :].rr("p k e -> p (k e)"), ub[i, :, :])
                pb.dma(V_[:, :], vb[i, :, :])
                for k in range(ts):
                    pb.tr(pW[:, 128 * k:128 * k + 128], Wsum[k][:, ii, :], ident[:, :])
                for kc in range(8):
                    pb.mm(pA[:, 0:tw], U_[:, kc, :], HT[:, kc, 0:tw], start=(kc == 0), stop=(kc == 7))
                pb.act(G1[:, 0:tw], pA[:, 0:tw], AF.Gelu)
                pb.tt(G2[:, 0:tw], G1[:, 0:tw], pW[:, 0:tw], ALU.mult)
                for k in range(ts):
                    for nt in range(2):
                        pb.mm(psO[2 * k + nt][:, :], G2[:, 128 * k:128 * k + 128], V_[:, 512 * nt:512 * nt + 512],
                              start=(i == 0), stop=(i == 127))
        for k in range(ts):
            b = b0 + k
            m = mods[1 if (ctx_last and b == NB - 1) else 0]
            for nt in range(2):
                pb.tt(T[:, 512 * nt:512 * nt + 512], psO[2 * k + nt][:, :], m[:, 512 * nt:512 * nt + 512], ALU.mult)
            pb.tt(O[:, :], X1[:, k, :], T[:, :], ALU.add, e="pool")
            if final:
                pb.act(T[:, :], O[:, :], AF.Square, accum=S[:, 0:1], scale=float(D) ** -0.5)
                rstd(pb, S[:, 1:2], S[:, 0:1])
                pb.stt(O[:, :], O[:, :], S[:, 1:2], fgb[:, :], ALU.mult, ALU.mult)
            pb.dma(out[b, :, :], O[:, :])
    pb.finish()
    return pb


def run_out_peer(x, xc, ycat_lat, ycat_ctx, c, c_ctx, lp, need_ctx, final, final_g):
    B, T, _ = x.shape
    NBL = T // 4 // 128
    NB = NBL + (1 if need_ctx else 0)
    pbo = _prog(("out", NB, need_ctx), lambda: build_out(NB, need_ctx))
    pbp = _prog(("peer", NB, need_ctx, final), lambda: build_peer(NB, need_ctx, final))
    eye = np.eye(128, dtype=np.float32)
    keysT = np.ascontiguousarray(lp["peer_keys"].reshape(16, 128, 128).transpose(2, 0, 1))
    uT = np.ascontiguousarray(lp["peer_u"].reshape(16384, 8, 128).transpose(2, 1, 0))

    def blocks(lat, ctx, k):
        b, qd = k // 4, k % 4
        a = lat[b, qd * NBL * 128:(qd + 1) * NBL * 128].reshape(NBL, 128, -1)
        if need_ctx:
            a = np.concatenate([a, ctx[b, (k % 2) * 128:(k % 2) * 128 + 128][None]], 0)
        return np.ascontiguousarray(a, np.float32)

    in_maps = []
    for k in range(NCORES):
        b = k // 4
        in_maps.append({"x": blocks(x, xc, k), "ycat": blocks(ycat_lat, ycat_ctx, k),
                        "cT": cT_layout(np.stack([c[b], c_ctx])), "ident": eye, "norm_g": lp["norm2_g"],
                        "w_ada": lp["w_ada"], "b_ada": lp["b_ada"], "w_out": lp["w_out"], "wq": lp["peer_wq"],
                        "keysT": keysT})
    r1 = run_bass_kernel_spmd(pbo.nc, in_maps, core_ids=list(range(NCORES))).results
    in_maps = []
    for k in range(NCORES):
        b = k // 4
        h2 = r1[k]["h2"]
        h2T = np.ascontiguousarray(h2.reshape(NB, 128, 8, 128).transpose(0, 3, 2, 1))
        in_maps.append({"h2T": h2T, "shp": r1[k]["shp"], "par": r1[k]["par"], "x1": r1[k]["x1"],
                        "cT": cT_layout(np.stack([c[b], c_ctx])), "ident": eye,
                        "w_ada": lp["w_ada"], "b_ada": lp["b_ada"], "uT": uT, "v": lp["peer_v"],
                        "final_g": final_g})
    r2 = run_bass_kernel_spmd(pbp.nc, in_maps, core_ids=list(range(NCORES))).results
    xn = np.empty_like(x)
    xcn = np.empty_like(xc) if need_ctx else None
    for k in range(NCORES):
        b, qd = k // 4, k % 4
        o = r2[k]["out"]
        xn[b, qd * NBL * 128:(qd + 1) * NBL * 128] = o[:NBL].reshape(NBL * 128, D)
        if need_ctx and qd < 2:
            xcn[b, qd * 128:(qd + 1) * 128] = o[NBL]
    return xn, xcn


def _tick(msg, t0=[None]):
    import time
    now = time.time()
    if t0[0] is not None:
        print("[kernel] %s %.1fs" % (msg, now - t0[0]), flush=True)
    t0[0] = now


def run_layer(x, xc, c, c_ctx, lp, need_ctx, final, final_g):
    _tick("start")
    P_lat, P_ctx = run_l1(x, xc, c, c_ctx, lp)
    _tick("l1")
    ya, yac = run_attn("na", P_lat, P_ctx, lp)
    _tick("na")
    yb, ybc = run_mlstm(P_lat, P_ctx, lp)
    _tick("mlstm")
    ym, ymc = run_attn("mla", P_lat, P_ctx, lp)
    _tick("mla")
    yd, ydc = run_attn("swa", P_lat, P_ctx, lp)
    _tick("swa")
    ycat = np.concatenate([ya, yb, ym, yd], -1)
    ycatc = np.concatenate([yac, ybc, ymc, ydc], -1)
    return run_out_peer(x, xc, ycat, ycatc, c, c_ctx, lp, need_ctx, final, final_g)


def kernel(x, c, ctx, c_ctx, norm1_g, norm2_g, w_ada, b_ada, w_in, na_rpb, ml_conv, ml_gate_b,
           mla_q_norm, mla_w_uq, mla_kv_norm, mla_w_ukv, swa_sink, w_out,
           peer_wq, peer_keys, peer_u, peer_v, final_norm_g):
    f = lambda a: np.ascontiguousarray(np.asarray(a), np.float32)
    P = dict(norm1_g=norm1_g, norm2_g=norm2_g, w_ada=w_ada, b_ada=b_ada, w_in=w_in, na_rpb=na_rpb,
             ml_conv=ml_conv, ml_gate_b=ml_gate_b, mla_q_norm=mla_q_norm, mla_w_uq=mla_w_uq,
             mla_kv_norm=mla_kv_norm, mla_w_ukv=mla_w_ukv, swa_sink=swa_sink, w_out=w_out,
             peer_wq=peer_wq, peer_keys=peer_keys, peer_u=peer_u, peer_v=peer_v)
    P = {k: f(v) for k, v in P.items()}
    xx, xc = f(x), f(ctx)
    cc, cctx, fg = f(c), f(c_ctx), f(final_norm_g)
    L = P["w_in"].shape[0]
    for l in range(L):
        lp = {k: v[l] for k, v in P.items()}
        xx, xc = run_layer(xx, xc, cc, cctx, lp, l < L - 1, l == L - 1, fg)
    return xx
```

```python
import numpy as np
import concourse.bass as bass
import concourse.mybir as mybir
from concourse.bass_utils import run_bass_kernel_spmd

F32 = mybir.dt.float32
BF16 = mybir.dt.bfloat16
AF = mybir.ActivationFunctionType
ALU = mybir.AluOpType
AX = mybir.AxisListType

D = 1024
CTX = 256
GW = 64
NCORES = 8
EPS = 1e-6
IN_W = 2736
EPOCH = 30000
NDMA = 6


class View:
    def __init__(self, t, ap):
        self.t = t
        self.ap = ap

    def __getitem__(self, idx):
        return View(self.t, self.ap[idx])

    def bc(self, shape):
        return View(self.t, self.ap.broadcast_to(list(shape)))

    def rr(self, s, **kw):
        return View(self.t, self.ap.rearrange(s, **kw))


class Tl:
    def __init__(self, h, is_dram=False):
        self.h = h
        self.last_w = None
        self.readers = {}
        self.is_dram = is_dram

    def __getitem__(self, idx):
        ap = self.h.ap() if self.is_dram else self.h
        return View(self, ap[idx])


class PB:
    def __init__(self):
        self.nc = bass.Bass("TRN2", target_bir_lowering=False)
        nc = self.nc
        self.eng = {"pe": nc.tensor, "dve": nc.vector, "act": nc.scalar, "pool": nc.gpsimd, "sp": nc.sync}
        self.sems = {}
        self.cnt = {}
        self.seen = {e: {} for e in self.eng}
        self.epoch = {e: 0 for e in ("pe", "dve", "act", "pool")}
        self.dma_rr = {}
        self.dma_ep = {}
        self.n_inst = 0
        self.uid = 0

    def _sem(self, key):
        if key not in self.sems:
            self.sems[key] = self.nc.alloc_semaphore(name="s%d" % len(self.sems))
            self.cnt[key] = 0
        return self.sems[key]

    def name(self, n):
        self.uid += 1
        return "%s_%d" % (n, self.uid)

    def din(self, name, shape, dt=F32):
        return Tl(self.nc.dram_tensor(name, list(shape), dt, kind="ExternalInput"), True)

    def dout(self, name, shape, dt=F32):
        return Tl(self.nc.dram_tensor(name, list(shape), dt, kind="ExternalOutput"), True)

    def sb(self, name, shape, dt=F32):
        return Tl(self.nc.alloc_sbuf_tensor(self.name(name), list(shape), dt))

    def ps(self, name, shape, dt=F32):
        return Tl(self.nc.alloc_psum_tensor(self.name(name), list(shape), dt))

    def _wait(self, e, deps):
        for key, v in deps:
            if e == "pe" and key[0] == "pe":
                continue
            if self.seen[e].get(key, 0) >= v:
                continue
            self.eng[e].wait_ge(self.sems[key], v)
            self.seen[e][key] = v

    @staticmethod
    def _deps(reads, writes):
        deps = []
        for t in reads:
            if t.last_w is not None:
                deps.append(t.last_w)
        for t in writes:
            if t.last_w is not None:
                deps.append(t.last_w)
            deps.extend(t.readers.items())
        return deps

    @staticmethod
    def _mark(dep, reads, writes):
        key, v = dep
        for t in reads:
            if t.readers.get(key, 0) < v:
                t.readers[key] = v
        for t in writes:
            t.last_w = dep
            t.readers = {}

    def op(self, e, reads, writes, fn):
        reads = [v.t for v in reads if isinstance(v, View)]
        writes = [v.t for v in writes]
        self._wait(e, self._deps(reads, writes))
        key = (e, self.epoch[e])
        sem = self._sem(key)
        inst = fn(self.eng[e])
        inst.then_inc(sem, 1)
        self.cnt[key] += 1
        self.n_inst += 1
        self._mark((key, self.cnt[key]), reads, writes)
        if self.cnt[key] >= EPOCH:
            self.epoch[e] += 1

    def dma(self, out, in_, q="sp"):
        i = self.dma_rr.get(q, 0)
        self.dma_rr[q] = (i + 1) % NDMA
        ep = self.dma_ep.get((q, i), 0)
        key = ("d" + q, i, ep)
        sem = self._sem(key)
        if self.cnt[key] >= 60000:
            self.dma_ep[(q, i)] = ep + 1
            old = (key, self.cnt[key])
            key = ("d" + q, i, ep + 1)
            sem = self._sem(key)
            self._wait(q, [old])
        deps = self._deps([in_.t], [out.t])
        if self.cnt[key] > 0:
            deps.append((key, self.cnt[key]))
        self._wait(q, deps)
        self.eng[q].dma_start(out=out.ap, in_=in_.ap).then_inc(sem, 16)
        self.cnt[key] += 16
        self.n_inst += 1
        self._mark((key, self.cnt[key]), [in_.t], [out.t])

    def finish(self):
        deps = [(k, v) for k, v in self.cnt.items() if v > 0]
        self._wait("sp", deps)

    def mm(self, out, lhsT, rhs, start=True, stop=True, skip=False):
        self.op("pe", [lhsT, rhs], [out],
                lambda e: e.matmul(out.ap, lhsT=lhsT.ap, rhs=rhs.ap, start=start, stop=stop, skip_group_check=skip))

    def tr(self, out, in_, ident):
        self.op("pe", [in_, ident], [out], lambda e: e.transpose(out.ap, in_.ap, ident.ap))

    def act(self, out, in_, func, bias=None, scale=1.0, accum=None, e="act"):
        kw = {}
        if bias is not None:
            kw["bias"] = bias.ap if isinstance(bias, View) else bias
        kw["scale"] = scale.ap if isinstance(scale, View) else scale
        w = [out]
        if accum is not None:
            kw["accum_out"] = accum.ap
            w.append(accum)
        self.op(e, [in_, bias, scale], w,
                lambda g: g.activation(out=out.ap, in_=in_.ap, func=func, **kw))

    def tt(self, out, a, b, op, e="dve"):
        self.op(e, [a, b], [out], lambda g: g.tensor_tensor(out=out.ap, in0=a.ap, in1=b.ap, op=op))

    def ts(self, out, a, s1, op0, s2=None, op1=None, e="dve", accum=None):
        x1 = s1.ap if isinstance(s1, View) else s1
        x2 = s2.ap if isinstance(s2, View) else s2
        kw = {}
        if op1 is not None:
            kw["op1"] = op1
        w = [out]
        if accum is not None:
            kw["accum_out"] = accum.ap
            w.append(accum)
        self.op(e, [a, s1, s2], w,
                lambda g: g.tensor_scalar(out=out.ap, in0=a.ap, scalar1=x1, scalar2=x2, op0=op0, **kw))

    def stt(self, out, a, s, b, op0, op1, e="dve"):
        x = s.ap if isinstance(s, View) else s
        self.op(e, [a, s, b], [out],
                lambda g: g.scalar_tensor_tensor(out=out.ap, in0=a.ap, scalar=x, in1=b.ap, op0=op0, op1=op1))

    def cp(self, out, in_, e="dve"):
        if e == "act":
            self.act(out, in_, AF.Copy)
        else:
            self.op(e, [in_], [out], lambda g: g.tensor_copy(out=out.ap, in_=in_.ap))

    def memset(self, out, val, e="dve"):
        self.op(e, [], [out], lambda g: g.memset(out.ap, val))

    def vmax8(self, out, in_):
        self.op("dve", [in_], [out], lambda g: g.max(out=out.ap, in_=in_.ap))

    def match_replace(self, out, vals, in_, imm):
        self.op("dve", [vals, in_], [out],
                lambda g: g.match_replace(out=out.ap, in_to_replace=vals.ap, in_values=in_.ap, imm_value=imm))

    def reduce(self, out, in_, op, e="dve"):
        self.op(e, [in_], [out], lambda g: g.tensor_reduce(out=out.ap, in_=in_.ap, axis=AX.X, op=op))


P_NAQ, P_NAK, P_NAV, P_MLQK, P_MLV, P_MLO, P_MLG = 0, 256, 512, 768, 1280, 1536, 1792
P_CQ, P_CKV, P_KR, P_SWQ, P_SWK, P_SWV = 1808, 2064, 2192, 2224, 2480, 2608
P_RSW, P_RKR, P_END = 2736, 3120, 3152
O_MLAQ, O_MLAK, O_MLAV, O_SWQ, O_SWK, O_SWV, O_END = 1808, 2192, 2576, 2832, 3088, 3216, 3344


def rstd(pb, out, ms):
    pb.act(out, ms, AF.Ln, bias=pb.eps_t[:, 0:1])
    pb.act(out, out, AF.Exp, scale=-0.5)


def rot_half(pb, dst, src, nblk, bs, e="dve"):
    h = bs // 2
    d3 = dst.rr("p (b t h) -> p b t h", b=nblk, t=2, h=h)
    s3 = src.rr("p (b t h) -> p b t h", b=nblk, t=2, h=h)
    pb.ts(d3[:, :, 0, :], s3[:, :, 1, :], -1.0, ALU.mult, e=e)
    pb.cp(d3[:, :, 1, :], s3[:, :, 0, :], e=e)


def stage(pb):
    if not hasattr(pb, "stg"):
        pb.stg = [pb.sb("stg%d" % i, [128, 8, 256], F32) for i in range(2)]
        pb.stg_i = 0
    pb.stg_i += 1
    return pb.stg[pb.stg_i % 2]


def load_mod_bc(pb, cT, w_ada, b_ada, col0, ncols, g, ps_pool, tag, sl):
    out = pb.sb("mod" + tag, [128, ncols], F32)
    pb.act(sl[:, :, :], cT[:, :, g:g + 1].bc([128, 8, 128]), AF.Silu)
    pb.dma(out[:, :], View(b_ada, b_ada.h.ap()[col0:col0 + ncols].partition_broadcast(128)))
    for n0 in range(0, ncols, 256):
        wst = stage(pb)
        pb.dma(wst[:, :, :], View(w_ada, w_ada.h.ap()[:, col0 + n0:col0 + n0 + 256].rearrange("(kc k) n -> k kc n", k=128)))
        pt = ps_pool[(n0 // 256) % len(ps_pool)]
        for kc in range(8):
            pb.mm(pt[:, 0:256], sl[:, kc, :], wst[:, kc, :], start=(kc == 0), stop=(kc == 7))
        pb.tt(out[:, n0:n0 + 256], pt[:, 0:256], out[:, n0:n0 + 256], ALU.add)
    return out


def build_l1(NB):
    pb = PB()
    x = pb.din("x", [NB, 128, D])
    cT = pb.din("cT", [128, 8, 2])
    ident_d = pb.din("ident", [128, 128])
    g1 = pb.din("norm_g", [D])
    w_ada = pb.din("w_ada", [D, 6 * D])
    b_ada = pb.din("b_ada", [6 * D])
    w_in = pb.din("w_in", [D, IN_W])
    qn = pb.din("q_norm", [256])
    kvn = pb.din("kv_norm", [128])
    w_uq = pb.din("w_uq", [256, 384])
    w_ukv = pb.din("w_ukv", [128, 512])
    cs_sw = pb.din("cs_sw", [NB, 128, 2, 384])
    cs_r = pb.din("cs_r", [NB, 128, 2, 32])
    out = pb.dout("out", [NB, 128, O_END])

    psA = [pb.ps("psA%d" % i, [128, 512]) for i in range(4)]
    psT = [pb.ps("psT%d" % i, [128, 512]) for i in range(2)]
    ident = pb.sb("ident", [128, 128])
    pb.dma(ident[:, :], ident_d[:, :])
    pb.eps_t = pb.sb("eps", [128, 1])
    pb.memset(pb.eps_t[:, :], EPS)
    cTs = pb.sb("cTs", [128, 8, 2])
    pb.dma(cTs[:, :, :], cT[:, :, :])
    mods = []
    sl = pb.sb("sl", [128, 8, 128])
    gbc = pb.sb("gbc", [128, D])
    pb.dma(gbc[:, :], View(g1, g1.h.ap().partition_broadcast(128)))
    for g in range(2):
        m = load_mod_bc(pb, cTs, w_ada, b_ada, 0, 2048, g, psA, "g%d" % g, sl)
        gm = pb.sb("gm%d" % g, [128, D])
        pb.stt(gm[:, :], m[:, 1024:2048], 1.0, gbc[:, :], ALU.add, ALU.mult)
        mods.append((gm, m))
    W = pb.sb("W", [128, 8, P_END], BF16)
    for n0 in range(0, IN_W, 256):
        n1 = min(IN_W, n0 + 256)
        st = stage(pb)
        pb.dma(st[:, :, 0:n1 - n0], View(w_in, w_in.h.ap()[:, n0:n1].rearrange("(kc k) n -> k kc n", k=128)))
        pb.cp(W[:, :, n0:n1], st[:, :, 0:n1 - n0], e="pool")
    for kc in range(8):
        rot_half(pb, W[:, kc, P_RSW:P_RSW + 384], W[:, kc, P_SWQ:P_SWQ + 384], 12, 32)
        rot_half(pb, W[:, kc, P_RKR:P_RKR + 32], W[:, kc, P_KR:P_KR + 32], 2, 16)
    uq32 = pb.sb("uq32", [128, 2, 384])
    pb.dma(uq32[:, :, :], View(w_uq, w_uq.h.ap().rearrange("(kc k) n -> k kc n", k=128)))
    Wuq = pb.sb("Wuq", [128, 2, 512], BF16)
    pb.cp(Wuq[:, :, 0:384], uq32[:, :, :])
    for kc in range(2):
        for h in range(4):
            rot_half(pb, Wuq[:, kc, 384 + 32 * h:384 + 32 * h + 32], Wuq[:, kc, 96 * h + 64:96 * h + 96], 2, 16)
    ukv32 = pb.sb("ukv32", [128, 512])
    pb.dma(ukv32[:, :], w_ukv[:, :])
    Wukv = pb.sb("Wukv", [128, 512], BF16)
    pb.cp(Wukv[:, :], ukv32[:, :])
    qnb = pb.sb("qnb", [128, 256])
    pb.dma(qnb[:, :], View(qn, qn.h.ap().partition_broadcast(128)))
    kvnb = pb.sb("kvnb", [128, 128])
    pb.dma(kvnb[:, :], View(kvn, kvn.h.ap().partition_broadcast(128)))

    NBUF = 2
    xt = [pb.sb("xt%d" % i, [128, D]) for i in range(NBUF)]
    hT = [pb.sb("hT%d" % i, [128, 8, 128], BF16) for i in range(NBUF)]
    Pb = [pb.sb("Pb%d" % i, [128, P_END]) for i in range(NBUF)]
    Qb = [pb.sb("Qb%d" % i, [128, O_SWV - O_MLAQ]) for i in range(NBUF)]
    cst = [pb.sb("cst%d" % i, [128, 2, 384]) for i in range(NBUF)]
    crt = [pb.sb("crt%d" % i, [128, 2, 32]) for i in range(NBUF)]
    sm = [pb.sb("sm%d" % i, [128, 8]) for i in range(NBUF)]
    tmp = [pb.sb("tmp%d" % i, [128, D]) for i in range(NBUF)]
    cqT = [pb.sb("cqT%d" % i, [128, 3, 128], BF16) for i in range(NBUF)]
    for b in range(NB):
        i = b % NBUF
        g = 1 if b == NB - 1 else 0
        gm, m = mods[g]
        X, H, Pt, Q, S, T = xt[i], hT[i], Pb[i], Qb[i], sm[i], tmp[i]
        pb.dma(X[:, :], x[b, :, :])
        pb.dma(cst[i][:, :, :], cs_sw[b, :, :, :])
        pb.dma(crt[i][:, :, :], cs_r[b, :, :, :])
        pb.act(T[:, :], X[:, :], AF.Square, accum=S[:, 0:1], scale=float(D) ** -0.5)
        rstd(pb, S[:, 1:2], S[:, 0:1])
        pb.stt(T[:, :], X[:, :], S[:, 1:2], gm[:, :], ALU.mult, ALU.mult)
        pb.tt(T[:, :], T[:, :], m[:, 0:1024], ALU.add, e="pool")
        for half in range(2):
            pt = psT[half]
            for j in range(4):
                kc = half * 4 + j
                pb.tr(pt[:, 128 * j:128 * j + 128], T[:, 128 * kc:128 * kc + 128], ident[:, :])
            pb.cp(H[:, 4 * half:4 * half + 4, :], pt[:, :].rr("p (a b) -> p a b", a=4), e=("act" if half else "dve"))
        nt = 0
        for n0 in range(0, P_END, 512):
            n1 = min(P_END, n0 + 512)
            pt = psA[nt % 4]
            for kc in range(8):
                pb.mm(pt[:, 0:n1 - n0], H[:, kc, :], W[:, kc, n0:n1], start=(kc == 0), stop=(kc == 7))
            pb.cp(Pt[:, n0:n1], pt[:, 0:n1 - n0], e=("act" if nt % 2 else "dve"))
            nt += 1
        pb.dma(out[b, :, 0:O_MLAQ], Pt[:, 0:P_CQ])
        pb.dma(out[b, :, O_SWV:O_END], Pt[:, P_SWV:P_SWV + 128])
        qo = lambda a, n: Q[:, a - O_MLAQ:a - O_MLAQ + n]
        pb.tt(qo(O_SWQ, 384), Pt[:, P_SWQ:P_SWQ + 384], cst[i][:, 0, :], ALU.mult)
        pb.tt(T[:, 0:384], Pt[:, P_RSW:P_RSW + 384], cst[i][:, 1, :], ALU.mult, e="pool")
        pb.tt(qo(O_SWQ, 384), qo(O_SWQ, 384), T[:, 0:384], ALU.add)
        pb.tt(T[:, 400:432], Pt[:, P_KR:P_KR + 32], crt[i][:, 0, :], ALU.mult)
        pb.tt(T[:, 432:464], Pt[:, P_RKR:P_RKR + 32], crt[i][:, 1, :], ALU.mult)
        pb.tt(T[:, 400:432], T[:, 400:432], T[:, 432:464], ALU.add)
        pb.act(T[:, 512:768], Pt[:, P_CQ:P_CQ + 256], AF.Square, accum=S[:, 2:3], scale=1.0 / 16.0)
        rstd(pb, S[:, 3:4], S[:, 2:3])
        pb.stt(T[:, 512:768], Pt[:, P_CQ:P_CQ + 256], S[:, 3:4], qnb[:, :], ALU.mult, ALU.mult)
        pb.act(T[:, 768:896], Pt[:, P_CKV:P_CKV + 128], AF.Square, accum=S[:, 4:5], scale=128.0 ** -0.5)
        rstd(pb, S[:, 5:6], S[:, 4:5])
        pb.stt(T[:, 768:896], Pt[:, P_CKV:P_CKV + 128], S[:, 5:6], kvnb[:, :], ALU.mult, ALU.mult)
        pt = psT[0]
        for j in range(3):
            pb.tr(pt[:, 128 * j:128 * j + 128], T[:, 512 + 128 * j:512 + 128 * j + 128], ident[:, :])
        pb.cp(cqT[i][:, :, :], pt[:, 0:384].rr("p (a b) -> p a b", a=3))
        pq = psA[nt % 4]
        pb.mm(pq[:, :], cqT[i][:, 0, :], Wuq[:, 0, :], start=True, stop=False)
        pb.mm(pq[:, :], cqT[i][:, 1, :], Wuq[:, 1, :], start=False, stop=True)
        pk = psA[(nt + 1) % 4]
        pb.mm(pk[:, :], cqT[i][:, 2, :], Wukv[:, :], start=True, stop=True)
        q3 = qo(O_MLAQ, 384).rr("p (h d) -> p h d", h=4)
        pq3 = pq[:, 0:384].rr("p (h d) -> p h d", h=4)
        pqr = pq[:, 384:512].rr("p (h d) -> p h d", h=4)
        pb.cp(q3[:, :, 0:64], pq3[:, :, 0:64], e="act")
        cosb = crt[i][:, 0:1, :].bc([128, 4, 32])
        sinb = crt[i][:, 1:2, :].bc([128, 4, 32])
        t3 = T[:, 0:128].rr("p (h d) -> p h d", h=4)
        pb.tt(q3[:, :, 64:96], pq3[:, :, 64:96], cosb, ALU.mult)
        pb.tt(t3, pqr, sinb, ALU.mult)
        pb.tt(q3[:, :, 64:96], q3[:, :, 64:96], t3, ALU.add)
        k3 = qo(O_MLAK, 384).rr("p (h d) -> p h d", h=4)
        v3 = qo(O_MLAV, 256).rr("p (h d) -> p h d", h=4)
        pk3 = pk[:, :].rr("p (h d) -> p h d", h=4)
        pb.cp(k3[:, :, 0:64], pk3[:, :, 0:64], e="act")
        pb.cp(v3[:, :, :], pk3[:, :, 64:128], e="act")
        pb.cp(k3[:, :, 64:96], T[:, 400:432].rr("p (o d) -> p o d", o=1).bc([128, 4, 32]), e="pool")
        pb.dma(out[b, :, O_MLAQ:O_SWV], Q[:, :])
    pb.finish()
    return pb


_CACHE = {}


def _prog(key, fn):
    if key not in _CACHE:
        _CACHE[key] = fn()
    return _CACHE[key]


def rope_tables(T):
    t = np.arange(T)
    row = (t // GW).astype(np.float32)
    col = (t % GW).astype(np.float32)

    def tab(rot):
        half = rot // 2
        inv = (1.0 / (np.float32(10000.0) ** (np.arange(0, half, 2, dtype=np.float32) / np.float32(half)))).astype(np.float32)
        ar = (row[:, None] * inv).astype(np.float32)
        ac = (col[:, None] * inv).astype(np.float32)
        cos = np.concatenate([np.cos(ar), np.cos(ar), np.cos(ac), np.cos(ac)], -1)
        sin = np.concatenate([np.sin(ar), np.sin(ar), np.sin(ac), np.sin(ac)], -1)
        return np.stack([cos, sin], 1).astype(np.float32)

    return tab(64), tab(32)


def ident_tables(n):
    one = np.ones((n, 2, 1), np.float32)
    one[:, 1] = 0
    return one


def cT_layout(cvec2):
    return np.ascontiguousarray(cvec2.reshape(2, 8, 128).transpose(2, 1, 0))


def run_l1(x, xc, c, c_ctx, lp):
    B, T, _ = x.shape
    NBL = T // 4 // 128
    NB = NBL + 1
    pb = _prog(("l1", NB), lambda: build_l1(NB))
    cs64, cs32 = rope_tables(T)
    cs_sw_lat = np.tile(cs64, (1, 1, 6))
    in_maps = []
    for k in range(NCORES):
        b, qd = k // 4, k % 4
        t0 = qd * (T // 4)
        xb = np.concatenate([x[b, t0:t0 + T // 4].reshape(NBL, 128, D),
                             xc[b, (k % 2) * 128:(k % 2) * 128 + 128][None]], 0)
        csw = np.concatenate([cs_sw_lat[t0:t0 + T // 4].reshape(NBL, 128, 2, 384),
                              np.broadcast_to(ident_tables(128), (128, 2, 384))[None]], 0)
        csr = np.concatenate([cs32[t0:t0 + T // 4].reshape(NBL, 128, 2, 32),
                              np.broadcast_to(ident_tables(128), (128, 2, 32))[None]], 0)
        in_maps.append({
            "x": np.ascontiguousarray(xb, np.float32),
            "cT": cT_layout(np.stack([c[b], c_ctx])),
            "ident": np.eye(128, dtype=np.float32),
            "norm_g": lp["norm1_g"], "w_ada": lp["w_ada"], "b_ada": lp["b_ada"], "w_in": lp["w_in"],
            "q_norm": lp["mla_q_norm"], "kv_norm": lp["mla_kv_norm"], "w_uq": lp["mla_w_uq"], "w_ukv": lp["mla_w_ukv"],
            "cs_sw": np.ascontiguousarray(csw, np.float32), "cs_r": np.ascontiguousarray(csr, np.float32),
        })
    res = run_bass_kernel_spmd(pb.nc, in_maps, core_ids=list(range(NCORES))).results
    P_lat = np.empty((B, T, O_END), np.float32)
    P_ctx = np.empty((B, CTX, O_END), np.float32)
    for k in range(NCORES):
        b, qd = k // 4, k % 4
        o = res[k]["out"]
        P_lat[b, qd * (T // 4):(qd + 1) * (T // 4)] = o[:NBL].reshape(T // 4, O_END)
        if qd < 2:
            P_ctx[b, qd * 128:(qd + 1) * 128] = o[NBL]
    return P_lat, P_ctx


def build_attn(NQB, H, Hkv, dq, dv, NKB, sched, n_bias, n_mask, use_sink, scale):
    pb = PB()
    NQ, NK = NQB * 128, NKB * 128
    QT = pb.din("QT", [H, dq, NQ])
    KT = pb.din("KT", [Hkv, dq, NK])
    V = pb.din("V", [NKB, 128, Hkv, dv])
    bias = pb.din("bias", [H, max(n_bias, 1), 128, 128])
    mask = pb.din("mask", [max(n_mask, 1), 128, 128])
    sink = pb.din("sink", [128, H])
    Yd = pb.dout("Y", [NQB, 128, H * dv])

    psS = [pb.ps("psS%d" % i, [128, 512]) for i in range(3)]
    psO = [pb.ps("psO%d" % i, [128, 4, 128]) for i in range(2)]
    QTb = pb.sb("QTb", [dq, H, NQ], BF16)
    KTb = pb.sb("KTb", [dq, NK], BF16)
    Vb = pb.sb("Vb", [128, NKB, dv + 1], BF16)
    Y = pb.sb("Y", [128, NQB, H * dv])
    CH = 2048
    kst = [pb.sb("kst%d" % i, [dq, CH]) for i in range(2)]
    VC = 16
    vst = [pb.sb("vst%d" % i, [128, VC, dv]) for i in range(2)]
    Et = [pb.sb("Et%d" % i, [128, 512], BF16) for i in range(3)]
    tmpS = [pb.sb("tmpS%d" % i, [128, 128]) for i in range(2)]
    bt = [pb.sb("bt%d" % i, [128, 128]) for i in range(3)]
    mt = [pb.sb("mt%d" % i, [128, 128]) for i in range(3)]
    den = [pb.sb("den%d" % i, [128, 4, 2]) for i in range(2)]
    sk = pb.sb("sk", [128, H])
    pb.dma(sk[:, :], sink[:, :])
    pb.act(sk[:, :], sk[:, :], AF.Exp)
    pb.memset(Vb[:, :, dv:dv + 1], 1.0)
    n = 0
    for h in range(H):
        for c0 in range(0, NQ, CH):
            c1 = min(NQ, c0 + CH)
            st = kst[n % 2]
            n += 1
            pb.dma(st[:, 0:c1 - c0], QT[h, :, c0:c1])
            pb.cp(QTb[:, h, c0:c1], st[:, 0:c1 - c0], e="pool")
    cnt = 0
    for hkv in range(Hkv):
        for c0 in range(0, NK, CH):
            c1 = min(NK, c0 + CH)
            st = kst[n % 2]
            n += 1
            pb.dma(st[:, 0:c1 - c0], KT[hkv, :, c0:c1])
            pb.cp(KTb[:, c0:c1], st[:, 0:c1 - c0], e="pool")
        for b0 in range(0, NKB, VC):
            b1 = min(NKB, b0 + VC)
            st = vst[n % 2]
            n += 1
            pb.dma(st[:, 0:b1 - b0, :], V[b0:b1, :, hkv, :].rr("b p d -> p b d"))
            pb.cp(Vb[:, b0:b1, 0:dv], st[:, 0:b1 - b0, :], e="pool")
        for h in range(hkv * (H // Hkv), (hkv + 1) * (H // Hkv)):
            for (i0, g, klist) in sched:
                po = psO[cnt % 2]
                dn = den[cnt % 2]
                cnt += 1
                nk = len(klist)
                def qk_exp(ki):
                    j, bid, mid = klist[ki]
                    ps = psS[ki % 3]
                    E = Et[ki % 3]
                    pb.mm(ps[:, 0:128 * g], KTb[:, 128 * j:128 * j + 128], QTb[:, h, 128 * i0:128 * (i0 + g)])
                    if bid is not None:
                        assert g == 1
                        B_ = bt[ki % 3]
                        pb.dma(B_[:, :], bias[h, bid, :, :])
                        tS = tmpS[ki % 2]
                        pb.stt(tS[:, :], ps[:, 0:128], scale, B_[:, :], ALU.mult, ALU.add)
                        pb.act(E[:, 0:128], tS[:, :], AF.Exp)
                    else:
                        pb.act(E[:, 0:128 * g], ps[:, 0:128 * g], AF.Exp, scale=scale)
                    if mid is not None:
                        assert g == 1
                        M_ = mt[ki % 3]
                        pb.dma(M_[:, :], mask[mid, :, :])
                        pb.tt(E[:, 0:128], E[:, 0:128], M_[:, :], ALU.mult, e="pool")

                def pv(ki):
                    j = klist[ki][0]
                    E = Et[ki % 3]
                    for s_ in range(g):
                        pb.mm(po[:, s_, 0:dv + 1], E[:, 128 * s_:128 * s_ + 128], Vb[:, j, :],
                              start=(ki == 0 and s_ == 0), stop=(ki == nk - 1), skip=(g > 1))

                qk_exp(0)
                for ki in range(nk):
                    if ki + 1 < nk:
                        qk_exp(ki + 1)
                    pv(ki)
                if use_sink:
                    pb.ts(dn[:, 0:g, 0:1], po[:, 0:g, dv:dv + 1], sk[:, h:h + 1], ALU.add)
                    pb.op("dve", [dn[:, :, :]], [dn[:, :, :]],
                          lambda e, dn=dn, g=g: e.reciprocal(out=dn.h[:, 0:g, 1:2], in_=dn.h[:, 0:g, 0:1]))
                else:
                    pb.op("dve", [po[:, :, :]], [dn[:, :, :]],
                          lambda e, dn=dn, po=po, g=g: e.reciprocal(out=dn.h[:, 0:g, 1:2], in_=po.h[:, 0:g, dv:dv + 1]))
                pb.tt(Y[:, i0:i0 + g, h * dv:(h + 1) * dv], po[:, 0:g, 0:dv],
                      dn[:, 0:g, 1:2].bc([128, g, dv]), ALU.mult)
    for i in range(NQB):
        pb.dma(Yd[i, :, :], Y[:, i, :])
    pb.finish()
    return pb


def _cls(i, NBL, edge):
    ncls = min(NBL, 2 * edge + 1)
    if i < edge:
        return i, ncls
    if i >= NBL - edge:
        return ncls - (NBL - i), ncls
    return edge, ncls


def _na_tiles(m, delta, nblk, rpb):
    rows = nblk * 2
    mk = m + delta
    if mk < 0 or mk >= nblk:
        return np.zeros((4, 128, 128), np.float32), np.zeros((128, 128), np.float32)
    idx = np.arange(128)
    qr, qc = 2 * m + idx // 64, idx % 64
    kr, kc = 2 * mk + idx // 64, idx % 64
    rs = np.clip(qr - 4, 0, rows - 8)
    cs = np.clip(qc - 8, 0, 64 - 16)
    valid = ((kr[:, None] >= rs[None, :]) & (kr[:, None] < rs[None, :] + 8) &
             (kc[:, None] >= cs[None, :]) & (kc[:, None] < cs[None, :] + 16))
    dr = np.clip(kr[:, None] - qr[None, :] + 7, 0, 14)
    dc = np.clip(kc[:, None] - qc[None, :] + 15, 0, 30)
    return np.ascontiguousarray(rpb[:, dr, dc], np.float32), valid.astype(np.float32)


def _swa_mask(m, delta, nblk):
    mk = m + delta
    if mk < 0 or mk >= nblk:
        return np.zeros((128, 128), np.float32)
    k = np.arange(128)[:, None]
    q = np.arange(128)[None, :]
    if delta == -1:
        return (q <= k).astype(np.float32)
    if delta == 1:
        return (k <= q).astype(np.float32)
    return np.ones((128, 128), np.float32)


def _halo(a, lo, hi):
    n = a.shape[0]
    out = np.zeros((hi - lo,) + a.shape[1:], a.dtype)
    s, e = max(lo, 0), min(hi, n)
    if e > s:
        out[s - lo:e - lo] = a[s:e]
    return out


def run_attn(kind, P_lat, P_ctx, lp):
    B, T, _ = P_lat.shape
    NBL = T // 4 // 128
    NBLT = T // 128
    NQB = NBL + 1
    if kind == "na":
        H, Hkv, dq, dv, halo, scale = 4, 4, 64, 64, 3, 64 ** -0.5
        oq, ok, ov = P_NAQ, P_NAK, P_NAV
    elif kind == "swa":
        H, Hkv, dq, dv, halo, scale = 4, 2, 64, 64, 1, 64 ** -0.5
        oq, ok, ov = O_SWQ, O_SWK, O_SWV
    else:
        H, Hkv, dq, dv, halo, scale = 4, 4, 96, 64, None, 96 ** -0.5
        oq, ok, ov = O_MLAQ, O_MLAK, O_MLAV
    NKB = 2 + (NBLT if halo is None else NBL + 2 * halo)
    sched = []
    ctxk = [(0, None, None), (1, None, None)]
    if kind == "mla":
        allk = [(j, None, None) for j in range(NKB)]
        for i0 in range(0, NBL, 4):
            sched.append((i0, min(4, NBL - i0), allk))
        n_bias = n_mask = 0
    else:
        nd = 2 * halo + 1
        edge = 2 if kind == "na" else 1
        for i in range(NBL):
            c, ncls = _cls(i, NBL, edge)
            kl = list(ctxk)
            for d in range(-halo, halo + 1):
                tid = c * nd + d + halo
                kl.append((2 + i + d + halo, tid if kind == "na" else None, tid))
            sched.append((i, 1, kl))
        n_mask = ncls * nd
        n_bias = n_mask if kind == "na" else 0
    sched.append((NBL, 1, ctxk))
    key = ("attn", kind, NQB)
    pb = _prog(key, lambda: build_attn(NQB, H, Hkv, dq, dv, NKB, sched, n_bias, n_mask, kind == "swa", scale))
    in_maps = []
    for k in range(NCORES):
        b, qd = k // 4, k % 4
        t0, t1 = qd * NBL * 128, (qd + 1) * NBL * 128
        ch = (k % 2) * 128
        q = np.concatenate([P_lat[b, t0:t1, oq:oq + H * dq], P_ctx[b, ch:ch + 128, oq:oq + H * dq]], 0)
        QT = np.ascontiguousarray(q.reshape(NQB * 128, H, dq).transpose(1, 2, 0))
        kl = P_lat[b, :, ok:ok + Hkv * dq].reshape(NBLT, 128, Hkv, dq)
        vl = P_lat[b, :, ov:ov + Hkv * dv].reshape(NBLT, 128, Hkv, dv)
        if halo is not None:
            kl = _halo(kl, qd * NBL - halo, (qd + 1) * NBL + halo)
            vl = _halo(vl, qd * NBL - halo, (qd + 1) * NBL + halo)
        kk = np.concatenate([P_ctx[b, :, ok:ok + Hkv * dq].reshape(2, 128, Hkv, dq), kl], 0)
        vv = np.concatenate([P_ctx[b, :, ov:ov + Hkv * dv].reshape(2, 128, Hkv, dv), vl], 0)
        KT = np.ascontiguousarray(kk.reshape(NKB * 128, Hkv, dq).transpose(1, 2, 0))
        bias = np.zeros((H, max(n_bias, 1), 128, 128), np.float32)
        mask = np.zeros((max(n_mask, 1), 128, 128), np.float32)
        if kind != "mla":
            for i in range(NBL):
                c, _ = _cls(i, NBL, edge)
                for d in range(-halo, halo + 1):
                    tid = c * nd + d + halo
                    if kind == "na":
                        g_, m_ = _na_tiles(qd * NBL + i, d, NBLT, lp["na_rpb"])
                        bias[:, tid] = g_
                        mask[tid] = m_
                    else:
                        mask[tid] = _swa_mask(qd * NBL + i, d, NBLT)
        in_maps.append({"QT": QT, "KT": KT, "V": np.ascontiguousarray(vv), "bias": bias, "mask": mask,
                        "sink": np.ascontiguousarray(np.broadcast_to(lp["swa_sink"][None, :], (128, H)), np.float32)})
    res = run_bass_kernel_spmd(pb.nc, in_maps, core_ids=list(range(NCORES))).results
    y_lat = np.empty((B, T, H * dv), np.float32)
    y_ctx = np.empty((B, CTX, H * dv), np.float32)
    for k in range(NCORES):
        b, qd = k // 4, k % 4
        o = res[k]["Y"]
        y_lat[b, qd * NBL * 128:(qd + 1) * NBL * 128] = o[:NBL].reshape(NBL * 128, H * dv)
        if qd < 2:
            y_ctx[b, qd * 128:(qd + 1) * 128] = o[NBL]
    return y_lat, y_ctx


SC = 16


def build_mlstm(T):
    pb = PB()
    NCH = (CTX + T) // 64
    LP = CTX + T + 8
    groups = [(0, 4)] + [(4 + SC * i, SC) for i in range((T // 64) // SC)]
    raw = pb.din("raw", [2, 64, LP])
    convw = pb.din("convw", [2, 64, 5])
    vd = pb.din("v", [64, NCH, 64])
    od = pb.din("o", [64, NCH, 64])
    gd = pb.din("g", [64, NCH, 4])
    gbd = pb.din("gb", [64, 4])
    cd = pb.din("consts", [64, 6, 64])
    out = pb.dout("out", [64, NCH, 64])
    hf = Tl(pb.nc.dram_tensor("hf_scratch", [64, NCH, 64], F32), True)

    cs = pb.sb("cs", [64, 6, 64])
    pb.dma(cs[:, :, :], cd[:, :, :])
    cw = pb.sb("cw", [64, 2, 5])
    pb.dma(cw[:, :, :], convw[:, :, :].rr("a p j -> p a j"))
    gb = pb.sb("gb", [64, 4])
    pb.dma(gb[:, :], gbd[:, :])
    one = pb.sb("one", [64, 1])
    pb.memset(one[:, :], 1.0)
    psS = [pb.ps("psS%d" % i, [64, 64]) for i in range(2)]
    psO = [pb.ps("psO%d" % i, [64, 65]) for i in range(2)]
    psU = [pb.ps("psU%d" % i, [64, 65]) for i in range(2)]
    psT = pb.ps("psT", [64, 512])
    psG = pb.ps("psG", [64, 3, SC])
    W = 64 * SC
    rw = [pb.sb("rw%d" % i, [64, W + 4]) for i in range(2)]
    qk = [pb.sb("qk%d" % i, [64, W]) for i in range(2)]
    ktok = pb.sb("ktok", [64, SC, 64])
    vaug = pb.sb("vaug", [64, SC, 65])
    pb.memset(vaug[:, :, 64:65], 1.0)
    gt = pb.sb("gt", [64, SC, 4])
    gi = pb.sb("gi", [64, SC])
    lf = pb.sb("lf", [64, SC])
    ex = pb.sb("ex", [64, 4, SC])
    tg = pb.sb("tg", [64, 2, SC])
    PT = [pb.sb("PT%d" % i, [64, 64]) for i in range(2)]
    kw = [pb.sb("kw%d" % i, [64, 64]) for i in range(2)]
    Cst = [pb.sb("C%d" % i, [64, 65]) for i in range(2)]
    Ob = pb.sb("Ob", [64, SC, 65])
    Hb = pb.sb("Hb", [64, SC, 64])
    H2 = pb.sb("H2", [64, SC, 64])
    ot = pb.sb("ot", [64, SC, 64])
    cf = pb.sb("cf", [64, 2, SC])
    for d in range(2):
        t_in, t_ex = (0, 1) if d == 0 else (2, 3)
        order = groups if d == 0 else [groups[0]] + groups[:0:-1]
        ci = 0
        pb.memset(Cst[0][:, :], 0.0)
        for (c0, n) in order:
            w = 64 * n
            start = (2 + 64 * c0) if c0 < 4 else (CTX + 6 + 64 * (c0 - 4))
            for a in range(2):
                pb.dma(rw[a][:, 0:w + 4], raw[a, :, start - 2:start + w + 2])
                acc = qk[a]
                pb.ts(acc[:, 0:w], rw[a][:, 0:w], cw[:, a, 0:1], ALU.mult)
                for j in range(1, 5):
                    pb.stt(acc[:, 0:w], rw[a][:, j:j + w], cw[:, a, j:j + 1], acc[:, 0:w], ALU.mult, ALU.add)
                pb.act(acc[:, 0:w], acc[:, 0:w], AF.Silu)
            pb.ts(qk[0][:, 0:w], qk[0][:, 0:w], 0.125, ALU.mult)
            pb.dma(vaug[:, 0:n, 0:64], vd[:, c0:c0 + n, :])
            pb.dma(gt[:, 0:n, :], gd[:, c0:c0 + n, :])
            for c in range(n):
                pb.tr(psT[:, 64 * (c % 8):64 * (c % 8) + 64], qk[1][:, 64 * c:64 * c + 64], cs[:, 5, :])
                if c % 8 == 7 or c == n - 1:
                    b0 = c - (c % 8)
                    pb.cp(ktok[:, b0:c + 1, :], psT[:, 0:64 * (c - b0 + 1)].rr("p (a b) -> p a b", b=64), e="act")
            pb.ts(gi[:, 0:n], gt[:, 0:n, 2 * d], gb[:, 2 * d:2 * d + 1], ALU.add)
            pb.ts(lf[:, 0:n], gt[:, 0:n, 2 * d + 1], gb[:, 2 * d + 1:2 * d + 2], ALU.add)
            pb.act(lf[:, 0:n], lf[:, 0:n], AF.Exp, scale=-1.0)
            pb.act(lf[:, 0:n], lf[:, 0:n], AF.Ln, bias=one[:, 0:1])
            pb.ts(lf[:, 0:n], lf[:, 0:n], -1.0, ALU.mult)
            for r, sel in enumerate((t_in, t_ex, 4)):
                pb.mm(psG[:, r, 0:n], cs[:, sel, :], lf[:, 0:n])
            pb.act(ex[:, 0, 0:n], psG[:, 0, 0:n], AF.Exp)
            pb.tt(tg[:, 0, 0:n], gi[:, 0:n], psG[:, 0, 0:n], ALU.subtract)
            pb.act(ex[:, 1, 0:n], tg[:, 0, 0:n], AF.Exp)
            pb.tt(tg[:, 1, 0:n], gi[:, 0:n], psG[:, 1, 0:n], ALU.add)
            pb.act(ex[:, 2, 0:n], tg[:, 1, 0:n], AF.Exp)
            pb.act(ex[:, 3, 0:n], psG[:, 2, 0:n], AF.Exp)
            chunks = list(range(n)) if d == 0 else list(range(n - 1, -1, -1))
            for c in chunks:
                Cc, Cn = Cst[ci % 2], Cst[(ci + 1) % 2]
                pS, pO, pU = psS[ci % 2], psO[ci % 2], psU[ci % 2]
                P_, K_ = PT[ci % 2], kw[ci % 2]
                ci += 1
                qT = qk[0][:, 64 * c:64 * c + 64]
                kT = qk[1][:, 64 * c:64 * c + 64]
                pb.mm(pS[:, :], kT, qT)
                pb.stt(P_[:, :], pS[:, :], ex[:, 1, c:c + 1], cs[:, t_in, :], ALU.mult, ALU.mult)
                pb.ts(K_[:, :], ktok[:, c, :], ex[:, 2, c:c + 1], ALU.mult, e="pool")
                pb.mm(pO[:, :], P_[:, :], vaug[:, c, :], start=True, stop=False)
                pb.mm(pO[:, :], qT, Cc[:, :], start=False, stop=True)
                pb.cp(Ob[:, c, :], pO[:, :], e="act")
                pb.mm(pU[:, :], K_[:, :], vaug[:, c, :])
                pb.stt(Cn[:, :], Cc[:, :], ex[:, 3, c:c + 1], pU[:, :], ALU.mult, ALU.add)
            pb.tt(cf[:, 0, 0:n], Ob[:, 0:n, 64], ex[:, 0, 0:n], ALU.mult)
            pb.act(cf[:, 0, 0:n], cf[:, 0, 0:n], AF.Abs)
            pb.ts(cf[:, 0, 0:n], cf[:, 0, 0:n], 1.0, ALU.max)
            pb.op("dve", [cf[:, :, :]], [cf[:, :, :]],
                  lambda e, n=n: e.reciprocal(out=cf.h[:, 1, 0:n], in_=cf.h[:, 0, 0:n]))
            pb.tt(cf[:, 1, 0:n], cf[:, 1, 0:n], ex[:, 0, 0:n], ALU.mult)
            pb.tt(Hb[:, 0:n, :], Ob[:, 0:n, 0:64], cf[:, 1, 0:n].rr("p (n o) -> p n o", o=1).bc([64, n, 64]), ALU.mult)
            if d == 0:
                pb.dma(hf[:, c0:c0 + n, :], Hb[:, 0:n, :])
            else:
                pb.dma(H2[:, 0:n, :], hf[:, c0:c0 + n, :])
                pb.dma(ot[:, 0:n, :], od[:, c0:c0 + n, :])
                pb.act(ot[:, 0:n, :], ot[:, 0:n, :], AF.Sigmoid)
                pb.tt(H2[:, 0:n, :], H2[:, 0:n, :], Hb[:, 0:n, :], ALU.add)
                pb.tt(H2[:, 0:n, :], H2[:, 0:n, :], ot[:, 0:n, :], ALU.mult, e="pool")
                pb.dma(out[:, c0:c0 + n, :], H2[:, 0:n, :])
    pb.finish()
    return pb


def run_mlstm(P_lat, P_ctx, lp):
    B, T, _ = P_lat.shape
    NCH = (CTX + T) // 64
    pb = _prog(("mlstm", T), lambda: build_mlstm(T))
    u = np.arange(64)[:, None]
    t = np.arange(64)[None, :]
    consts = np.stack([(u <= t), (u > t), (u >= t), (u < t), np.ones((64, 64), bool), (u == t)], 1).astype(np.float32)
    in_maps = []
    for k in range(NCORES):
        b, h = k // 4, k % 4
        seq = np.concatenate([P_ctx[b], P_lat[b]], 0)
        raw = np.zeros((2, 64, CTX + T + 8), np.float32)
        for a in range(2):
            col = P_MLQK + 256 * a + 64 * h
            raw[a, :, 2:2 + CTX] = seq[:CTX, col:col + 64].T
            raw[a, :, CTX + 6:CTX + 6 + T] = seq[CTX:, col:col + 64].T
        cw = np.stack([lp["ml_conv"][:, 256 * a + 64 * h:256 * a + 64 * h + 64].T for a in range(2)], 0)
        chunk = lambda a: np.ascontiguousarray(a.reshape(NCH, 64, -1).transpose(1, 0, 2))
        gcols = [P_MLG + 4 * j + h for j in range(4)]
        in_maps.append({
            "raw": raw, "convw": np.ascontiguousarray(cw, np.float32),
            "v": chunk(seq[:, P_MLV + 64 * h:P_MLV + 64 * h + 64]),
            "o": chunk(seq[:, P_MLO + 64 * h:P_MLO + 64 * h + 64]),
            "g": chunk(seq[:, gcols]),
            "gb": np.ascontiguousarray(np.broadcast_to(lp["ml_gate_b"][[h, 4 + h, 8 + h, 12 + h]][None, :], (64, 4)), np.float32),
            "consts": consts,
        })
    res = run_bass_kernel_spmd(pb.nc, in_maps, core_ids=list(range(NCORES))).results
    y_lat = np.empty((B, T, 256), np.float32)
    y_ctx = np.empty((B, CTX, 256), np.float32)
    for k in range(NCORES):
        b, h = k // 4, k % 4
        o = res[k]["out"].transpose(1, 0, 2).reshape(CTX + T, 64)
        y_ctx[b, :, 64 * h:64 * h + 64] = o[:CTX]
        y_lat[b, :, 64 * h:64 * h + 64] = o[CTX:]
    return y_lat, y_ctx


def build_out(NB, ctx_last):
    pb = PB()
    x = pb.din("x", [NB, 128, D])
    yc = pb.din("ycat", [NB, 128, D])
    cT = pb.din("cT", [128, 8, 2])
    ident_d = pb.din("ident", [128, 128])
    g2n = pb.din("norm_g", [D])
    w_ada = pb.din("w_ada", [D, 6 * D])
    b_ada = pb.din("b_ada", [6 * D])
    w_out = pb.din("w_out", [D, D])
    wq_d = pb.din("wq", [D, 2048])
    keysT = pb.din("keysT", [128, 16, 128])
    x1o = pb.dout("x1", [NB, 128, D])
    h2o = pb.dout("h2", [NB, 128, D])
    shpo = pb.dout("shp", [NB, 128, 16, 128])
    paro = pb.dout("par", [NB, 128, 8, 2])

    psA = [pb.ps("psA%d" % i, [128, 512]) for i in range(4)]
    psT = [pb.ps("psT%d" % i, [128, 512]) for i in range(2)]
    psK = [pb.ps("psK%d" % i, [128, 512]) for i in range(2)]
    ident = pb.sb("ident", [128, 128])
    pb.dma(ident[:, :], ident_d[:, :])
    pb.eps_t = pb.sb("eps", [128, 1])
    pb.memset(pb.eps_t[:, :], EPS)
    cTs = pb.sb("cTs", [128, 8, 2])
    pb.dma(cTs[:, :, :], cT[:, :, :])
    QR = pb.sb("QR", [128, 2048])
    QT = pb.sb("QT", [128, 16, 128])
    svs = [QR[:, :].rr("p (a b) -> p a b", a=8), QT[:, :, :].rr("p (a c) b -> p a (c b)", a=8)]
    sl = pb.sb("sl", [128, 8, 128])
    gbc = pb.sb("gbc", [128, D])
    pb.dma(gbc[:, :], View(g2n, g2n.h.ap().partition_broadcast(128)))
    mods = []
    for g in range(2 if ctx_last else 1):
        m = pb.sb("modo%d" % g, [128, 4096])
        pb.act(sl[:, :, :], cTs[:, :, g:g + 1].bc([128, 8, 128]), AF.Silu)
        pb.dma(m[:, :], View(b_ada, b_ada.h.ap()[2048:6144].partition_broadcast(128)))
        for n0 in range(0, 4096, 256):
            sv = svs[(n0 // 256) % 2]
            pb.dma(sv, View(w_ada, w_ada.h.ap()[:, 2048 + n0:2048 + n0 + 256].rearrange("(kc k) n -> k kc n", k=128)))
            pt = psA[(n0 // 256) % 4]
            for kc in range(8):
                pb.mm(pt[:, 0:256], sl[:, kc, :], sv[:, kc, :], start=(kc == 0), stop=(kc == 7))
            pb.tt(m[:, n0:n0 + 256], pt[:, 0:256], m[:, n0:n0 + 256], ALU.add)
        pb.stt(m[:, 2048:3072], m[:, 2048:3072], 1.0, gbc[:, :], ALU.add, ALU.mult)
        mods.append(m)
    Wo = pb.sb("Wo", [128, 8, D], BF16)
    for kc in range(8):
        pb.dma(QR[:, 0:1024], w_out[128 * kc:128 * kc + 128, :])
        pb.cp(Wo[:, kc, :], QR[:, 0:1024])
    wq = pb.sb("wq", [128, 8, 2048])
    for kc in range(8):
        pb.dma(wq[:, kc, :], wq_d[128 * kc:128 * kc + 128, :])
    kT = pb.sb("kT", [128, 16, 128])
    pb.dma(kT[:, :, :], keysT[:, :, :])

    X = pb.sb("X", [128, D])
    Yc = pb.sb("Yc", [128, D])
    YT = pb.sb("YT", [128, 8, 128], BF16)
    X1 = pb.sb("X1", [128, D])
    T = pb.sb("T", [128, D])
    H2 = pb.sb("H2", [128, D])
    H2T = pb.sb("H2T", [128, 8, 128])
    SHP = pb.sb("SHP", [128, 16, 128])
    T16 = pb.sb("T16", [128, 16, 16])
    CAND = pb.sb("CAND", [128, 8, 256])
    F16 = pb.sb("F16", [128, 8, 16])
    tS = pb.sb("tS", [128, 256])
    S = pb.sb("S", [128, 8])
    Z = pb.sb("Z", [128, 8, 2])
    PAR = pb.sb("PAR", [128, 8, 2])
    tF = pb.sb("tF", [128, 8, 16])
    for b in range(NB):
        m = mods[1 if (ctx_last and b == NB - 1) else 0]
        pb.dma(X[:, :], x[b, :, :])
        pb.dma(Yc[:, :], yc[b, :, :])
        for half in range(2):
            for j in range(4):
                kc = half * 4 + j
                pb.tr(psT[half][:, 128 * j:128 * j + 128], Yc[:, 128 * kc:128 * kc + 128], ident[:, :])
            pb.cp(YT[:, 4 * half:4 * half + 4, :], psT[half][:, :].rr("p (a b) -> p a b", a=4), e=("act" if half else "dve"))
        for nt in range(2):
            for kc in range(8):
                pb.mm(psA[nt][:, :], YT[:, kc, :], Wo[:, kc, 512 * nt:512 * nt + 512], start=(kc == 0), stop=(kc == 7))
            pb.tt(T[:, 512 * nt:512 * nt + 512], psA[nt][:, :], m[:, 512 * nt:512 * nt + 512], ALU.mult)
        pb.tt(X1[:, :], X[:, :], T[:, :], ALU.add, e="pool")
        pb.dma(x1o[b, :, :], X1[:, :])
        pb.act(T[:, :], X1[:, :], AF.Square, accum=S[:, 0:1], scale=float(D) ** -0.5)
        rstd(pb, S[:, 1:2], S[:, 0:1])
        pb.stt(H2[:, :], X1[:, :], S[:, 1:2], m[:, 2048:3072], ALU.mult, ALU.mult)
        pb.tt(H2[:, :], H2[:, :], m[:, 1024:2048], ALU.add, e="pool")
        pb.dma(h2o[b, :, :], H2[:, :])
        for half in range(2):
            for j in range(4):
                kc = half * 4 + j
                pb.tr(psT[half][:, 128 * j:128 * j + 128], H2[:, 128 * kc:128 * kc + 128], ident[:, :])
            pb.cp(H2T[:, 4 * half:4 * half + 4, :], psT[half][:, :].rr("p (a b) -> p a b", a=4), e=("act" if half else "dve"))
        for nt in range(4):
            pt = psA[2 + nt % 2]
            for kc in range(8):
                pb.mm(pt[:, :], H2T[:, kc, :], wq[:, kc, 512 * nt:512 * nt + 512], start=(kc == 0), stop=(kc == 7))
            pb.cp(QR[:, 512 * nt:512 * nt + 512], pt[:, :], e=("act" if nt % 2 else "dve"))
        for q4 in range(4):
            pt = psT[q4 % 2]
            for j in range(4):
                pb.tr(pt[:, 128 * j:128 * j + 128], QR[:, 128 * (4 * q4 + j):128 * (4 * q4 + j) + 128], ident[:, :])
            pb.cp(QT[:, 4 * q4:4 * q4 + 4, :], pt[:, :].rr("p (a b) -> p a b", a=4), e=("act" if q4 % 2 else "dve"))
        for q4 in range(4):
            pt = psK[q4 % 2]
            for j in range(4):
                pb.mm(pt[:, 128 * j:128 * j + 128], QT[:, 4 * q4 + j, :], kT[:, 4 * q4 + j, :], start=True, stop=True)
            pb.cp(SHP[:, 4 * q4:4 * q4 + 4, :], pt[:, :].rr("p (a b) -> p a b", a=4), e=("act" if q4 % 2 else "dve"))
        pb.dma(shpo[b, :, :, :], SHP[:, :, :])
        for j in range(16):
            pb.vmax8(T16[:, j, 0:8], SHP[:, j, :])
            pb.match_replace(tS[:, 0:128], T16[:, j, 0:8], SHP[:, j, :], -1e30)
            pb.vmax8(T16[:, j, 8:16], tS[:, 0:128])
        A = T16[:, :, :].rr("p (h two) r -> p h two r", two=2)
        pb.tt(CAND[:, :, :].rr("p h (a b) -> p h a b", a=16),
              A[:, :, 0, :].rr("p h (a o) -> p h a o", o=1).bc([128, 8, 16, 16]),
              A[:, :, 1, :].rr("p h (o b) -> p h o b", o=1).bc([128, 8, 16, 16]), ALU.add)
        for h in range(8):
            pb.vmax8(F16[:, h, 0:8], CAND[:, h, :])
            pb.match_replace(tS[:, :], F16[:, h, 0:8], CAND[:, h, :], -1e30)
            pb.vmax8(F16[:, h, 8:16], tS[:, :])
        pb.tt(tF[:, :, :], F16[:, :, :], F16[:, :, 0:1].bc([128, 8, 16]), ALU.subtract)
        pb.act(tF[:, :, :], tF[:, :, :], AF.Exp)
        pb.reduce(Z[:, :, 0], tF[:, :, :], ALU.add)
        pb.act(Z[:, :, 1], Z[:, :, 0], AF.Ln)
        pb.cp(PAR[:, :, 0], F16[:, :, 15], e="pool")
        pb.tt(PAR[:, :, 1], F16[:, :, 0], Z[:, :, 1], ALU.add)
        pb.ts(PAR[:, :, 1], PAR[:, :, 1], -1.0, ALU.mult)
        pb.dma(paro[b, :, :, :], PAR[:, :, :])
    pb.finish()
    return pb


IC = 8


def build_peer(NB, ctx_last, final):
    pb = PB()
    h2T = pb.din("h2T", [NB, 128, 8, 128])
    shp = pb.din("shp", [NB, 128, 16, 128])
    par = pb.din("par", [NB, 128, 8, 2])
    x1 = pb.din("x1", [NB, 128, D])
    cT = pb.din("cT", [128, 8, 2])
    ident_d = pb.din("ident", [128, 128])
    w_ada = pb.din("w_ada", [D, 6 * D])
    b_ada = pb.din("b_ada", [6 * D])
    uT = pb.din("uT", [128, 8, 16384])
    vd = pb.din("v", [16384, D])
    fg = pb.din("final_g", [D])
    out = pb.dout("out", [NB, 128, D])

    psO = [pb.ps("psO%d" % i, [128, 512]) for i in range(4)]
    psA = [pb.ps("psA%d" % i, [128, 256]) for i in range(2)]
    psW = [pb.ps("psW%d" % i, [128, 256]) for i in range(2)]
    psM = psO[0]
    ident = pb.sb("ident", [128, 128])
    pb.dma(ident[:, :], ident_d[:, :])
    pb.eps_t = pb.sb("eps", [128, 1])
    pb.memset(pb.eps_t[:, :], EPS)
    cTs = pb.sb("cTs", [128, 8, 2])
    pb.dma(cTs[:, :, :], cT[:, :, :])
    sl = pb.sb("sl", [128, 8, 128])
    fgb = pb.sb("fgb", [128, D])
    pb.dma(fgb[:, :], View(fg, fg.h.ap().partition_broadcast(128)))
    stg = [pb.sb("stg%d" % i, [128, 8, 256]) for i in range(2)]
    mods = []
    for g in range(2 if ctx_last else 1):
        m = pb.sb("modp%d" % g, [128, D])
        pb.act(sl[:, :, :], cTs[:, :, g:g + 1].bc([128, 8, 128]), AF.Silu)
        pb.dma(m[:, :], View(b_ada, b_ada.h.ap()[5120:6144].partition_broadcast(128)))
        for n0 in range(0, D, 256):
            sv = stg[(n0 // 256) % 2]
            pb.dma(sv[:, :, :], View(w_ada, w_ada.h.ap()[:, 5120 + n0:5120 + n0 + 256].rearrange("(kc k) n -> k kc n", k=128)))
            for kc in range(8):
                pb.mm(psM[:, 0:256], sl[:, kc, :], sv[:, kc, :], start=(kc == 0), stop=(kc == 7))
            pb.tt(m[:, n0:n0 + 256], psM[:, 0:256], m[:, n0:n0 + 256], ALU.add)
        mods.append(m)

    ub = Tl(pb.nc.dram_tensor("ub_scratch", [128, 128, 1024], BF16), True)
    vb = Tl(pb.nc.dram_tensor("vb_scratch", [128, 128, 1024], BF16), True)
    c32 = [pb.sb("c32_%d" % i, [128, 1024]) for i in range(4)]
    c16 = [pb.sb("c16_%d" % i, [128, 1024], BF16) for i in range(4)]
    for i in range(128):
        a, b_ = c32[(2 * i) % 4], c32[(2 * i + 1) % 4]
        a16, b16 = c16[(2 * i) % 4], c16[(2 * i + 1) % 4]
        pb.dma(a[:, :].rr("p (k e) -> p k e", k=8), uT[:, :, 128 * i:128 * i + 128])
        pb.dma(b_[:, :], vd[128 * i:128 * i + 128, :])
        pb.cp(a16[:, :], a[:, :], e="act")
        pb.cp(b16[:, :], b_[:, :], e="dve")
        pb.dma(ub[i, :, :], a16[:, :])
        pb.dma(vb[i, :, :], b16[:, :])
    HT32 = pb.sb("HT32", [128, 8, 128])
    HT = pb.sb("HT", [128, 8, 256], BF16)
    SH = pb.sb("SH", [128, 2, 16, 128])
    PR = pb.sb("PR", [128, 2, 8, 2])
    X1 = pb.sb("X1", [128, 2, D])
    Sb = [pb.sb("Sb%d" % i, [128, IC, 128]) for i in range(2)]
    Eb = [pb.sb("Eb%d" % i, [128, IC, 128]) for i in range(2)]
    Whb = [[[pb.sb("Wh%d_%d_%d" % (p, k, h), [128, IC, 128], BF16) for h in range(8)] for k in range(2)] for p in range(2)]
    identb = pb.sb("identb", [128, 128], BF16)
    pb.cp(identb[:, :], ident[:, :])
    ut = [pb.sb("ut%d" % i, [128, 8, 128], BF16) for i in range(3)]
    vt = [pb.sb("vt%d" % i, [128, D], BF16) for i in range(3)]
    Gs = [pb.sb("Gs%d" % i, [128, 256]) for i in range(2)]
    GT = [pb.sb("GT%d" % i, [128, 256], BF16) for i in range(2)]
    O = pb.sb("O", [128, D])
    T = pb.sb("T", [128, D])
    S = pb.sb("S", [128, 4])
    tiles = [(b0, min(2, NB - b0)) for b0 in range(0, NB, 2)]
    cnt = 0
    wcnt = 0
    for (b0, ts) in tiles:
        tw = 128 * ts
        for k in range(ts):
            pb.dma(HT32[:, :, :], h2T[b0 + k, :, :, :])
            pb.cp(HT[:, :, 128 * k:128 * k + 128], HT32[:, :, :], e="act")
            pb.dma(SH[:, k, :, :], shp[b0 + k, :, :, :])
            pb.dma(PR[:, k, :, :], par[b0 + k, :, :, :])
            pb.dma(X1[:, k, :], x1[b0 + k, :, :])
        def dense(ic, sub):
            nonlocal wcnt
            Wc = Whb[ic % 2]
            kh = [(k, h) for k in range(ts) for h in range(8)]
            for (k, h) in (kh if sub is None else kh[sub::IC]):
                S_, E_ = Sb[wcnt % 2], Eb[wcnt % 2]
                wcnt += 1
                pb.tt(S_[:, :, :],
                      SH[:, k, 2 * h, IC * ic:IC * ic + IC].rr("p (a o) -> p a o", o=1).bc([128, IC, 128]),
                      SH[:, k, 2 * h + 1, :].rr("p (o b) -> p o b", o=1).bc([128, IC, 128]), ALU.add)
                pb.act(E_[:, :, :], S_[:, :, :], AF.Exp, bias=PR[:, k, h, 1:2])
                pb.stt(Wc[k][h][:, :, :], S_[:, :, :], PR[:, k, h, 0:1], E_[:, :, :], ALU.is_ge, ALU.mult)

        def stage_a(i):
            nonlocal cnt
            Wc = Whb[(i // IC) % 2]
            ii = i % IC
            U_, V_ = ut[cnt % 3], vt[cnt % 3]
            pA, pW = psA[cnt % 2], psW[cnt % 2]
            G1, G2 = Gs[cnt % 2], GT[cnt % 2]
            cnt += 1
            pb.dma(U_[:, :, :].rr("p k e -> p (k e)"), ub[i, :, :])
            pb.dma(V_[:, :], vb[i, :, :])
            for k in range(ts):
                for h in range(8):
                    pb.mm(pW[:, 128 * k:128 * k + 128], Wc[k][h][:, ii, :], identb[:, :],
                          start=(h == 0), stop=(h == 7), skip=True)
            for kc in range(8):
                pb.mm(pA[:, 0:tw], U_[:, kc, :], HT[:, kc, 0:tw], start=(kc == 0), stop=(kc == 7))
            pb.act(G1[:, 0:tw], pA[:, 0:tw], AF.Gelu)
            pb.tt(G2[:, 0:tw], G1[:, 0:tw], pW[:, 0:tw], ALU.mult)
            return G2, V_

        def stage_b(i, G2, V_):
            for k in range(ts):
                for nt in range(2):
                    pb.mm(psO[2 * k + nt][:, :], G2[:, 128 * k:128 * k + 128], V_[:, 512 * nt:512 * nt + 512],
                          start=(i == 0), stop=(i == 127))

        dense(0, None)
        prev = None
        for i in range(128):
            ic, ii = i // IC, i % IC
            cur = stage_a(i)
            if prev is not None:
                stage_b(i - 1, *prev)
            prev = cur
            if ic + 1 < 128 // IC:
                dense(ic + 1, ii)
        stage_b(127, *prev)
        for k in range(ts):
            b = b0 + k
            m = mods[1 if (ctx_last and b == NB - 1) else 0]
            for nt in range(2):
                pb.tt(T[:, 512 * nt:512 * nt + 512], psO[2 * k + nt][:, :], m[:, 512 * nt:512 * nt + 512], ALU.mult)
            pb.tt(O[:, :], X1[:, k, :], T[:, :], ALU.add, e="pool")
            if final:
                pb.act(T[:, :], O[:, :], AF.Square, accum=S[:, 0:1], scale=float(D) ** -0.5)
                rstd(pb, S[:, 1:2], S[:, 0:1])
                pb.stt(O[:, :], O[:, :], S[:, 1:2], fgb[:, :], ALU.mult, ALU.mult)
            pb.dma(out[b, :, :], O[:, :])
    pb.finish()
    return pb


def run_out_peer(x, xc, ycat_lat, ycat_ctx, c, c_ctx, lp, need_ctx, final, final_g):
    B, T, _ = x.shape
    NBL = T // 4 // 128
    NB = NBL + (1 if need_ctx else 0)
    pbo = _prog(("out", NB, need_ctx), lambda: build_out(NB, need_ctx))
    pbp = _prog(("peer", NB, need_ctx, final), lambda: build_peer(NB, need_ctx, final))
    eye = np.eye(128, dtype=np.float32)
    keysT = np.ascontiguousarray(lp["peer_keys"].reshape(16, 128, 128).transpose(2, 0, 1))
    uT = np.ascontiguousarray(lp["peer_u"].reshape(16384, 8, 128).transpose(2, 1, 0))

    def blocks(lat, ctx, k):
        b, qd = k // 4, k % 4
        a = lat[b, qd * NBL * 128:(qd + 1) * NBL * 128].reshape(NBL, 128, -1)
        if need_ctx:
            a = np.concatenate([a, ctx[b, (k % 2) * 128:(k % 2) * 128 + 128][None]], 0)
        return np.ascontiguousarray(a, np.float32)

    in_maps = []
    for k in range(NCORES):
        b = k // 4
        in_maps.append({"x": blocks(x, xc, k), "ycat": blocks(ycat_lat, ycat_ctx, k),
                        "cT": cT_layout(np.stack([c[b], c_ctx])), "ident": eye, "norm_g": lp["norm2_g"],
                        "w_ada": lp["w_ada"], "b_ada": lp["b_ada"], "w_out": lp["w_out"], "wq": lp["peer_wq"],
                        "keysT": keysT})
    r1 = run_bass_kernel_spmd(pbo.nc, in_maps, core_ids=list(range(NCORES))).results
    in_maps = []
    for k in range(NCORES):
        b = k // 4
        h2 = r1[k]["h2"]
        h2T = np.ascontiguousarray(h2.reshape(NB, 128, 8, 128).transpose(0, 3, 2, 1))
        in_maps.append({"h2T": h2T, "shp": r1[k]["shp"], "par": r1[k]["par"], "x1": r1[k]["x1"],
                        "cT": cT_layout(np.stack([c[b], c_ctx])), "ident": eye,
                        "w_ada": lp["w_ada"], "b_ada": lp["b_ada"], "uT": uT, "v": lp["peer_v"],
                        "final_g": final_g})
    r2 = run_bass_kernel_spmd(pbp.nc, in_maps, core_ids=list(range(NCORES))).results
    xn = np.empty_like(x)
    xcn = np.empty_like(xc) if need_ctx else None
    for k in range(NCORES):
        b, qd = k // 4, k % 4
        o = r2[k]["out"]
        xn[b, qd * NBL * 128:(qd + 1) * NBL * 128] = o[:NBL].reshape(NBL * 128, D)
        if need_ctx and qd < 2:
            xcn[b, qd * 128:(qd + 1) * 128] = o[NBL]
    return xn, xcn


def _tick(msg, t0=[None]):
    import time
    now = time.time()
    if t0[0] is not None:
        print("[kernel] %s %.1fs" % (msg, now - t0[0]), flush=True)
    t0[0] = now


def run_layer(x, xc, c, c_ctx, lp, need_ctx, final, final_g):
    _tick("start")
    P_lat, P_ctx = run_l1(x, xc, c, c_ctx, lp)
    _tick("l1")
    ya, yac = run_attn("na", P_lat, P_ctx, lp)
    _tick("na")
    yb, ybc = run_mlstm(P_lat, P_ctx, lp)
    _tick("mlstm")
    ym, ymc = run_attn("mla", P_lat, P_ctx, lp)
    _tick("mla")
    yd, ydc = run_attn("swa", P_lat, P_ctx, lp)
    _tick("swa")
    ycat = np.concatenate([ya, yb, ym, yd], -1)
    ycatc = np.concatenate([yac, ybc, ymc, ydc], -1)
    return run_out_peer(x, xc, ycat, ycatc, c, c_ctx, lp, need_ctx, final, final_g)


def kernel(x, c, ctx, c_ctx, norm1_g, norm2_g, w_ada, b_ada, w_in, na_rpb, ml_conv, ml_gate_b,
           mla_q_norm, mla_w_uq, mla_kv_norm, mla_w_ukv, swa_sink, w_out,
           peer_wq, peer_keys, peer_u, peer_v, final_norm_g):
    f = lambda a: np.ascontiguousarray(np.asarray(a), np.float32)
    P = dict(norm1_g=norm1_g, norm2_g=norm2_g, w_ada=w_ada, b_ada=b_ada, w_in=w_in, na_rpb=na_rpb,
             ml_conv=ml_conv, ml_gate_b=ml_gate_b, mla_q_norm=mla_q_norm, mla_w_uq=mla_w_uq,
             mla_kv_norm=mla_kv_norm, mla_w_ukv=mla_w_ukv, swa_sink=swa_sink, w_out=w_out,
             peer_wq=peer_wq, peer_keys=peer_keys, peer_u=peer_u, peer_v=peer_v)
    P = {k: f(v) for k, v in P.items()}
    xx, xc = f(x), f(ctx)
    cc, cctx, fg = f(c), f(c_ctx), f(final_norm_g)
    L = P["w_in"].shape[0]
    for l in range(L):
        lp = {k: v[l] for k, v in P.items()}
        xx, xc = run_layer(xx, xc, cc, cctx, lp, l < L - 1, l == L - 1, fg)
    return xx
```

```python
import numpy as np
import concourse.bass as bass
import concourse.mybir as mybir
from concourse.bass_utils import run_bass_kernel_spmd

F32 = mybir.dt.float32
BF16 = mybir.dt.bfloat16
AF = mybir.ActivationFunctionType
ALU = mybir.AluOpType
AX = mybir.AxisListType

D = 1024
CTX = 256
GW = 64
NCORES = 8
EPS = 1e-6
IN_W = 2736
EPOCH = 30000
NDMA = 6


class View:
    def __init__(self, t, ap):
        self.t = t
        self.ap = ap

    def __getitem__(self, idx):
        return View(self.t, self.ap[idx])

    def bc(self, shape):
        return View(self.t, self.ap.broadcast_to(list(shape)))

    def rr(self, s, **kw):
        return View(self.t, self.ap.rearrange(s, **kw))


class Tl:
    def __init__(self, h, is_dram=False):
        self.h = h
        self.last_w = None
        self.readers = {}
        self.is_dram = is_dram

    def __getitem__(self, idx):
        ap = self.h.ap() if self.is_dram else self.h
        return View(self, ap[idx])


class PB:
    def __init__(self):
        self.nc = bass.Bass("TRN2", target_bir_lowering=False)
        nc = self.nc
        self.eng = {"pe": nc.tensor, "dve": nc.vector, "act": nc.scalar, "pool": nc.gpsimd, "sp": nc.sync}
        self.sems = {}
        self.cnt = {}
        self.seen = {e: {} for e in self.eng}
        self.epoch = {e: 0 for e in ("pe", "dve", "act", "pool")}
        self.dma_rr = {}
        self.dma_ep = {}
        self.n_inst = 0
        self.uid = 0

    def _sem(self, key):
        if key not in self.sems:
            self.sems[key] = self.nc.alloc_semaphore(name="s%d" % len(self.sems))
            self.cnt[key] = 0
        return self.sems[key]

    def name(self, n):
        self.uid += 1
        return "%s_%d" % (n, self.uid)

    def din(self, name, shape, dt=F32):
        return Tl(self.nc.dram_tensor(name, list(shape), dt, kind="ExternalInput"), True)

    def dout(self, name, shape, dt=F32):
        return Tl(self.nc.dram_tensor(name, list(shape), dt, kind="ExternalOutput"), True)

    def sb(self, name, shape, dt=F32):
        return Tl(self.nc.alloc_sbuf_tensor(self.name(name), list(shape), dt))

    def ps(self, name, shape, dt=F32):
        return Tl(self.nc.alloc_psum_tensor(self.name(name), list(shape), dt))

    def _wait(self, e, deps):
        for key, v in deps:
            if e == "pe" and key[0] == "pe":
                continue
            if self.seen[e].get(key, 0) >= v:
                continue
            self.eng[e].wait_ge(self.sems[key], v)
            self.seen[e][key] = v

    @staticmethod
    def _deps(reads, writes):
        deps = []
        for t in reads:
            if t.last_w is not None:
                deps.append(t.last_w)
        for t in writes:
            if t.last_w is not None:
                deps.append(t.last_w)
            deps.extend(t.readers.items())
        return deps

    @staticmethod
    def _mark(dep, reads, writes):
        key, v = dep
        for t in reads:
            if t.readers.get(key, 0) < v:
                t.readers[key] = v
        for t in writes:
            t.last_w = dep
            t.readers = {}

    def op(self, e, reads, writes, fn):
        reads = [v.t for v in reads if isinstance(v, View)]
        writes = [v.t for v in writes]
        self._wait(e, self._deps(reads, writes))
        key = (e, self.epoch[e])
        sem = self._sem(key)
        inst = fn(self.eng[e])
        inst.then_inc(sem, 1)
        self.cnt[key] += 1
        self.n_inst += 1
        self._mark((key, self.cnt[key]), reads, writes)
        if self.cnt[key] >= EPOCH:
            self.epoch[e] += 1

    def dma(self, out, in_, q="sp"):
        i = self.dma_rr.get(q, 0)
        self.dma_rr[q] = (i + 1) % NDMA
        ep = self.dma_ep.get((q, i), 0)
        key = ("d" + q, i, ep)
        sem = self._sem(key)
        if self.cnt[key] >= 60000:
            self.dma_ep[(q, i)] = ep + 1
            old = (key, self.cnt[key])
            key = ("d" + q, i, ep + 1)
            sem = self._sem(key)
            self._wait(q, [old])
        deps = self._deps([in_.t], [out.t])
        if self.cnt[key] > 0:
            deps.append((key, self.cnt[key]))
        self._wait(q, deps)
        self.eng[q].dma_start(out=out.ap, in_=in_.ap).then_inc(sem, 16)
        self.cnt[key] += 16
        self.n_inst += 1
        self._mark((key, self.cnt[key]), [in_.t], [out.t])

    def finish(self):
        deps = [(k, v) for k, v in self.cnt.items() if v > 0]
        self._wait("sp", deps)

    def mm(self, out, lhsT, rhs, start=True, stop=True, skip=False):
        self.op("pe", [lhsT, rhs], [out],
                lambda e: e.matmul(out.ap, lhsT=lhsT.ap, rhs=rhs.ap, start=start, stop=stop, skip_group_check=skip))

    def tr(self, out, in_, ident):
        self.op("pe", [in_, ident], [out], lambda e: e.transpose(out.ap, in_.ap, ident.ap))

    def act(self, out, in_, func, bias=None, scale=1.0, accum=None, e="act"):
        kw = {}
        if bias is not None:
            kw["bias"] = bias.ap if isinstance(bias, View) else bias
        kw["scale"] = scale.ap if isinstance(scale, View) else scale
        w = [out]
        if accum is not None:
            kw["accum_out"] = accum.ap
            w.append(accum)
        self.op(e, [in_, bias, scale], w,
                lambda g: g.activation(out=out.ap, in_=in_.ap, func=func, **kw))

    def tt(self, out, a, b, op, e="dve"):
        self.op(e, [a, b], [out], lambda g: g.tensor_tensor(out=out.ap, in0=a.ap, in1=b.ap, op=op))

    def ts(self, out, a, s1, op0, s2=None, op1=None, e="dve", accum=None):
        x1 = s1.ap if isinstance(s1, View) else s1
        x2 = s2.ap if isinstance(s2, View) else s2
        kw = {}
        if op1 is not None:
            kw["op1"] = op1
        w = [out]
        if accum is not None:
            kw["accum_out"] = accum.ap
            w.append(accum)
        self.op(e, [a, s1, s2], w,
                lambda g: g.tensor_scalar(out=out.ap, in0=a.ap, scalar1=x1, scalar2=x2, op0=op0, **kw))

    def stt(self, out, a, s, b, op0, op1, e="dve"):
        x = s.ap if isinstance(s, View) else s
        self.op(e, [a, s, b], [out],
                lambda g: g.scalar_tensor_tensor(out=out.ap, in0=a.ap, scalar=x, in1=b.ap, op0=op0, op1=op1))

    def cp(self, out, in_, e="dve"):
        if e == "act":
            self.act(out, in_, AF.Copy)
        else:
            self.op(e, [in_], [out], lambda g: g.tensor_copy(out=out.ap, in_=in_.ap))

    def memset(self, out, val, e="dve"):
        self.op(e, [], [out], lambda g: g.memset(out.ap, val))

    def vmax8(self, out, in_):
        self.op("dve", [in_], [out], lambda g: g.max(out=out.ap, in_=in_.ap))

    def match_replace(self, out, vals, in_, imm):
        self.op("dve", [vals, in_], [out],
                lambda g: g.match_replace(out=out.ap, in_to_replace=vals.ap, in_values=in_.ap, imm_value=imm))

    def reduce(self, out, in_, op, e="dve"):
        self.op(e, [in_], [out], lambda g: g.tensor_reduce(out=out.ap, in_=in_.ap, axis=AX.X, op=op))


P_NAQ, P_NAK, P_NAV, P_MLQK, P_MLV, P_MLO, P_MLG = 0, 256, 512, 768, 1280, 1536, 1792
P_CQ, P_CKV, P_KR, P_SWQ, P_SWK, P_SWV = 1808, 2064, 2192, 2224, 2480, 2608
P_RSW, P_RKR, P_END = 2736, 3120, 3152
O_MLAQ, O_MLAK, O_MLAV, O_SWQ, O_SWK, O_SWV, O_END = 1808, 2192, 2576, 2832, 3088, 3216, 3344


def rstd(pb, out, ms):
    pb.act(out, ms, AF.Ln, bias=pb.eps_t[:, 0:1])
    pb.act(out, out, AF.Exp, scale=-0.5)


def rot_half(pb, dst, src, nblk, bs, e="dve"):
    h = bs // 2
    d3 = dst.rr("p (b t h) -> p b t h", b=nblk, t=2, h=h)
    s3 = src.rr("p (b t h) -> p b t h", b=nblk, t=2, h=h)
    pb.ts(d3[:, :, 0, :], s3[:, :, 1, :], -1.0, ALU.mult, e=e)
    pb.cp(d3[:, :, 1, :], s3[:, :, 0, :], e=e)


def stage(pb):
    if not hasattr(pb, "stg"):
        pb.stg = [pb.sb("stg%d" % i, [128, 8, 256], F32) for i in range(2)]
        pb.stg_i = 0
    pb.stg_i += 1
    return pb.stg[pb.stg_i % 2]


def load_mod_bc(pb, cT, w_ada, b_ada, col0, ncols, g, ps_pool, tag, sl):
    out = pb.sb("mod" + tag, [128, ncols], F32)
    pb.act(sl[:, :, :], cT[:, :, g:g + 1].bc([128, 8, 128]), AF.Silu)
    pb.dma(out[:, :], View(b_ada, b_ada.h.ap()[col0:col0 + ncols].partition_broadcast(128)))
    for n0 in range(0, ncols, 256):
        wst = stage(pb)
        pb.dma(wst[:, :, :], View(w_ada, w_ada.h.ap()[:, col0 + n0:col0 + n0 + 256].rearrange("(kc k) n -> k kc n", k=128)))
        pt = ps_pool[(n0 // 256) % len(ps_pool)]
        for kc in range(8):
            pb.mm(pt[:, 0:256], sl[:, kc, :], wst[:, kc, :], start=(kc == 0), stop=(kc == 7))
        pb.tt(out[:, n0:n0 + 256], pt[:, 0:256], out[:, n0:n0 + 256], ALU.add)
    return out


def build_l1(NB):
    pb = PB()
    x = pb.din("x", [NB, 128, D])
    cT = pb.din("cT", [128, 8, 2])
    ident_d = pb.din("ident", [128, 128])
    g1 = pb.din("norm_g", [D])
    w_ada = pb.din("w_ada", [D, 6 * D])
    b_ada = pb.din("b_ada", [6 * D])
    w_in = pb.din("w_in", [D, IN_W])
    qn = pb.din("q_norm", [256])
    kvn = pb.din("kv_norm", [128])
    w_uq = pb.din("w_uq", [256, 384])
    w_ukv = pb.din("w_ukv", [128, 512])
    cs_sw = pb.din("cs_sw", [NB, 128, 2, 384])
    cs_r = pb.din("cs_r", [NB, 128, 2, 32])
    out = pb.dout("out", [NB, 128, O_END])

    psA = [pb.ps("psA%d" % i, [128, 512]) for i in range(4)]
    psT = [pb.ps("psT%d" % i, [128, 512]) for i in range(2)]
    ident = pb.sb("ident", [128, 128])
    pb.dma(ident[:, :], ident_d[:, :])
    pb.eps_t = pb.sb("eps", [128, 1])
    pb.memset(pb.eps_t[:, :], EPS)
    cTs = pb.sb("cTs", [128, 8, 2])
    pb.dma(cTs[:, :, :], cT[:, :, :])
    mods = []
    sl = pb.sb("sl", [128, 8, 128])
    gbc = pb.sb("gbc", [128, D])
    pb.dma(gbc[:, :], View(g1, g1.h.ap().partition_broadcast(128)))
    for g in range(2):
        m = load_mod_bc(pb, cTs, w_ada, b_ada, 0, 2048, g, psA, "g%d" % g, sl)
        gm = pb.sb("gm%d" % g, [128, D])
        pb.stt(gm[:, :], m[:, 1024:2048], 1.0, gbc[:, :], ALU.add, ALU.mult)
        mods.append((gm, m))
    W = pb.sb("W", [128, 8, P_END], BF16)
    for n0 in range(0, IN_W, 256):
        n1 = min(IN_W, n0 + 256)
        st = stage(pb)
        pb.dma(st[:, :, 0:n1 - n0], View(w_in, w_in.h.ap()[:, n0:n1].rearrange("(kc k) n -> k kc n", k=128)))
        pb.cp(W[:, :, n0:n1], st[:, :, 0:n1 - n0], e="pool")
    for kc in range(8):
        rot_half(pb, W[:, kc, P_RSW:P_RSW + 384], W[:, kc, P_SWQ:P_SWQ + 384], 12, 32)
        rot_half(pb, W[:, kc, P_RKR:P_RKR + 32], W[:, kc, P_KR:P_KR + 32], 2, 16)
    uq32 = pb.sb("uq32", [128, 2, 384])
    pb.dma(uq32[:, :, :], View(w_uq, w_uq.h.ap().rearrange("(kc k) n -> k kc n", k=128)))
    Wuq = pb.sb("Wuq", [128, 2, 512], BF16)
    pb.cp(Wuq[:, :, 0:384], uq32[:, :, :])
    for kc in range(2):
        for h in range(4):
            rot_half(pb, Wuq[:, kc, 384 + 32 * h:384 + 32 * h + 32], Wuq[:, kc, 96 * h + 64:96 * h + 96], 2, 16)
    ukv32 = pb.sb("ukv32", [128, 512])
    pb.dma(ukv32[:, :], w_ukv[:, :])
    Wukv = pb.sb("Wukv", [128, 512], BF16)
    pb.cp(Wukv[:, :], ukv32[:, :])
    qnb = pb.sb("qnb", [128, 256])
    pb.dma(qnb[:, :], View(qn, qn.h.ap().partition_broadcast(128)))
    kvnb = pb.sb("kvnb", [128, 128])
    pb.dma(kvnb[:, :], View(kvn, kvn.h.ap().partition_broadcast(128)))

    NBUF = 2
    xt = [pb.sb("xt%d" % i, [128, D]) for i in range(NBUF)]
    hT = [pb.sb("hT%d" % i, [128, 8, 128], BF16) for i in range(NBUF)]
    Pb = [pb.sb("Pb%d" % i, [128, P_END]) for i in range(NBUF)]
    Qb = [pb.sb("Qb%d" % i, [128, O_SWV - O_MLAQ]) for i in range(NBUF)]
    cst = [pb.sb("cst%d" % i, [128, 2, 384]) for i in range(NBUF)]
    crt = [pb.sb("crt%d" % i, [128, 2, 32]) for i in range(NBUF)]
    sm = [pb.sb("sm%d" % i, [128, 8]) for i in range(NBUF)]
    tmp = [pb.sb("tmp%d" % i, [128, D]) for i in range(NBUF)]
    cqT = [pb.sb("cqT%d" % i, [128, 3, 128], BF16) for i in range(NBUF)]
    for b in range(NB):
        i = b % NBUF
        g = 1 if b == NB - 1 else 0
        gm, m = mods[g]
        X, H, Pt, Q, S, T = xt[i], hT[i], Pb[i], Qb[i], sm[i], tmp[i]
        pb.dma(X[:, :], x[b, :, :])
        pb.dma(cst[i][:, :, :], cs_sw[b, :, :, :])
        pb.dma(crt[i][:, :, :], cs_r[b, :, :, :])
        pb.act(T[:, :], X[:, :], AF.Square, accum=S[:, 0:1], scale=float(D) ** -0.5)
        rstd(pb, S[:, 1:2], S[:, 0:1])
        pb.stt(T[:, :], X[:, :], S[:, 1:2], gm[:, :], ALU.mult, ALU.mult)
        pb.tt(T[:, :], T[:, :], m[:, 0:1024], ALU.add, e="pool")
        for half in range(2):
            pt = psT[half]
            for j in range(4):
                kc = half * 4 + j
                pb.tr(pt[:, 128 * j:128 * j + 128], T[:, 128 * kc:128 * kc + 128], ident[:, :])
            pb.cp(H[:, 4 * half:4 * half + 4, :], pt[:, :].rr("p (a b) -> p a b", a=4), e=("act" if half else "dve"))
        nt = 0
        for n0 in range(0, P_END, 512):
            n1 = min(P_END, n0 + 512)
            pt = psA[nt % 4]
            for kc in range(8):
                pb.mm(pt[:, 0:n1 - n0], H[:, kc, :], W[:, kc, n0:n1], start=(kc == 0), stop=(kc == 7))
            pb.cp(Pt[:, n0:n1], pt[:, 0:n1 - n0], e=("act" if nt % 2 else "dve"))
            nt += 1
        pb.dma(out[b, :, 0:O_MLAQ], Pt[:, 0:P_CQ])
        pb.dma(out[b, :, O_SWV:O_END], Pt[:, P_SWV:P_SWV + 128])
        qo = lambda a, n: Q[:, a - O_MLAQ:a - O_MLAQ + n]
        pb.tt(qo(O_SWQ, 384), Pt[:, P_SWQ:P_SWQ + 384], cst[i][:, 0, :], ALU.mult)
        pb.tt(T[:, 0:384], Pt[:, P_RSW:P_RSW + 384], cst[i][:, 1, :], ALU.mult, e="pool")
        pb.tt(qo(O_SWQ, 384), qo(O_SWQ, 384), T[:, 0:384], ALU.add)
        pb.tt(T[:, 400:432], Pt[:, P_KR:P_KR + 32], crt[i][:, 0, :], ALU.mult)
        pb.tt(T[:, 432:464], Pt[:, P_RKR:P_RKR + 32], crt[i][:, 1, :], ALU.mult)
        pb.tt(T[:, 400:432], T[:, 400:432], T[:, 432:464], ALU.add)
        pb.act(T[:, 512:768], Pt[:, P_CQ:P_CQ + 256], AF.Square, accum=S[:, 2:3], scale=1.0 / 16.0)
        rstd(pb, S[:, 3:4], S[:, 2:3])
        pb.stt(T[:, 512:768], Pt[:, P_CQ:P_CQ + 256], S[:, 3:4], qnb[:, :], ALU.mult, ALU.mult)
        pb.act(T[:, 768:896], Pt[:, P_CKV:P_CKV + 128], AF.Square, accum=S[:, 4:5], scale=128.0 ** -0.5)
        rstd(pb, S[:, 5:6], S[:, 4:5])
        pb.stt(T[:, 768:896], Pt[:, P_CKV:P_CKV + 128], S[:, 5:6], kvnb[:, :], ALU.mult, ALU.mult)
        pt = psT[0]
        for j in range(3):
            pb.tr(pt[:, 128 * j:128 * j + 128], T[:, 512 + 128 * j:512 + 128 * j + 128], ident[:, :])
        pb.cp(cqT[i][:, :, :], pt[:, 0:384].rr("p (a b) -> p a b", a=3))
        pq = psA[nt % 4]
        pb.mm(pq[:, :], cqT[i][:, 0, :], Wuq[:, 0, :], start=True, stop=False)
        pb.mm(pq[:, :], cqT[i][:, 1, :], Wuq[:, 1, :], start=False, stop=True)
        pk = psA[(nt + 1) % 4]
        pb.mm(pk[:, :], cqT[i][:, 2, :], Wukv[:, :], start=True, stop=True)
        q3 = qo(O_MLAQ, 384).rr("p (h d) -> p h d", h=4)
        pq3 = pq[:, 0:384].rr("p (h d) -> p h d", h=4)
        pqr = pq[:, 384:512].rr("p (h d) -> p h d", h=4)
        pb.cp(q3[:, :, 0:64], pq3[:, :, 0:64], e="act")
        cosb = crt[i][:, 0:1, :].bc([128, 4, 32])
        sinb = crt[i][:, 1:2, :].bc([128, 4, 32])
        t3 = T[:, 0:128].rr("p (h d) -> p h d", h=4)
        pb.tt(q3[:, :, 64:96], pq3[:, :, 64:96], cosb, ALU.mult)
        pb.tt(t3, pqr, sinb, ALU.mult)
        pb.tt(q3[:, :, 64:96], q3[:, :, 64:96], t3, ALU.add)
        k3 = qo(O_MLAK, 384).rr("p (h d) -> p h d", h=4)
        v3 = qo(O_MLAV, 256).rr("p (h d) -> p h d", h=4)
        pk3 = pk[:, :].rr("p (h d) -> p h d", h=4)
        pb.cp(k3[:, :, 0:64], pk3[:, :, 0:64], e="act")
        pb.cp(v3[:, :, :], pk3[:, :, 64:128], e="act")
        pb.cp(k3[:, :, 64:96], T[:, 400:432].rr("p (o d) -> p o d", o=1).bc([128, 4, 32]), e="pool")
        pb.dma(out[b, :, O_MLAQ:O_SWV], Q[:, :])
    pb.finish()
    return pb


_CACHE = {}


def _prog(key, fn):
    if key not in _CACHE:
        _CACHE[key] = fn()
    return _CACHE[key]


def rope_tables(T):
    t = np.arange(T)
    row = (t // GW).astype(np.float32)
    col = (t % GW).astype(np.float32)

    def tab(rot):
        half = rot // 2
        inv = (1.0 / (np.float32(10000.0) ** (np.arange(0, half, 2, dtype=np.float32) / np.float32(half)))).astype(np.float32)
        ar = (row[:, None] * inv).astype(np.float32)
        ac = (col[:, None] * inv).astype(np.float32)
        cos = np.concatenate([np.cos(ar), np.cos(ar), np.cos(ac), np.cos(ac)], -1)
        sin = np.concatenate([np.sin(ar), np.sin(ar), np.sin(ac), np.sin(ac)], -1)
        return np.stack([cos, sin], 1).astype(np.float32)

    return tab(64), tab(32)


def ident_tables(n):
    one = np.ones((n, 2, 1), np.float32)
    one[:, 1] = 0
    return one


def cT_layout(cvec2):
    return np.ascontiguousarray(cvec2.reshape(2, 8, 128).transpose(2, 1, 0))


def run_l1(x, xc, c, c_ctx, lp):
    B, T, _ = x.shape
    NBL = T // 4 // 128
    NB = NBL + 1
    pb = _prog(("l1", NB), lambda: build_l1(NB))
    cs64, cs32 = rope_tables(T)
    cs_sw_lat = np.tile(cs64, (1, 1, 6))
    in_maps = []
    for k in range(NCORES):
        b, qd = k // 4, k % 4
        t0 = qd * (T // 4)
        xb = np.concatenate([x[b, t0:t0 + T // 4].reshape(NBL, 128, D),
                             xc[b, (k % 2) * 128:(k % 2) * 128 + 128][None]], 0)
        csw = np.concatenate([cs_sw_lat[t0:t0 + T // 4].reshape(NBL, 128, 2, 384),
                              np.broadcast_to(ident_tables(128), (128, 2, 384))[None]], 0)
        csr = np.concatenate([cs32[t0:t0 + T // 4].reshape(NBL, 128, 2, 32),
                              np.broadcast_to(ident_tables(128), (128, 2, 32))[None]], 0)
        in_maps.append({
            "x": np.ascontiguousarray(xb, np.float32),
            "cT": cT_layout(np.stack([c[b], c_ctx])),
            "ident": np.eye(128, dtype=np.float32),
            "norm_g": lp["norm1_g"], "w_ada": lp["w_ada"], "b_ada": lp["b_ada"], "w_in": lp["w_in"],
            "q_norm": lp["mla_q_norm"], "kv_norm": lp["mla_kv_norm"], "w_uq": lp["mla_w_uq"], "w_ukv": lp["mla_w_ukv"],
            "cs_sw": np.ascontiguousarray(csw, np.float32), "cs_r": np.ascontiguousarray(csr, np.float32),
        })
    res = run_bass_kernel_spmd(pb.nc, in_maps, core_ids=list(range(NCORES))).results
    P_lat = np.empty((B, T, O_END), np.float32)
    P_ctx = np.empty((B, CTX, O_END), np.float32)
    for k in range(NCORES):
        b, qd = k // 4, k % 4
        o = res[k]["out"]
        P_lat[b, qd * (T // 4):(qd + 1) * (T // 4)] = o[:NBL].reshape(T // 4, O_END)
        if qd < 2:
            P_ctx[b, qd * 128:(qd + 1) * 128] = o[NBL]
    return P_lat, P_ctx


def build_attn(NQB, H, Hkv, dq, dv, NKB, sched, n_bias, n_mask, use_sink, scale):
    pb = PB()
    NQ, NK = NQB * 128, NKB * 128
    QT = pb.din("QT", [H, dq, NQ])
    KT = pb.din("KT", [Hkv, dq, NK])
    V = pb.din("V", [NKB, 128, Hkv, dv])
    bias = pb.din("bias", [H, max(n_bias, 1), 128, 128])
    mask = pb.din("mask", [max(n_mask, 1), 128, 128])
    sink = pb.din("sink", [128, H])
    Yd = pb.dout("Y", [NQB, 128, H * dv])

    psS = [pb.ps("psS%d" % i, [128, 512]) for i in range(3)]
    psO = [pb.ps("psO%d" % i, [128, 4, 128]) for i in range(2)]
    QTb = pb.sb("QTb", [dq, H, NQ], BF16)
    KTb = pb.sb("KTb", [dq, NK], BF16)
    Vb = pb.sb("Vb", [128, NKB, dv + 1], BF16)
    Y = pb.sb("Y", [128, NQB, H * dv])
    CH = 2048
    kst = [pb.sb("kst%d" % i, [dq, CH]) for i in range(2)]
    VC = 16
    vst = [pb.sb("vst%d" % i, [128, VC, dv]) for i in range(2)]
    Et = [pb.sb("Et%d" % i, [128, 512], BF16) for i in range(3)]
    tmpS = [pb.sb("tmpS%d" % i, [128, 128]) for i in range(2)]
    bt = [pb.sb("bt%d" % i, [128, 128]) for i in range(3)]
    mt = [pb.sb("mt%d" % i, [128, 128]) for i in range(3)]
    den = [pb.sb("den%d" % i, [128, 4, 2]) for i in range(2)]
    sk = pb.sb("sk", [128, H])
    pb.dma(sk[:, :], sink[:, :])
    pb.act(sk[:, :], sk[:, :], AF.Exp)
    pb.memset(Vb[:, :, dv:dv + 1], 1.0)
    n = 0
    for h in range(H):
        for c0 in range(0, NQ, CH):
            c1 = min(NQ, c0 + CH)
            st = kst[n % 2]
            n += 1
            pb.dma(st[:, 0:c1 - c0], QT[h, :, c0:c1])
            pb.cp(QTb[:, h, c0:c1], st[:, 0:c1 - c0], e="pool")
    cnt = 0
    for hkv in range(Hkv):
        for c0 in range(0, NK, CH):
            c1 = min(NK, c0 + CH)
            st = kst[n % 2]
            n += 1
            pb.dma(st[:, 0:c1 - c0], KT[hkv, :, c0:c1])
            pb.cp(KTb[:, c0:c1], st[:, 0:c1 - c0], e="pool")
        for b0 in range(0, NKB, VC):
            b1 = min(NKB, b0 + VC)
            st = vst[n % 2]
            n += 1
            pb.dma(st[:, 0:b1 - b0, :], V[b0:b1, :, hkv, :].rr("b p d -> p b d"))
            pb.cp(Vb[:, b0:b1, 0:dv], st[:, 0:b1 - b0, :], e="pool")
        for h in range(hkv * (H // Hkv), (hkv + 1) * (H // Hkv)):
            for (i0, g, klist) in sched:
                po = psO[cnt % 2]
                dn = den[cnt % 2]
                cnt += 1
                nk = len(klist)
                def qk_exp(ki):
                    j, bid, mid = klist[ki]
                    ps = psS[ki % 3]
                    E = Et[ki % 3]
                    pb.mm(ps[:, 0:128 * g], KTb[:, 128 * j:128 * j + 128], QTb[:, h, 128 * i0:128 * (i0 + g)])
                    if bid is not None:
                        assert g == 1
                        B_ = bt[ki % 3]
                        pb.dma(B_[:, :], bias[h, bid, :, :])
                        tS = tmpS[ki % 2]
                        pb.stt(tS[:, :], ps[:, 0:128], scale, B_[:, :], ALU.mult, ALU.add)
                        pb.act(E[:, 0:128], tS[:, :], AF.Exp)
                    else:
                        pb.act(E[:, 0:128 * g], ps[:, 0:128 * g], AF.Exp, scale=scale)
                    if mid is not None:
                        assert g == 1
                        M_ = mt[ki % 3]
                        pb.dma(M_[:, :], mask[mid, :, :])
                        pb.tt(E[:, 0:128], E[:, 0:128], M_[:, :], ALU.mult, e="pool")

                def pv(ki):
                    j = klist[ki][0]
                    E = Et[ki % 3]
                    for s_ in range(g):
                        pb.mm(po[:, s_, 0:dv + 1], E[:, 128 * s_:128 * s_ + 128], Vb[:, j, :],
                              start=(ki == 0 and s_ == 0), stop=(ki == nk - 1), skip=(g > 1))

                qk_exp(0)
                for ki in range(nk):
                    if ki + 1 < nk:
                        qk_exp(ki + 1)
                    pv(ki)
                if use_sink:
                    pb.ts(dn[:, 0:g, 0:1], po[:, 0:g, dv:dv + 1], sk[:, h:h + 1], ALU.add)
                    pb.op("dve", [dn[:, :, :]], [dn[:, :, :]],
                          lambda e, dn=dn, g=g: e.reciprocal(out=dn.h[:, 0:g, 1:2], in_=dn.h[:, 0:g, 0:1]))
                else:
                    pb.op("dve", [po[:, :, :]], [dn[:, :, :]],
                          lambda e, dn=dn, po=po, g=g: e.reciprocal(out=dn.h[:, 0:g, 1:2], in_=po.h[:, 0:g, dv:dv + 1]))
                pb.tt(Y[:, i0:i0 + g, h * dv:(h + 1) * dv], po[:, 0:g, 0:dv],
                      dn[:, 0:g, 1:2].bc([128, g, dv]), ALU.mult)
    for i in range(NQB):
        pb.dma(Yd[i, :, :], Y[:, i, :])
    pb.finish()
    return pb


def _cls(i, NBL, edge):
    ncls = min(NBL, 2 * edge + 1)
    if i < edge:
        return i, ncls
    if i >= NBL - edge:
        return ncls - (NBL - i), ncls
    return edge, ncls


def _na_tiles(m, delta, nblk, rpb):
    rows = nblk * 2
    mk = m + delta
    if mk < 0 or mk >= nblk:
        return np.zeros((4, 128, 128), np.float32), np.zeros((128, 128), np.float32)
    idx = np.arange(128)
    qr, qc = 2 * m + idx // 64, idx % 64
    kr, kc = 2 * mk + idx // 64, idx % 64
    rs = np.clip(qr - 4, 0, rows - 8)
    cs = np.clip(qc - 8, 0, 64 - 16)
    valid = ((kr[:, None] >= rs[None, :]) & (kr[:, None] < rs[None, :] + 8) &
             (kc[:, None] >= cs[None, :]) & (kc[:, None] < cs[None, :] + 16))
    dr = np.clip(kr[:, None] - qr[None, :] + 7, 0, 14)
    dc = np.clip(kc[:, None] - qc[None, :] + 15, 0, 30)
    return np.ascontiguousarray(rpb[:, dr, dc], np.float32), valid.astype(np.float32)


def _swa_mask(m, delta, nblk):
    mk = m + delta
    if mk < 0 or mk >= nblk:
        return np.zeros((128, 128), np.float32)
    k = np.arange(128)[:, None]
    q = np.arange(128)[None, :]
    if delta == -1:
        return (q <= k).astype(np.float32)
    if delta == 1:
        return (k <= q).astype(np.float32)
    return np.ones((128, 128), np.float32)


def _halo(a, lo, hi):
    n = a.shape[0]
    out = np.zeros((hi - lo,) + a.shape[1:], a.dtype)
    s, e = max(lo, 0), min(hi, n)
    if e > s:
        out[s - lo:e - lo] = a[s:e]
    return out


def run_attn(kind, P_lat, P_ctx, lp):
    B, T, _ = P_lat.shape
    NBL = T // 4 // 128
    NBLT = T // 128
    NQB = NBL + 1
    if kind == "na":
        H, Hkv, dq, dv, halo, scale = 4, 4, 64, 64, 3, 64 ** -0.5
        oq, ok, ov = P_NAQ, P_NAK, P_NAV
    elif kind == "swa":
        H, Hkv, dq, dv, halo, scale = 4, 2, 64, 64, 1, 64 ** -0.5
        oq, ok, ov = O_SWQ, O_SWK, O_SWV
    else:
        H, Hkv, dq, dv, halo, scale = 4, 4, 96, 64, None, 96 ** -0.5
        oq, ok, ov = O_MLAQ, O_MLAK, O_MLAV
    NKB = 2 + (NBLT if halo is None else NBL + 2 * halo)
    sched = []
    ctxk = [(0, None, None), (1, None, None)]
    if kind == "mla":
        allk = [(j, None, None) for j in range(NKB)]
        for i0 in range(0, NBL, 4):
            sched.append((i0, min(4, NBL - i0), allk))
        n_bias = n_mask = 0
    else:
        nd = 2 * halo + 1
        edge = 2 if kind == "na" else 1
        for i in range(NBL):
            c, ncls = _cls(i, NBL, edge)
            kl = list(ctxk)
            for d in range(-halo, halo + 1):
                tid = c * nd + d + halo
                kl.append((2 + i + d + halo, tid if kind == "na" else None, tid))
            sched.append((i, 1, kl))
        n_mask = ncls * nd
        n_bias = n_mask if kind == "na" else 0
    sched.append((NBL, 1, ctxk))
    key = ("attn", kind, NQB)
    pb = _prog(key, lambda: build_attn(NQB, H, Hkv, dq, dv, NKB, sched, n_bias, n_mask, kind == "swa", scale))
    in_maps = []
    for k in range(NCORES):
        b, qd = k // 4, k % 4
        t0, t1 = qd * NBL * 128, (qd + 1) * NBL * 128
        ch = (k % 2) * 128
        q = np.concatenate([P_lat[b, t0:t1, oq:oq + H * dq], P_ctx[b, ch:ch + 128, oq:oq + H * dq]], 0)
        QT = np.ascontiguousarray(q.reshape(NQB * 128, H, dq).transpose(1, 2, 0))
        kl = P_lat[b, :, ok:ok + Hkv * dq].reshape(NBLT, 128, Hkv, dq)
        vl = P_lat[b, :, ov:ov + Hkv * dv].reshape(NBLT, 128, Hkv, dv)
        if halo is not None:
            kl = _halo(kl, qd * NBL - halo, (qd + 1) * NBL + halo)
            vl = _halo(vl, qd * NBL - halo, (qd + 1) * NBL + halo)
        kk = np.concatenate([P_ctx[b, :, ok:ok + Hkv * dq].reshape(2, 128, Hkv, dq), kl], 0)
        vv = np.concatenate([P_ctx[b, :, ov:ov + Hkv * dv].reshape(2, 128, Hkv, dv), vl], 0)
        KT = np.ascontiguousarray(kk.reshape(NKB * 128, Hkv, dq).transpose(1, 2, 0))
        bias = np.zeros((H, max(n_bias, 1), 128, 128), np.float32)
        mask = np.zeros((max(n_mask, 1), 128, 128), np.float32)
        if kind != "mla":
            for i in range(NBL):
                c, _ = _cls(i, NBL, edge)
                for d in range(-halo, halo + 1):
                    tid = c * nd + d + halo
                    if kind == "na":
                        g_, m_ = _na_tiles(qd * NBL + i, d, NBLT, lp["na_rpb"])
                        bias[:, tid] = g_
                        mask[tid] = m_
                    else:
                        mask[tid] = _swa_mask(qd * NBL + i, d, NBLT)
        in_maps.append({"QT": QT, "KT": KT, "V": np.ascontiguousarray(vv), "bias": bias, "mask": mask,
                        "sink": np.ascontiguousarray(np.broadcast_to(lp["swa_sink"][None, :], (128, H)), np.float32)})
    res = run_bass_kernel_spmd(pb.nc, in_maps, core_ids=list(range(NCORES))).results
    y_lat = np.empty((B, T, H * dv), np.float32)
    y_ctx = np.empty((B, CTX, H * dv), np.float32)
    for k in range(NCORES):
        b, qd = k // 4, k % 4
        o = res[k]["Y"]
        y_lat[b, qd * NBL * 128:(qd + 1) * NBL * 128] = o[:NBL].reshape(NBL * 128, H * dv)
        if qd < 2:
            y_ctx[b, qd * 128:(qd + 1) * 128] = o[NBL]
    return y_lat, y_ctx


SC = 16


def build_mlstm(T):
    pb = PB()
    NCH = (CTX + T) // 64
    LP = CTX + T + 8
    groups = [(0, 4)] + [(4 + SC * i, SC) for i in range((T // 64) // SC)]
    raw = pb.din("raw", [2, 64, LP])
    convw = pb.din("convw", [2, 64, 5])
    vd = pb.din("v", [64, NCH, 64])
    od = pb.din("o", [64, NCH, 64])
    gd = pb.din("g", [64, NCH, 4])
    gbd = pb.din("gb", [64, 4])
    cd = pb.din("consts", [64, 6, 64])
    out = pb.dout("out", [64, NCH, 64])
    hf = Tl(pb.nc.dram_tensor("hf_scratch", [64, NCH, 64], F32), True)

    cs = pb.sb("cs", [64, 6, 64])
    pb.dma(cs[:, :, :], cd[:, :, :])
    cw = pb.sb("cw", [64, 2, 5])
    pb.dma(cw[:, :, :], convw[:, :, :].rr("a p j -> p a j"))
    gb = pb.sb("gb", [64, 4])
    pb.dma(gb[:, :], gbd[:, :])
    one = pb.sb("one", [64, 1])
    pb.memset(one[:, :], 1.0)
    psS = [pb.ps("psS%d" % i, [64, 64]) for i in range(2)]
    psO = [pb.ps("psO%d" % i, [64, 65]) for i in range(2)]
    psU = [pb.ps("psU%d" % i, [64, 65]) for i in range(2)]
    psT = pb.ps("psT", [64, 512])
    psG = pb.ps("psG", [64, 3, SC])
    W = 64 * SC
    rw = [pb.sb("rw%d" % i, [64, W + 4]) for i in range(2)]
    qk = [pb.sb("qk%d" % i, [64, W]) for i in range(2)]
    ktok = pb.sb("ktok", [64, SC, 64])
    vaug = pb.sb("vaug", [64, SC, 65])
    pb.memset(vaug[:, :, 64:65], 1.0)
    gt = pb.sb("gt", [64, SC, 4])
    gi = pb.sb("gi", [64, SC])
    lf = pb.sb("lf", [64, SC])
    ex = pb.sb("ex", [64, 4, SC])
    tg = pb.sb("tg", [64, 2, SC])
    PT = [pb.sb("PT%d" % i, [64, 64]) for i in range(2)]
    kw = [pb.sb("kw%d" % i, [64, 64]) for i in range(2)]
    Cst = [pb.sb("C%d" % i, [64, 65]) for i in range(2)]
    Ob = pb.sb("Ob", [64, SC, 65])
    Hb = pb.sb("Hb", [64, SC, 64])
    H2 = pb.sb("H2", [64, SC, 64])
    ot = pb.sb("ot", [64, SC, 64])
    cf = pb.sb("cf", [64, 2, SC])
    for d in range(2):
        t_in, t_ex = (0, 1) if d == 0 else (2, 3)
        order = groups if d == 0 else [groups[0]] + groups[:0:-1]
        ci = 0
        pb.memset(Cst[0][:, :], 0.0)
        for (c0, n) in order:
            w = 64 * n
            start = (2 + 64 * c0) if c0 < 4 else (CTX + 6 + 64 * (c0 - 4))
            for a in range(2):
                pb.dma(rw[a][:, 0:w + 4], raw[a, :, start - 2:start + w + 2])
                acc = qk[a]
                pb.ts(acc[:, 0:w], rw[a][:, 0:w], cw[:, a, 0:1], ALU.mult)
                for j in range(1, 5):
                    pb.stt(acc[:, 0:w], rw[a][:, j:j + w], cw[:, a, j:j + 1], acc[:, 0:w], ALU.mult, ALU.add)
                pb.act(acc[:, 0:w], acc[:, 0:w], AF.Silu)
            pb.ts(qk[0][:, 0:w], qk[0][:, 0:w], 0.125, ALU.mult)
            pb.dma(vaug[:, 0:n, 0:64], vd[:, c0:c0 + n, :])
            pb.dma(gt[:, 0:n, :], gd[:, c0:c0 + n, :])
            for c in range(n):
                pb.tr(psT[:, 64 * (c % 8):64 * (c % 8) + 64], qk[1][:, 64 * c:64 * c + 64], cs[:, 5, :])
                if c % 8 == 7 or c == n - 1:
                    b0 = c - (c % 8)
                    pb.cp(ktok[:, b0:c + 1, :], psT[:, 0:64 * (c - b0 + 1)].rr("p (a b) -> p a b", b=64), e="act")
            pb.ts(gi[:, 0:n], gt[:, 0:n, 2 * d], gb[:, 2 * d:2 * d + 1], ALU.add)
            pb.ts(lf[:, 0:n], gt[:, 0:n, 2 * d + 1], gb[:, 2 * d + 1:2 * d + 2], ALU.add)
            pb.act(lf[:, 0:n], lf[:, 0:n], AF.Exp, scale=-1.0)
            pb.act(lf[:, 0:n], lf[:, 0:n], AF.Ln, bias=one[:, 0:1])
            pb.ts(lf[:, 0:n], lf[:, 0:n], -1.0, ALU.mult)
            for r, sel in enumerate((t_in, t_ex, 4)):
                pb.mm(psG[:, r, 0:n], cs[:, sel, :], lf[:, 0:n])
            pb.act(ex[:, 0, 0:n], psG[:, 0, 0:n], AF.Exp)
            pb.tt(tg[:, 0, 0:n], gi[:, 0:n], psG[:, 0, 0:n], ALU.subtract)
            pb.act(ex[:, 1, 0:n], tg[:, 0, 0:n], AF.Exp)
            pb.tt(tg[:, 1, 0:n], gi[:, 0:n], psG[:, 1, 0:n], ALU.add)
            pb.act(ex[:, 2, 0:n], tg[:, 1, 0:n], AF.Exp)
            pb.act(ex[:, 3, 0:n], psG[:, 2, 0:n], AF.Exp)
            chunks = list(range(n)) if d == 0 else list(range(n - 1, -1, -1))
            for c in chunks:
                Cc, Cn = Cst[ci % 2], Cst[(ci + 1) % 2]
                pS, pO, pU = psS[ci % 2], psO[ci % 2], psU[ci % 2]
                P_, K_ = PT[ci % 2], kw[ci % 2]
                ci += 1
                qT = qk[0][:, 64 * c:64 * c + 64]
                kT = qk[1][:, 64 * c:64 * c + 64]
                pb.mm(pS[:, :], kT, qT)
                pb.stt(P_[:, :], pS[:, :], ex[:, 1, c:c + 1], cs[:, t_in, :], ALU.mult, ALU.mult)
                pb.ts(K_[:, :], ktok[:, c, :], ex[:, 2, c:c + 1], ALU.mult, e="pool")
                pb.mm(pO[:, :], P_[:, :], vaug[:, c, :], start=True, stop=False)
                pb.mm(pO[:, :], qT, Cc[:, :], start=False, stop=True)
                pb.cp(Ob[:, c, :], pO[:, :], e="act")
                pb.mm(pU[:, :], K_[:, :], vaug[:, c, :])
                pb.stt(Cn[:, :], Cc[:, :], ex[:, 3, c:c + 1], pU[:, :], ALU.mult, ALU.add)
            pb.tt(cf[:, 0, 0:n], Ob[:, 0:n, 64], ex[:, 0, 0:n], ALU.mult)
            pb.act(cf[:, 0, 0:n], cf[:, 0, 0:n], AF.Abs)
            pb.ts(cf[:, 0, 0:n], cf[:, 0, 0:n], 1.0, ALU.max)
            pb.op("dve", [cf[:, :, :]], [cf[:, :, :]],
                  lambda e, n=n: e.reciprocal(out=cf.h[:, 1, 0:n], in_=cf.h[:, 0, 0:n]))
            pb.tt(cf[:, 1, 0:n], cf[:, 1, 0:n], ex[:, 0, 0:n], ALU.mult)
            pb.tt(Hb[:, 0:n, :], Ob[:, 0:n, 0:64], cf[:, 1, 0:n].rr("p (n o) -> p n o", o=1).bc([64, n, 64]), ALU.mult)
            if d == 0:
                pb.dma(hf[:, c0:c0 + n, :], Hb[:, 0:n, :])
            else:
                pb.dma(H2[:, 0:n, :], hf[:, c0:c0 + n, :])
                pb.dma(ot[:, 0:n, :], od[:, c0:c0 + n, :])
                pb.act(ot[:, 0:n, :], ot[:, 0:n, :], AF.Sigmoid)
                pb.tt(H2[:, 0:n, :], H2[:, 0:n, :], Hb[:, 0:n, :], ALU.add)
                pb.tt(H2[:, 0:n, :], H2[:, 0:n, :], ot[:, 0:n, :], ALU.mult, e="pool")
                pb.dma(out[:, c0:c0 + n, :], H2[:, 0:n, :])
    pb.finish()
    return pb


def run_mlstm(P_lat, P_ctx, lp):
    B, T, _ = P_lat.shape
    NCH = (CTX + T) // 64
    pb = _prog(("mlstm", T), lambda: build_mlstm(T))
    u = np.arange(64)[:, None]
    t = np.arange(64)[None, :]
    consts = np.stack([(u <= t), (u > t), (u >= t), (u < t), np.ones((64, 64), bool), (u == t)], 1).astype(np.float32)
    in_maps = []
    for k in range(NCORES):
        b, h = k // 4, k % 4
        seq = np.concatenate([P_ctx[b], P_lat[b]], 0)
        raw = np.zeros((2, 64, CTX + T + 8), np.float32)
        for a in range(2):
            col = P_MLQK + 256 * a + 64 * h
            raw[a, :, 2:2 + CTX] = seq[:CTX, col:col + 64].T
            raw[a, :, CTX + 6:CTX + 6 + T] = seq[CTX:, col:col + 64].T
        cw = np.stack([lp["ml_conv"][:, 256 * a + 64 * h:256 * a + 64 * h + 64].T for a in range(2)], 0)
        chunk = lambda a: np.ascontiguousarray(a.reshape(NCH, 64, -1).transpose(1, 0, 2))
        gcols = [P_MLG + 4 * j + h for j in range(4)]
        in_maps.append({
            "raw": raw, "convw": np.ascontiguousarray(cw, np.float32),
            "v": chunk(seq[:, P_MLV + 64 * h:P_MLV + 64 * h + 64]),
            "o": chunk(seq[:, P_MLO + 64 * h:P_MLO + 64 * h + 64]),
            "g": chunk(seq[:, gcols]),
            "gb": np.ascontiguousarray(np.broadcast_to(lp["ml_gate_b"][[h, 4 + h, 8 + h, 12 + h]][None, :], (64, 4)), np.float32),
            "consts": consts,
        })
    res = run_bass_kernel_spmd(pb.nc, in_maps, core_ids=list(range(NCORES))).results
    y_lat = np.empty((B, T, 256), np.float32)
    y_ctx = np.empty((B, CTX, 256), np.float32)
    for k in range(NCORES):
        b, h = k // 4, k % 4
        o = res[k]["out"].transpose(1, 0, 2).reshape(CTX + T, 64)
        y_ctx[b, :, 64 * h:64 * h + 64] = o[:CTX]
        y_lat[b, :, 64 * h:64 * h + 64] = o[CTX:]
    return y_lat, y_ctx


def build_out(NB, ctx_last):
    pb = PB()
    x = pb.din("x", [NB, 128, D])
    yc = pb.din("ycat", [NB, 128, D])
    cT = pb.din("cT", [128, 8, 2])
    ident_d = pb.din("ident", [128, 128])
    g2n = pb.din("norm_g", [D])
    w_ada = pb.din("w_ada", [D, 6 * D])
    b_ada = pb.din("b_ada", [6 * D])
    w_out = pb.din("w_out", [D, D])
    wq_d = pb.din("wq", [D, 2048])
    keysT = pb.din("keysT", [128, 16, 128])
    x1o = pb.dout("x1", [NB, 128, D])
    h2o = pb.dout("h2", [NB, 128, D])
    shpo = pb.dout("shp", [NB, 128, 16, 128])
    paro = pb.dout("par", [NB, 128, 8, 2])

    psA = [pb.ps("psA%d" % i, [128, 512]) for i in range(4)]
    psT = [pb.ps("psT%d" % i, [128, 512]) for i in range(2)]
    psK = [pb.ps("psK%d" % i, [128, 512]) for i in range(2)]
    ident = pb.sb("ident", [128, 128])
    pb.dma(ident[:, :], ident_d[:, :])
    pb.eps_t = pb.sb("eps", [128, 1])
    pb.memset(pb.eps_t[:, :], EPS)
    cTs = pb.sb("cTs", [128, 8, 2])
    pb.dma(cTs[:, :, :], cT[:, :, :])
    QR = pb.sb("QR", [128, 2048])
    QT = pb.sb("QT", [128, 16, 128])
    svs = [QR[:, :].rr("p (a b) -> p a b", a=8), QT[:, :, :].rr("p (a c) b -> p a (c b)", a=8)]
    sl = pb.sb("sl", [128, 8, 128])
    gbc = pb.sb("gbc", [128, D])
    pb.dma(gbc[:, :], View(g2n, g2n.h.ap().partition_broadcast(128)))
    mods = []
    for g in range(2 if ctx_last else 1):
        m = pb.sb("modo%d" % g, [128, 4096])
        pb.act(sl[:, :, :], cTs[:, :, g:g + 1].bc([128, 8, 128]), AF.Silu)
        pb.dma(m[:, :], View(b_ada, b_ada.h.ap()[2048:6144].partition_broadcast(128)))
        for n0 in range(0, 4096, 256):
            sv = svs[(n0 // 256) % 2]
            pb.dma(sv, View(w_ada, w_ada.h.ap()[:, 2048 + n0:2048 + n0 + 256].rearrange("(kc k) n -> k kc n", k=128)))
            pt = psA[(n0 // 256) % 4]
            for kc in range(8):
                pb.mm(pt[:, 0:256], sl[:, kc, :], sv[:, kc, :], start=(kc == 0), stop=(kc == 7))
            pb.tt(m[:, n0:n0 + 256], pt[:, 0:256], m[:, n0:n0 + 256], ALU.add)
        pb.stt(m[:, 2048:3072], m[:, 2048:3072], 1.0, gbc[:, :], ALU.add, ALU.mult)
        mods.append(m)
    Wo = pb.sb("Wo", [128, 8, D], BF16)
    for kc in range(8):
        pb.dma(QR[:, 0:1024], w_out[128 * kc:128 * kc + 128, :])
        pb.cp(Wo[:, kc, :], QR[:, 0:1024])
    wq = pb.sb("wq", [128, 8, 2048])
    for kc in range(8):
        pb.dma(wq[:, kc, :], wq_d[128 * kc:128 * kc + 128, :])
    kT = pb.sb("kT", [128, 16, 128])
    pb.dma(kT[:, :, :], keysT[:, :, :])

    X = pb.sb("X", [128, D])
    Yc = pb.sb("Yc", [128, D])
    YT = pb.sb("YT", [128, 8, 128], BF16)
    X1 = pb.sb("X1", [128, D])
    T = pb.sb("T", [128, D])
    H2 = pb.sb("H2", [128, D])
    H2T = pb.sb("H2T", [128, 8, 128])
    SHP = pb.sb("SHP", [128, 16, 128])
    T16 = pb.sb("T16", [128, 16, 16])
    CAND = pb.sb("CAND", [128, 8, 256])
    F16 = pb.sb("F16", [128, 8, 16])
    tS = pb.sb("tS", [128, 256])
    S = pb.sb("S", [128, 8])
    Z = pb.sb("Z", [128, 8, 2])
    PAR = pb.sb("PAR", [128, 8, 2])
    tF = pb.sb("tF", [128, 8, 16])
    for b in range(NB):
        m = mods[1 if (ctx_last and b == NB - 1) else 0]
        pb.dma(X[:, :], x[b, :, :])
        pb.dma(Yc[:, :], yc[b, :, :])
        for half in range(2):
            for j in range(4):
                kc = half * 4 + j
                pb.tr(psT[half][:, 128 * j:128 * j + 128], Yc[:, 128 * kc:128 * kc + 128], ident[:, :])
            pb.cp(YT[:, 4 * half:4 * half + 4, :], psT[half][:, :].rr("p (a b) -> p a b", a=4), e=("act" if half else "dve"))
        for nt in range(2):
            for kc in range(8):
                pb.mm(psA[nt][:, :], YT[:, kc, :], Wo[:, kc, 512 * nt:512 * nt + 512], start=(kc == 0), stop=(kc == 7))
            pb.tt(T[:, 512 * nt:512 * nt + 512], psA[nt][:, :], m[:, 512 * nt:512 * nt + 512], ALU.mult)
        pb.tt(X1[:, :], X[:, :], T[:, :], ALU.add, e="pool")
        pb.dma(x1o[b, :, :], X1[:, :])
        pb.act(T[:, :], X1[:, :], AF.Square, accum=S[:, 0:1], scale=float(D) ** -0.5)
        rstd(pb, S[:, 1:2], S[:, 0:1])
        pb.stt(H2[:, :], X1[:, :], S[:, 1:2], m[:, 2048:3072], ALU.mult, ALU.mult)
        pb.tt(H2[:, :], H2[:, :], m[:, 1024:2048], ALU.add, e="pool")
        pb.dma(h2o[b, :, :], H2[:, :])
        for half in range(2):
            for j in range(4):
                kc = half * 4 + j
                pb.tr(psT[half][:, 128 * j:128 * j + 128], H2[:, 128 * kc:128 * kc + 128], ident[:, :])
            pb.cp(H2T[:, 4 * half:4 * half + 4, :], psT[half][:, :].rr("p (a b) -> p a b", a=4), e=("act" if half else "dve"))
        for nt in range(4):
            pt = psA[2 + nt % 2]
            for kc in range(8):
                pb.mm(pt[:, :], H2T[:, kc, :], wq[:, kc, 512 * nt:512 * nt + 512], start=(kc == 0), stop=(kc == 7))
            pb.cp(QR[:, 512 * nt:512 * nt + 512], pt[:, :], e=("act" if nt % 2 else "dve"))
        for q4 in range(4):
            pt = psT[q4 % 2]
            for j in range(4):
                pb.tr(pt[:, 128 * j:128 * j + 128], QR[:, 128 * (4 * q4 + j):128 * (4 * q4 + j) + 128], ident[:, :])
            pb.cp(QT[:, 4 * q4:4 * q4 + 4, :], pt[:, :].rr("p (a b) -> p a b", a=4), e=("act" if q4 % 2 else "dve"))
        for q4 in range(4):
            pt = psK[q4 % 2]
            for j in range(4):
                pb.mm(pt[:, 128 * j:128 * j + 128], QT[:, 4 * q4 + j, :], kT[:, 4 * q4 + j, :], start=True, stop=True)
            pb.cp(SHP[:, 4 * q4:4 * q4 + 4, :], pt[:, :].rr("p (a b) -> p a b", a=4), e=("act" if q4 % 2 else "dve"))
        pb.dma(shpo[b, :, :, :], SHP[:, :, :])
        for j in range(16):
            pb.vmax8(T16[:, j, 0:8], SHP[:, j, :])
            pb.match_replace(tS[:, 0:128], T16[:, j, 0:8], SHP[:, j, :], -1e30)
            pb.vmax8(T16[:, j, 8:16], tS[:, 0:128])
        A = T16[:, :, :].rr("p (h two) r -> p h two r", two=2)
        pb.tt(CAND[:, :, :].rr("p h (a b) -> p h a b", a=16),
              A[:, :, 0, :].rr("p h (a o) -> p h a o", o=1).bc([128, 8, 16, 16]),
              A[:, :, 1, :].rr("p h (o b) -> p h o b", o=1).bc([128, 8, 16, 16]), ALU.add)
        for h in range(8):
            pb.vmax8(F16[:, h, 0:8], CAND[:, h, :])
            pb.match_replace(tS[:, :], F16[:, h, 0:8], CAND[:, h, :], -1e30)
            pb.vmax8(F16[:, h, 8:16], tS[:, :])
        pb.tt(tF[:, :, :], F16[:, :, :], F16[:, :, 0:1].bc([128, 8, 16]), ALU.subtract)
        pb.act(tF[:, :, :], tF[:, :, :], AF.Exp)
        pb.reduce(Z[:, :, 0], tF[:, :, :], ALU.add)
        pb.act(Z[:, :, 1], Z[:, :, 0], AF.Ln)
        pb.cp(PAR[:, :, 0], F16[:, :, 15], e="pool")
        pb.tt(PAR[:, :, 1], F16[:, :, 0], Z[:, :, 1], ALU.add)
        pb.ts(PAR[:, :, 1], PAR[:, :, 1], -1.0, ALU.mult)
        pb.dma(paro[b, :, :, :], PAR[:, :, :])
    pb.finish()
    return pb


IC = 8


def build_peer(NB, ctx_last, final):
    pb = PB()
    h2T = pb.din("h2T", [NB, 128, 8, 128])
    shp = pb.din("shp", [NB, 128, 16, 128])
    par = pb.din("par", [NB, 128, 8, 2])
    x1 = pb.din("x1", [NB, 128, D])
    cT = pb.din("cT", [128, 8, 2])
    ident_d = pb.din("ident", [128, 128])
    w_ada = pb.din("w_ada", [D, 6 * D])
    b_ada = pb.din("b_ada", [6 * D])
    uT = pb.din("uT", [128, 8, 16384])
    vd = pb.din("v", [16384, D])
    fg = pb.din("final_g", [D])
    out = pb.dout("out", [NB, 128, D])

    psO = [pb.ps("psO%d" % i, [128, 512]) for i in range(4)]
    psA = [pb.ps("psA%d" % i, [128, 256]) for i in range(2)]
    psW = [pb.ps("psW%d" % i, [128, 256]) for i in range(2)]
    psM = psO[0]
    ident = pb.sb("ident", [128, 128])
    pb.dma(ident[:, :], ident_d[:, :])
    pb.eps_t = pb.sb("eps", [128, 1])
    pb.memset(pb.eps_t[:, :], EPS)
    cTs = pb.sb("cTs", [128, 8, 2])
    pb.dma(cTs[:, :, :], cT[:, :, :])
    sl = pb.sb("sl", [128, 8, 128])
    fgb = pb.sb("fgb", [128, D])
    pb.dma(fgb[:, :], View(fg, fg.h.ap().partition_broadcast(128)))
    stg = [pb.sb("stg%d" % i, [128, 8, 256]) for i in range(2)]
    mods = []
    for g in range(2 if ctx_last else 1):
        m = pb.sb("modp%d" % g, [128, D])
        pb.act(sl[:, :, :], cTs[:, :, g:g + 1].bc([128, 8, 128]), AF.Silu)
        pb.dma(m[:, :], View(b_ada, b_ada.h.ap()[5120:6144].partition_broadcast(128)))
        for n0 in range(0, D, 256):
            sv = stg[(n0 // 256) % 2]
            pb.dma(sv[:, :, :], View(w_ada, w_ada.h.ap()[:, 5120 + n0:5120 + n0 + 256].rearrange("(kc k) n -> k kc n", k=128)))
            for kc in range(8):
                pb.mm(psM[:, 0:256], sl[:, kc, :], sv[:, kc, :], start=(kc == 0), stop=(kc == 7))
            pb.tt(m[:, n0:n0 + 256], psM[:, 0:256], m[:, n0:n0 + 256], ALU.add)
        mods.append(m)

    ub = Tl(pb.nc.dram_tensor("ub_scratch", [128, 128, 1024], BF16), True)
    vb = Tl(pb.nc.dram_tensor("vb_scratch", [128, 128, 1024], BF16), True)
    c32 = [pb.sb("c32_%d" % i, [128, 1024]) for i in range(2)]
    c16 = [pb.sb("c16_%d" % i, [128, 1024], BF16) for i in range(2)]
    for i in range(128):
        a, b_ = c32[0], c32[1]
        a16, b16 = c16[0], c16[1]
        pb.dma(a[:, :].rr("p (k e) -> p k e", k=8), uT[:, :, 128 * i:128 * i + 128])
        pb.dma(b_[:, :], vd[128 * i:128 * i + 128, :])
        pb.cp(a16[:, :], a[:, :], e="act")
        pb.cp(b16[:, :], b_[:, :], e="dve")
        pb.dma(ub[i, :, :], a16[:, :])
        pb.dma(vb[i, :, :], b16[:, :])
    HT32 = pb.sb("HT32", [128, 8, 128])
    HT = pb.sb("HT", [128, 8, 256], BF16)
    SH = pb.sb("SH", [128, 2, 16, 128])
    PR = pb.sb("PR", [128, 2, 8, 2])
    X1 = pb.sb("X1", [128, 2, D])
    Sb = [pb.sb("Sb%d" % i, [128, IC, 128]) for i in range(4)]
    Eb = [pb.sb("Eb%d" % i, [128, IC, 128]) for i in range(4)]
    Whb = [[[pb.sb("Wh%d_%d_%d" % (p, k, h), [128, IC, 128], BF16) for h in range(8)] for k in range(2)] for p in range(2)]
    identb = pb.sb("identb", [128, 128], BF16)
    pb.cp(identb[:, :], ident[:, :])
    ut = [pb.sb("ut%d" % i, [128, 8, 128], BF16) for i in range(3)]
    vt = [pb.sb("vt%d" % i, [128, D], BF16) for i in range(3)]
    Gs = [pb.sb("Gs%d" % i, [128, 256]) for i in range(2)]
    GT = [pb.sb("GT%d" % i, [128, 256], BF16) for i in range(2)]
    O = pb.sb("O", [128, D])
    T = pb.sb("T", [128, D])
    S = pb.sb("S", [128, 4])
    tiles = [(b0, min(2, NB - b0)) for b0 in range(0, NB, 2)]
    cnt = 0
    wcnt = 0
    for (b0, ts) in tiles:
        tw = 128 * ts
        for k in range(ts):
            pb.dma(HT32[:, :, :], h2T[b0 + k, :, :, :])
            pb.cp(HT[:, :, 128 * k:128 * k + 128], HT32[:, :, :], e="act")
            pb.dma(SH[:, k, :, :], shp[b0 + k, :, :, :])
            pb.dma(PR[:, k, :, :], par[b0 + k, :, :, :])
            pb.dma(X1[:, k, :], x1[b0 + k, :, :])
        def dense(ic, sub):
            nonlocal wcnt
            Wc = Whb[ic % 2]
            kh = [(k, h) for k in range(ts) for h in range(8)]
            todo = kh if sub is None else kh[sub::IC]
            for p0 in range(0, len(todo), 2):
                grp = []
                for (k, h) in todo[p0:p0 + 2]:
                    S_, E_ = Sb[wcnt % 4], Eb[wcnt % 4]
                    wcnt += 1
                    grp.append((k, h, S_, E_))
                    pb.tt(S_[:, :, :],
                          SH[:, k, 2 * h, IC * ic:IC * ic + IC].rr("p (a o) -> p a o", o=1).bc([128, IC, 128]),
                          SH[:, k, 2 * h + 1, :].rr("p (o b) -> p o b", o=1).bc([128, IC, 128]), ALU.add)
                for (k, h, S_, E_) in grp:
                    pb.act(E_[:, :, :], S_[:, :, :], AF.Exp, bias=PR[:, k, h, 1:2])
                for (k, h, S_, E_) in grp:
                    pb.stt(Wc[k][h][:, :, :], S_[:, :, :], PR[:, k, h, 0:1], E_[:, :, :], ALU.is_ge, ALU.mult)

        def stage_a(i):
            nonlocal cnt
            Wc = Whb[(i // IC) % 2]
            ii = i % IC
            U_, V_ = ut[cnt % 3], vt[cnt % 3]
            pA, pW = psA[cnt % 2], psW[cnt % 2]
            G1, G2 = Gs[cnt % 2], GT[cnt % 2]
            cnt += 1
            pb.dma(U_[:, :, :].rr("p k e -> p (k e)"), ub[i, :, :])
            pb.dma(V_[:, :], vb[i, :, :])
            for k in range(ts):
                for h in range(8):
                    pb.mm(pW[:, 128 * k:128 * k + 128], Wc[k][h][:, ii, :], identb[:, :],
                          start=(h == 0), stop=(h == 7), skip=True)
            for kc in range(8):
                pb.mm(pA[:, 0:tw], U_[:, kc, :], HT[:, kc, 0:tw], start=(kc == 0), stop=(kc == 7))
            pb.act(G1[:, 0:tw], pA[:, 0:tw], AF.Gelu)
            pb.tt(G2[:, 0:tw], G1[:, 0:tw], pW[:, 0:tw], ALU.mult)
            return G2, V_

        def stage_b(i, G2, V_):
            for k in range(ts):
                for nt in range(2):
                    pb.mm(psO[2 * k + nt][:, :], G2[:, 128 * k:128 * k + 128], V_[:, 512 * nt:512 * nt + 512],
                          start=(i == 0), stop=(i == 127))

        dense(0, None)
        prev = None
        for i in range(128):
            ic, ii = i // IC, i % IC
            cur = stage_a(i)
            if prev is not None:
                stage_b(i - 1, *prev)
            prev = cur
            if ic + 1 < 128 // IC:
                dense(ic + 1, ii)
        stage_b(127, *prev)
        for k in range(ts):
            b = b0 + k
            m = mods[1 if (ctx_last and b == NB - 1) else 0]
            for nt in range(2):
                pb.tt(T[:, 512 * nt:512 * nt + 512], psO[2 * k + nt][:, :], m[:, 512 * nt:512 * nt + 512], ALU.mult)
            pb.tt(O[:, :], X1[:, k, :], T[:, :], ALU.add, e="pool")
            if final:
                pb.act(T[:, :], O[:, :], AF.Square, accum=S[:, 0:1], scale=float(D) ** -0.5)
                rstd(pb, S[:, 1:2], S[:, 0:1])
                pb.stt(O[:, :], O[:, :], S[:, 1:2], fgb[:, :], ALU.mult, ALU.mult)
            pb.dma(out[b, :, :], O[:, :])
    pb.finish()
    return pb


def run_out_peer(x, xc, ycat_lat, ycat_ctx, c, c_ctx, lp, need_ctx, final, final_g):
    B, T, _ = x.shape
    NBL = T // 4 // 128
    NB = NBL + (1 if need_ctx else 0)
    pbo = _prog(("out", NB, need_ctx), lambda: build_out(NB, need_ctx))
    pbp = _prog(("peer", NB, need_ctx, final), lambda: build_peer(NB, need_ctx, final))
    eye = np.eye(128, dtype=np.float32)
    keysT = np.ascontiguousarray(lp["peer_keys"].reshape(16, 128, 128).transpose(2, 0, 1))
    uT = np.ascontiguousarray(lp["peer_u"].reshape(16384, 8, 128).transpose(2, 1, 0))

    def blocks(lat, ctx, k):
        b, qd = k // 4, k % 4
        a = lat[b, qd * NBL * 128:(qd + 1) * NBL * 128].reshape(NBL, 128, -1)
        if need_ctx:
            a = np.concatenate([a, ctx[b, (k % 2) * 128:(k % 2) * 128 + 128][None]], 0)
        return np.ascontiguousarray(a, np.float32)

    in_maps = []
    for k in range(NCORES):
        b = k // 4
        in_maps.append({"x": blocks(x, xc, k), "ycat": blocks(ycat_lat, ycat_ctx, k),
                        "cT": cT_layout(np.stack([c[b], c_ctx])), "ident": eye, "norm_g": lp["norm2_g"],
                        "w_ada": lp["w_ada"], "b_ada": lp["b_ada"], "w_out": lp["w_out"], "wq": lp["peer_wq"],
                        "keysT": keysT})
    r1 = run_bass_kernel_spmd(pbo.nc, in_maps, core_ids=list(range(NCORES))).results
    in_maps = []
    for k in range(NCORES):
        b = k // 4
        h2 = r1[k]["h2"]
        h2T = np.ascontiguousarray(h2.reshape(NB, 128, 8, 128).transpose(0, 3, 2, 1))
        in_maps.append({"h2T": h2T, "shp": r1[k]["shp"], "par": r1[k]["par"], "x1": r1[k]["x1"],
                        "cT": cT_layout(np.stack([c[b], c_ctx])), "ident": eye,
                        "w_ada": lp["w_ada"], "b_ada": lp["b_ada"], "uT": uT, "v": lp["peer_v"],
                        "final_g": final_g})
    r2 = run_bass_kernel_spmd(pbp.nc, in_maps, core_ids=list(range(NCORES))).results
    xn = np.empty_like(x)
    xcn = np.empty_like(xc) if need_ctx else None
    for k in range(NCORES):
        b, qd = k // 4, k % 4
        o = r2[k]["out"]
        xn[b, qd * NBL * 128:(qd + 1) * NBL * 128] = o[:NBL].reshape(NBL * 128, D)
        if need_ctx and qd < 2:
            xcn[b, qd * 128:(qd + 1) * 128] = o[NBL]
    return xn, xcn


def _tick(msg, t0=[None]):
    import time
    now = time.time()
    if t0[0] is not None:
        print("[kernel] %s %.1fs" % (msg, now - t0[0]), flush=True)
    t0[0] = now


def run_layer(x, xc, c, c_ctx, lp, need_ctx, final, final_g):
    _tick("start")
    P_lat, P_ctx = run_l1(x, xc, c, c_ctx, lp)
    _tick("l1")
    ya, yac = run_attn("na", P_lat, P_ctx, lp)
    _tick("na")
    yb, ybc = run_mlstm(P_lat, P_ctx, lp)
    _tick("mlstm")
    ym, ymc = run_attn("mla", P_lat, P_ctx, lp)
    _tick("mla")
    yd, ydc = run_attn("swa", P_lat, P_ctx, lp)
    _tick("swa")
    ycat = np.concatenate([ya, yb, ym, yd], -1)
    ycatc = np.concatenate([yac, ybc, ymc, ydc], -1)
    return run_out_peer(x, xc, ycat, ycatc, c, c_ctx, lp, need_ctx, final, final_g)


def kernel(x, c, ctx, c_ctx, norm1_g, norm2_g, w_ada, b_ada, w_in, na_rpb, ml_conv, ml_gate_b,
           mla_q_norm, mla_w_uq, mla_kv_norm, mla_w_ukv, swa_sink, w_out,
           peer_wq, peer_keys, peer_u, peer_v, final_norm_g):
    f = lambda a: np.ascontiguousarray(np.asarray(a), np.float32)
    P = dict(norm1_g=norm1_g, norm2_g=norm2_g, w_ada=w_ada, b_ada=b_ada, w_in=w_in, na_rpb=na_rpb,
             ml_conv=ml_conv, ml_gate_b=ml_gate_b, mla_q_norm=mla_q_norm, mla_w_uq=mla_w_uq,
             mla_kv_norm=mla_kv_norm, mla_w_ukv=mla_w_ukv, swa_sink=swa_sink, w_out=w_out,
             peer_wq=peer_wq, peer_keys=peer_keys, peer_u=peer_u, peer_v=peer_v)
    P = {k: f(v) for k, v in P.items()}
    xx, xc = f(x), f(ctx)
    cc, cctx, fg = f(c), f(c_ctx), f(final_norm_g)
    L = P["w_in"].shape[0]
    for l in range(L):
        lp = {k: v[l] for k, v in P.items()}
        xx, xc = run_layer(xx, xc, cc, cctx, lp, l < L - 1, l == L - 1, fg)
    return xx
```
